# Optimizing a Trainium2 kernel written in Bass

```python
import jax
import jax.numpy as jnp
from jax import lax
import numpy as np

D_MODEL = 4096
BATCH = 4
SEQ = 2048
DEPTH = 1

GRID_W = 64
CTX_LEN = 256
NORM_EPS = 1e-6
N_MOD = 6

M_HEADS = 8
M_DQK = 256
M_DV = 512
M_CHUNK = 64
FORGET_BIAS = 3.0

A_HEADS = 32
A_NOPE = 128
A_ROPE = 64
A_DV = 128
Q_LORA = 1024
KV_LORA = 512
Q_BLOCK = 128
ROPE_BASE = 10000.0
A_SCALE = (A_NOPE + A_ROPE) ** -0.5

N_EXPERTS = 32
TOP_K = 4
D_EXPERT = 1536
SWIGLU_LIMIT = 7.0
SWIGLU_ALPHA = 1.702
EXPERT_BLOCK = 128

M_QK_W = M_HEADS * M_DQK
M_V_W = M_HEADS * M_DV
A_V_W = A_HEADS * A_DV
N_GATE_COLS = 4 * M_HEADS
IN_SPLITS = (M_QK_W, M_QK_W, M_V_W, M_V_W, N_GATE_COLS, Q_LORA, KV_LORA, A_ROPE, D_MODEL, D_MODEL)
D_IN = sum(IN_SPLITS)
IN_SPLIT_POINTS = tuple(int(s) for s in np.cumsum(IN_SPLITS)[:-1])

kernel_name = "hybrid_mlstm_mla_moe_diffusion_block"


def rmsnorm(x, g):
    xf = x.astype(jnp.float32)
    y = xf * lax.rsqrt(jnp.mean(xf * xf, axis=-1, keepdims=True) + NORM_EPS)
    return y.astype(x.dtype) * g


def axial_rope_tables(rows, dtype):
    r, col = jnp.meshgrid(jnp.arange(rows, dtype=jnp.float32), jnp.arange(GRID_W, dtype=jnp.float32), indexing="ij")
    n_freq = A_ROPE // 4
    inv = ROPE_BASE ** (-jnp.arange(n_freq, dtype=jnp.float32) / n_freq)
    ang = jnp.concatenate([r.reshape(-1, 1) * inv, col.reshape(-1, 1) * inv], axis=-1)
    return jnp.cos(ang).astype(dtype), jnp.sin(ang).astype(dtype)


def apply_rope(x, cos, sin):
    xp = x.reshape(*x.shape[:-1], A_ROPE // 2, 2)
    x1, x2 = xp[..., 0], xp[..., 1]
    bshape = (1, cos.shape[0]) + (1,) * (x.ndim - 3) + (cos.shape[1],)
    c, s = cos.reshape(bshape), sin.reshape(bshape)
    return jnp.stack([x1 * c - x2 * s, x1 * s + x2 * c], axis=-1).reshape(x.shape)


def mlstm_prep(zq, zk, zv, zg, b_gates):
    B, S, _ = zq.shape
    q = zq.reshape(B, S, M_HEADS, M_DQK)
    k = zk.reshape(B, S, M_HEADS, M_DQK) * (M_DQK ** -0.5)
    v = zv.reshape(B, S, M_HEADS, M_DV)
    g = zg.astype(jnp.float32) + b_gates.astype(jnp.float32)
    i_f, f_f, i_b, f_b = jnp.split(g, 4, axis=-1)
    return q, k, v, (i_f, jax.nn.log_sigmoid(f_f)), (i_b, jax.nn.log_sigmoid(f_b))


def mlstm_zero_state(B):
    return (jnp.zeros((B, M_HEADS, M_DV, M_DQK), jnp.float32),
            jnp.zeros((B, M_HEADS, M_DQK), jnp.float32),
            jnp.zeros((B, M_HEADS), jnp.float32))


def mlstm_scan(q, k, v, i_pre, log_f, state):
    B, S = q.shape[:2]
    nc = S // M_CHUNK

    def to_chunks(a):
        return a.reshape(B, nc, M_CHUNK, *a.shape[2:]).swapaxes(0, 1)

    xs = tuple(to_chunks(a) for a in (q, k, v, i_pre, log_f))
    scan_order = jnp.tril(jnp.ones((M_CHUNK, M_CHUNK), dtype=bool))

    def step(carry, xc):
        C, n, m = carry
        qc, kc, vc, ic, fc = xc
        bT = jnp.cumsum(fc, axis=1).transpose(0, 2, 1)
        icT = ic.transpose(0, 2, 1)
        d = bT[:, :, :, None] - bT[:, :, None, :] + icT[:, :, None, :]
        d = jnp.where(scan_order, d, -jnp.inf)
        inter = bT + m[:, :, None]
        m_t = jnp.maximum(inter, jnp.max(d, axis=-1))
        w = jnp.exp(d - m_t[..., None])
        scale = jnp.exp(inter - m_t)
        s = jnp.einsum("blhk,bshk->bhls", qc, kc) * w
        num = (jnp.einsum("bhls,bshv->blhv", s, vc)
               + scale.transpose(0, 2, 1)[..., None] * jnp.einsum("bhvk,blhk->blhv", C, qc))
        den = jnp.sum(s, axis=-1) + scale * jnp.einsum("bhk,blhk->bhl", n, qc)
        h = num / jnp.maximum(jnp.abs(den), jnp.exp(-m_t)).transpose(0, 2, 1)[..., None]
        b_last = bT[:, :, -1]
        g = b_last[:, :, None] - bT + icT
        m_new = jnp.maximum(b_last + m, jnp.max(g, axis=-1))
        wk = jnp.exp(g - m_new[..., None])
        dec = jnp.exp(b_last + m - m_new)
        C_new = dec[..., None, None] * C + jnp.einsum("bhs,bshv,bshk->bhvk", wk, vc, kc)
        n_new = dec[..., None] * n + jnp.einsum("bhs,bshk->bhk", wk, kc)
        return (C_new, n_new, m_new), h

    state, hs = lax.scan(step, state, xs)
    return hs.swapaxes(0, 1).reshape(B, S, M_HEADS, M_DV), state


def mlstm_scan_reversed(q, k, v, i_pre, log_f, state):
    rev = lambda a: jnp.flip(a, axis=1)
    h, st = mlstm_scan(rev(q), rev(k), rev(v), rev(i_pre), rev(log_f), state)
    return rev(h), st


def mlstm_output(h_sum, zo, m_out_norm):
    B, S = h_sum.shape[:2]
    hn = h_sum * lax.rsqrt(jnp.mean(h_sum * h_sum, axis=-1, keepdims=True) + NORM_EPS)
    hn = hn.reshape(B, S, M_V_W).astype(zo.dtype) * m_out_norm
    return hn * jax.nn.sigmoid(zo)


def mla_q(zcq, q_norm, w_uq):
    B, S, _ = zcq.shape
    q = (rmsnorm(zcq, q_norm) @ w_uq).reshape(B, S, A_HEADS, A_NOPE + A_ROPE)
    return q[..., :A_NOPE], q[..., A_NOPE:]


def mla_kv(zckv, zkr, kv_norm, w_ukv):
    B, S, _ = zckv.shape
    kv = (rmsnorm(zckv, kv_norm) @ w_ukv).reshape(B, S, A_HEADS, A_NOPE + A_DV)
    return kv[..., :A_NOPE], zkr, kv[..., A_NOPE:]


def dense_attention(qn, qr, kn, kr, v):
    s = jnp.einsum("bqhd,bkhd->bhqk", qn, kn) + jnp.einsum("bqhr,bkr->bhqk", qr, kr)
    p = jax.nn.softmax(s.astype(jnp.float32) * A_SCALE, axis=-1).astype(v.dtype)
    return jnp.einsum("bhqk,bkhv->bqhv", p, v)


def latent_attention(qn, qr, kn, kr, v, ckn, ckr, cv):
    B, S = qn.shape[:2]
    kn_all = jnp.concatenate([ckn, kn], axis=1)
    kr_all = jnp.concatenate([ckr, kr], axis=1)
    v_all = jnp.concatenate([cv, v], axis=1)
    nb = S // Q_BLOCK

    def blocks(a):
        return a.reshape(B, nb, Q_BLOCK, *a.shape[2:]).swapaxes(0, 1)

    o = lax.map(lambda qs: dense_attention(qs[0], qs[1], kn_all, kr_all, v_all), (blocks(qn), blocks(qr)))
    return o.swapaxes(0, 1).reshape(B, S, A_V_W)


def merge_branches(ya, yb, zga, zgb, w_branch_a, w_branch_b, w_out):
    return (jax.nn.sigmoid(zga) * (ya @ w_branch_a) + jax.nn.sigmoid(zgb) * (yb @ w_branch_b)) @ w_out


def token_mixer(h, hc, cos, sin, w_in, b_gates, m_out_norm, q_norm, kv_norm, w_uq, w_ukv,
                w_branch_a, w_branch_b, w_out, with_ctx_out):
    B = h.shape[0]
    zq, zk, zv, zo, zg, zcq, zckv, zkr, zga, zgb = jnp.split(h @ w_in, IN_SPLIT_POINTS, axis=-1)
    czq, czk, czv, czo, czg, czcq, czckv, czkr, czga, czgb = jnp.split(hc @ w_in, IN_SPLIT_POINTS, axis=-1)

    q, k, v, gates_f, gates_b = mlstm_prep(zq, zk, zv, zg, b_gates)
    cq, ck, cv, cgates_f, cgates_b = mlstm_prep(czq, czk, czv, czg, b_gates)
    st0 = mlstm_zero_state(B)
    hc_f, st_f = mlstm_scan(cq, ck, cv, cgates_f[0], cgates_f[1], st0)
    hc_b, st_b = mlstm_scan_reversed(cq, ck, cv, cgates_b[0], cgates_b[1], st0)
    h_f, _ = mlstm_scan(q, k, v, gates_f[0], gates_f[1], st_f)
    h_b, _ = mlstm_scan_reversed(q, k, v, gates_b[0], gates_b[1], st_b)
    ya = mlstm_output(h_f + h_b, zo, m_out_norm)

    kn, kr, va = mla_kv(zckv, zkr, kv_norm, w_ukv)
    ckn, ckr, cva = mla_kv(czckv, czkr, kv_norm, w_ukv)
    qn, qr = mla_q(zcq, q_norm, w_uq)
    yb = latent_attention(qn, apply_rope(qr, cos, sin), kn, apply_rope(kr, cos, sin), va, ckn, ckr, cva)

    y = merge_branches(ya, yb, zga, zgb, w_branch_a, w_branch_b, w_out)
    if not with_ctx_out:
        return y, None
    ya_c = mlstm_output(hc_f + hc_b, czo, m_out_norm)
    cqn, cqr = mla_q(czcq, q_norm, w_uq)
    yb_c = dense_attention(cqn, cqr, ckn, ckr, cva).reshape(hc.shape[0], hc.shape[1], A_V_W)
    yc = merge_branches(ya_c, yb_c, czga, czgb, w_branch_a, w_branch_b, w_out)
    return y, yc


def moe_ffn(h, router_w, router_b, w_gu, b_gu, w_down, b_down):
    T = h.shape[0]
    logits = (h @ router_w + router_b).astype(jnp.float32)
    top_logit, top_idx = lax.top_k(logits, TOP_K)
    gates = jax.nn.softmax(top_logit, axis=-1)
    n_assign = T * TOP_K
    n_blocks = -(-n_assign // EXPERT_BLOCK) + N_EXPERTS
    n_slots = n_blocks * EXPERT_BLOCK
    flat_e = top_idx.reshape(-1)
    flat_tok = jnp.broadcast_to(jnp.arange(T, dtype=jnp.int32)[:, None], (T, TOP_K)).reshape(-1)
    flat_g = gates.reshape(-1)
    order = jnp.argsort(flat_e, stable=True)
    sorted_e = flat_e[order]
    counts = jnp.bincount(flat_e, length=N_EXPERTS)
    padded = (counts + EXPERT_BLOCK - 1) // EXPERT_BLOCK * EXPERT_BLOCK
    pad_end = jnp.cumsum(padded)
    pad_start = pad_end - padded
    start = jnp.cumsum(counts) - counts
    dest = pad_start[sorted_e] + jnp.arange(n_assign, dtype=jnp.int32) - start[sorted_e]
    slot_tok = jnp.zeros((n_slots,), jnp.int32).at[dest].set(flat_tok[order])
    slot_gate = jnp.zeros((n_slots,), jnp.float32).at[dest].set(flat_g[order])
    block_e = jnp.minimum(jnp.searchsorted(pad_end, jnp.arange(n_blocks, dtype=jnp.int32) * EXPERT_BLOCK, side="right"), N_EXPERTS - 1)

    def expert_block(args):
        toks, e = args
        gu = h[toks] @ w_gu[e] + b_gu[e]
        glu = jnp.minimum(gu[:, :D_EXPERT], SWIGLU_LIMIT)
        lin = jnp.clip(gu[:, D_EXPERT:], -SWIGLU_LIMIT, SWIGLU_LIMIT)
        act = glu * jax.nn.sigmoid(SWIGLU_ALPHA * glu) * (lin + 1)
        return act @ w_down[e] + b_down[e]

    ys = lax.map(expert_block, (slot_tok.reshape(n_blocks, EXPERT_BLOCK), block_e))
    ys = ys.reshape(n_slots, -1) * slot_gate[:, None].astype(h.dtype)
    return jnp.zeros_like(h).at[slot_tok].add(ys)


def trunk_layer(x, xc, c_silu, cc_silu, cos, sin, w_ada, b_ada, norm_pre_mix, norm_post_mix,
                norm_pre_ffn, norm_post_ffn, w_in, b_gates, m_out_norm, q_norm, kv_norm, w_uq, w_ukv,
                w_branch_a, w_branch_b, w_out, router_w, router_b, w_gu, b_gu, w_down, b_down, with_ctx_out):
    B, S, D = x.shape
    sh1, sc1, g1, sh2, sc2, g2 = jnp.split((c_silu @ w_ada + b_ada)[:, None, :], N_MOD, axis=-1)
    csh1, csc1, cg1, csh2, csc2, cg2 = jnp.split((cc_silu @ w_ada + b_ada)[None, None, :], N_MOD, axis=-1)
    h = rmsnorm(x, norm_pre_mix) * (1 + sc1) + sh1
    hc = rmsnorm(xc, norm_pre_mix) * (1 + csc1) + csh1
    y, yc = token_mixer(h, hc, cos, sin, w_in, b_gates, m_out_norm, q_norm, kv_norm, w_uq, w_ukv,
                        w_branch_a, w_branch_b, w_out, with_ctx_out)
    x = x + g1 * rmsnorm(y, norm_post_mix)
    h2 = rmsnorm(x, norm_pre_ffn) * (1 + sc2) + sh2
    f = moe_ffn(h2.reshape(B * S, D), router_w, router_b, w_gu, b_gu, w_down, b_down).reshape(B, S, D)
    x = x + g2 * rmsnorm(f, norm_post_ffn)
    if with_ctx_out:
        xc = xc + cg1 * rmsnorm(yc, norm_post_mix)
        h2c = rmsnorm(xc, norm_pre_ffn) * (1 + csc2) + csh2
        fc = moe_ffn(h2c.reshape(-1, D), router_w, router_b, w_gu, b_gu, w_down, b_down).reshape(xc.shape)
        xc = xc + cg2 * rmsnorm(fc, norm_post_ffn)
    return x, xc


def setup_inputs(seed: int = 0) -> dict:
    key = jax.random.key(seed)
    ks = jax.random.split(key, 26)
    f32 = jnp.float32
    L = DEPTH

    def nrm(k, shape, scale):
        return jax.random.normal(k, shape, f32) * scale

    def gain(k, shape):
        return 1.0 + nrm(k, shape, 0.02)

    gate_offset = jnp.concatenate([jnp.zeros((M_HEADS,), f32), jnp.full((M_HEADS,), FORGET_BIAS, f32)] * 2)
    return {
        "x": nrm(ks[0], (BATCH, SEQ, D_MODEL), 1.0),
        "c": nrm(ks[1], (BATCH, D_MODEL), 1.0),
        "ctx": nrm(ks[2], (BATCH, CTX_LEN, D_MODEL), 1.0),
        "c_ctx": nrm(ks[3], (D_MODEL,), 1.0),
        "w_ada": nrm(ks[4], (L, D_MODEL, N_MOD * D_MODEL), 0.5 * D_MODEL ** -0.5),
        "b_ada": nrm(ks[5], (L, N_MOD * D_MODEL), 0.02),
        "norm_pre_mix": gain(ks[6], (L, D_MODEL)),
        "norm_post_mix": gain(ks[7], (L, D_MODEL)),
        "norm_pre_ffn": gain(ks[8], (L, D_MODEL)),
        "norm_post_ffn": gain(ks[9], (L, D_MODEL)),
        "w_in": nrm(ks[10], (L, D_MODEL, D_IN), D_MODEL ** -0.5),
        "b_gates": gate_offset + nrm(ks[11], (L, N_GATE_COLS), 0.1),
        "m_out_norm": gain(ks[12], (L, M_V_W)),
        "q_norm": gain(ks[13], (L, Q_LORA)),
        "kv_norm": gain(ks[14], (L, KV_LORA)),
        "w_uq": nrm(ks[15], (L, Q_LORA, A_HEADS * (A_NOPE + A_ROPE)), Q_LORA ** -0.5),
        "w_ukv": nrm(ks[16], (L, KV_LORA, A_HEADS * (A_NOPE + A_DV)), KV_LORA ** -0.5),
        "w_branch_a": nrm(ks[17], (L, M_V_W, D_MODEL), M_V_W ** -0.5),
        "w_branch_b": nrm(ks[18], (L, A_V_W, D_MODEL), A_V_W ** -0.5),
        "w_out": nrm(ks[19], (L, D_MODEL, D_MODEL), D_MODEL ** -0.5),
        "router_w": nrm(ks[20], (L, D_MODEL, N_EXPERTS), D_MODEL ** -0.5),
        "router_b": nrm(ks[21], (L, N_EXPERTS), 0.01),
        "w_gu": nrm(ks[22], (L, N_EXPERTS, D_MODEL, 2 * D_EXPERT), D_MODEL ** -0.5),
        "b_gu": nrm(ks[23], (L, N_EXPERTS, 2 * D_EXPERT), 0.02),
        "w_down": nrm(ks[24], (L, N_EXPERTS, D_EXPERT, D_MODEL), D_EXPERT ** -0.5),
        "b_down": nrm(ks[25], (L, N_EXPERTS, D_MODEL), 0.02),
    }


def reference(x, c, ctx, c_ctx, w_ada, b_ada, norm_pre_mix, norm_post_mix, norm_pre_ffn, norm_post_ffn,
              w_in, b_gates, m_out_norm, q_norm, kv_norm, w_uq, w_ukv, w_branch_a, w_branch_b, w_out,
              router_w, router_b, w_gu, b_gu, w_down, b_down):
    ROWS = x.shape[1] // GRID_W
    cos, sin = axial_rope_tables(ROWS, x.dtype)
    c_silu = jax.nn.silu(c)
    cc_silu = jax.nn.silu(c_ctx)
    xc = ctx
    for l in range(DEPTH):
        x, xc = trunk_layer(x, xc, c_silu, cc_silu, cos, sin, w_ada[l], b_ada[l], norm_pre_mix[l], norm_post_mix[l],
                            norm_pre_ffn[l], norm_post_ffn[l], w_in[l], b_gates[l], m_out_norm[l], q_norm[l],
                            kv_norm[l], w_uq[l], w_ukv[l], w_branch_a[l], w_branch_b[l], w_out[l], router_w[l],
                            router_b[l], w_gu[l], b_gu[l], w_down[l], b_down[l], with_ctx_out=(l < DEPTH - 1))
    return x
```

```python
import numpy as np
from contextlib import ExitStack, contextmanager
import concourse.bass as bass
import concourse.mybir as mybir
from concourse.bass_utils import run_bass_kernel_spmd

F32 = mybir.dt.float32
BF16 = mybir.dt.bfloat16
I32 = mybir.dt.int32
ALU = mybir.AluOpType
AF = mybir.ActivationFunctionType
AX = mybir.AxisListType

D = 4096
NTOK = 2304
OWN0, OWN1 = 256, 1280
NOWN = 1024
EPS = 1e-6
NE = 32
FE = 1536
A_SCALE = 192 ** -0.5
CQ, CK, CV, CO, CG, CCQ, CCKV, CKR, CGA, CGB = 0, 2048, 4096, 8192, 12288, 12320, 13344, 13856, 13920, 18016
DIN = 22112


class Buf:
    __slots__ = ("name", "w", "r")

    def __init__(self, name=""):
        self.name = name
        self.w = None
        self.r = []


class Sched:
    DMAK = 6
    ROT = 28000

    def __init__(self, nc):
        self.nc = nc
        self.eng = {"pe": nc.tensor, "act": nc.scalar, "dve": nc.vector, "pool": nc.gpsimd, "sp": nc.sync}
        self.sem, self.cnt, self.nsem = {}, {}, 0
        for e in ("pe", "act", "dve", "pool"):
            self._fresh(e)
        self.q_issuer = {"sp": "sp", "pq": "pool"}
        self.qsem, self.qcnt, self.qi = {}, {}, {}
        for q in self.q_issuer:
            self.qsem[q] = [self._alloc(f"{q}{j}") for j in range(self.DMAK)]
            self.qcnt[q] = [0] * self.DMAK
            self.qi[q] = 0
        self.waited = {e: {} for e in self.eng}

    def _alloc(self, name):
        self.nsem += 1
        return self.nc.alloc_semaphore(name=f"s_{name}_{self.nsem}")

    def _fresh(self, e):
        self.sem[e] = self._alloc(e)
        self.cnt[e] = 0

    def _wait(self, ename, dep):
        sem, val, _ = dep
        w = self.waited[ename]
        key = id(sem)
        if w.get(key, (None, 0))[1] >= val:
            return
        w[key] = (sem, val)
        self.eng[ename].wait_ge(sem, val)

    def _deps(self, ename, reads, writes, is_dma):
        for b in reads:
            for d in b.w or ():
                self._wait(ename, d)
        for b in writes:
            for d in b.w or ():
                if is_dma or d[2] != ename:
                    self._wait(ename, d)
            for d in b.r:
                if is_dma or d[2] != ename:
                    self._wait(ename, d)

    def _update(self, tok, reads, writes):
        for b in reads:
            b.r.append(tok)
        for b in writes:
            if tok[2] == "dma":
                b.w = [d for d in (b.w or ()) if d[2] == "dma"][-(2 * self.DMAK - 1):] + [tok]
            else:
                b.w = [tok]
            b.r = []

    def run(self, ename, reads, writes, fn):
        if self.cnt[ename] >= self.ROT:
            self._fresh(ename)
        self._deps(ename, reads, writes, False)
        ins = fn()
        self.cnt[ename] += 1
        ins.then_inc(self.sem[ename], 1)
        tok = (self.sem[ename], self.cnt[ename], ename)
        self._update(tok, reads, writes)
        return tok

    def dma(self, q, out, in_, reads, writes):
        issuer = self.q_issuer[q]
        j = self.qi[q]
        self.qi[q] = (j + 1) % self.DMAK
        if self.qcnt[q][j] >= self.ROT:
            self._wait(issuer, (self.qsem[q][j], self.qcnt[q][j], "dma"))
            self.qsem[q][j] = self._alloc(f"{q}{j}")
            self.qcnt[q][j] = 0
        sem = self.qsem[q][j]
        if self.qcnt[q][j] > 0:
            self._wait(issuer, (sem, self.qcnt[q][j], "dma"))
        self._deps(issuer, reads, writes, True)
        eng = self.nc.sync if q == "sp" else self.nc.gpsimd
        eng.dma_start(out=out, in_=in_).then_inc(sem, 16)
        self.qcnt[q][j] += 16
        tok = (sem, self.qcnt[q][j], "dma")
        self._update(tok, reads, writes)
        return tok

    def barrier(self):
        toks = [(self.sem[e], self.cnt[e], e) for e in ("pe", "act", "dve", "pool") if self.cnt[e] > 0]
        for q in self.q_issuer:
            for j in range(self.DMAK):
                if self.qcnt[q][j] > 0:
                    toks.append((self.qsem[q][j], self.qcnt[q][j], "dma"))
        for e in self.eng:
            for t in toks:
                self._wait(e, t)

    def finish(self, bufs):
        for b in bufs:
            for d in b.w or ():
                self._wait("sp", d)


class Rot:
    def __init__(self, tiles, name):
        self.tiles = tiles
        self.bufs = [Buf(f"{name}{i}") for i in range(len(tiles))]
        self.i = 0

    def next(self):
        j = self.i % len(self.tiles)
        self.i += 1
        return self.tiles[j], self.bufs[j]


class Prog:
    def __init__(self, debug=()):
        self.nc = bass.Bass("TRN2", target_bir_lowering=False)
        self.S = Sched(self.nc)
        self.inputs = {}
        self.debug = set(debug)
        self.outs = []
        self.k = 0

    @contextmanager
    def scope(self):
        with ExitStack() as es:
            yield es
            self.S.barrier()

    def inp(self, name, shape, dtype=F32):
        if name not in self.inputs:
            self.inputs[name] = self.nc.dram_tensor(name, list(shape), dtype, kind="ExternalInput").ap()
        return self.inputs[name]

    def scratch(self, name, shape, dtype):
        if name in self.debug:
            self.outs.append(name)
            return self.nc.dram_tensor(name, list(shape), dtype, kind="ExternalOutput").ap(), Buf(name)
        return self.nc.dram_tensor(name, list(shape), dtype).ap(), Buf(name)

    def sb(self, es, name, shape, dtype=F32):
        return es.enter_context(self.nc.sbuf_tensor(name, list(shape), dtype))

    def ps(self, es, name, shape, dtype=F32):
        return es.enter_context(self.nc.psum_tensor(name, list(shape), dtype))

    def rot_sb(self, es, name, shape, dtype, n):
        return Rot([self.sb(es, f"{name}{i}", shape, dtype) for i in range(n)], name)

    def rot_ps(self, es, name, shape, dtype, n):
        return Rot([self.ps(es, f"{name}{i}", shape, dtype) for i in range(n)], name)

    def evac(self, reads, writes, out, in_):
        nc = self.nc
        self.k += 1
        if self.k % 2 == 0:
            return self.S.run("act", reads, writes, lambda: nc.scalar.copy(out=out, in_=in_))
        return self.S.run("dve", reads, writes, lambda: nc.vector.tensor_copy(out=out, in_=in_))

    def consts(self, es):
        nc, S = self.nc, self.S
        self.idf = self.sb(es, "idf", [128, 128], F32)
        self.idb = self.sb(es, "idb", [128, 128], BF16)
        self.b_id = Buf("ident")
        self.onesb = self.sb(es, "onesb", [128, 128], BF16)
        self.onesf = self.sb(es, "onesf", [128, 128], F32)

        def mk():
            nc.gpsimd.memset(self.onesb[:], 1.0)
            nc.gpsimd.memset(self.onesf[:], 1.0)
            return nc.gpsimd.memset(self.idf[:], 1.0)
        S.run("pool", [], [self.b_id], mk)
        S.run("pool", [self.b_id], [self.b_id], lambda: nc.gpsimd.affine_select(
            out=self.idf[:], in_=self.idf[:], pattern=[[-1, 128]], compare_op=ALU.is_equal, fill=0.0, base=0, channel_multiplier=1))
        S.run("dve", [self.b_id], [self.b_id], lambda: nc.vector.tensor_copy(out=self.idb[:], in_=self.idf[:]))

    def mm_phase(self, name, act, actbuf, T, K, jobs, TG=1024, act_pre=None):
        nc, S = self.nc, self.S
        KT = K // 128
        with self.scope() as es:
            ngroups = (T + TG - 1) // TG
            if act_pre is None:
                actv = act.rearrange("(kt p) t -> p kt t", p=128)
                a_rot = self.rot_sb(es, f"{name}_a", [128, KT, TG], BF16, 2 if ngroups > 1 else 1)
            NBmax = max(j.get("NB", 512) for j in jobs)
            w_rot = self.rot_sb(es, f"{name}_w", [128, KT, NBmax], BF16, 2)
            p_rot = self.rot_ps(es, f"{name}_ps", [128, 512], F32, 4)
            for g0 in range(0, T, TG):
                tg = min(TG, T - g0)
                if act_pre is None:
                    asb, ab = a_rot.next()
                    S.dma("sp", asb[:, :, :tg], actv[:, :, g0:g0 + tg], [actbuf] if actbuf else [], [ab])
                else:
                    asb, ab = act_pre
                for job in jobs:
                    N, mode, epi, NB = job["N"], job["mode"], job["epi"], job.get("NB", 512)
                    wv = job["w"].rearrange("(kt p) n -> p kt n", p=128)
                    for n0 in range(0, N, NB):
                        nb = min(NB, N - n0)
                        wsb, wb = w_rot.next()
                        S.dma("pq", wsb[:, :, :nb], wv[:, :, n0:n0 + nb], [], [wb])
                        if mode == "tm":
                            for t0 in range(0, tg, 128):
                                ts = min(128, tg - t0)
                                p, pb = p_rot.next()

                                def mm(p=p, t0=t0, ts=ts, wsb=wsb, nb=nb):
                                    for kt in range(KT):
                                        ins = nc.tensor.matmul(p[:ts, :nb], lhsT=asb[:, kt, t0:t0 + ts],
                                                               rhs=wsb[:, kt, :nb], start=(kt == 0), stop=(kt == KT - 1))
                                    return ins
                                S.run("pe", [ab, wb], [pb], mm)
                                epi(p[:ts, :nb], g0 + t0, n0, ts, nb, pb)
                        else:
                            for c0 in range(0, nb, 128):
                                cs = min(128, nb - c0)
                                for t0 in range(0, tg, 512):
                                    ts = min(512, tg - t0)
                                    p, pb = p_rot.next()

                                    def mm(p=p, c0=c0, cs=cs, t0=t0, ts=ts, wsb=wsb):
                                        for kt in range(KT):
                                            ins = nc.tensor.matmul(p[:cs, :ts], lhsT=wsb[:, kt, c0:c0 + cs],
                                                                   rhs=asb[:, kt, t0:t0 + ts],
                                                                   start=(kt == 0), stop=(kt == KT - 1))
                                        return ins
                                    S.run("pe", [ab, wb], [pb], mm)
                                    epi(p[:cs, :ts], n0 + c0, g0 + t0, cs, ts, pb)

    def store_epi(self, st, dst, dbuf, dtype, row_off=0, col_off=0, func=None, scale=None, bias=None):
        nc, S = self.nc, self.S

        def epi(p, r0, c0, rs, cs, pb):
            t, tb = st.next()
            o = t[:rs, :cs]
            if func is not None or bias is not None:
                kw = {}
                if bias is not None:
                    kw["bias"] = bias(r0, rs)
                S.run("act", [pb] + ([self.b_bias] if bias is not None else []), [tb],
                      lambda: nc.scalar.activation(out=o, in_=p, func=func or AF.Identity,
                                                   scale=1.0 if scale is None else scale, **kw))
            elif scale is not None:
                self.k += 1
                if self.k % 2 == 0:
                    S.run("act", [pb], [tb], lambda: nc.scalar.mul(out=o, in_=p, mul=scale))
                else:
                    S.run("dve", [pb], [tb], lambda: nc.vector.tensor_scalar(out=o, in0=p, scalar1=scale, scalar2=None, op0=ALU.mult))
            else:
                self.evac([pb], [tb], o, p)
            S.dma("sp", dst[row_off + r0:row_off + r0 + rs, col_off + c0:col_off + c0 + cs], o, [tb], [dbuf])
        return epi

    def norm_T(self, name, src, sbuf_src, T, F, gain_of_tile, shift_of_tile, dst, dbuf, gbufs, src_row0=0):
        nc, S = self.nc, self.S
        FT = F // 128
        with self.scope() as es:
            x_rot = self.rot_sb(es, f"{name}_x", [128, F], F32, 2)
            junk = self.sb(es, f"{name}_junk", [128, F], BF16)
            b_junk = Buf("junk")
            xn_rot = self.rot_sb(es, f"{name}_xn", [128, F], BF16, 2)
            o_rot = self.rot_sb(es, f"{name}_o", [128, FT, 128], BF16, 2)
            st_rot = self.rot_sb(es, f"{name}_st", [128, 2], F32, 2)
            p_rot = self.rot_ps(es, f"{name}_pt", [128, 8, 128], BF16, 3)
            dstv = dst.rearrange("(kt p) t -> p kt t", p=128)
            for ti, t0 in enumerate(range(0, T, 128)):
                ts = min(128, T - t0)
                x, xb = x_rot.next()
                S.dma("sp", x[:ts, :], src[src_row0 + t0:src_row0 + t0 + ts, :], [sbuf_src] if sbuf_src else [], [xb])
                st, stb = st_rot.next()
                S.run("dve", [], [stb], lambda: nc.vector.memset(st[:, :], 0.0))
                S.run("act", [xb], [b_junk, stb], lambda: nc.scalar.activation(
                    out=junk[:ts, :], in_=x[:ts, :], func=AF.Square, accum_out=st[:ts, 0:1]))
                S.run("act", [stb], [stb], lambda: nc.scalar.activation(
                    out=st[:ts, 1:2], in_=st[:ts, 0:1], func=AF.Sqrt, bias=EPS, scale=1.0 / F))
                S.run("dve", [stb], [stb], lambda: nc.vector.reciprocal(out=st[:ts, 1:2], in_=st[:ts, 1:2]))
                xn, xnb = xn_rot.next()
                g = gain_of_tile(ti)
                sh = shift_of_tile(ti) if shift_of_tile else None
                if sh is None:
                    S.run("dve", [xb, stb] + gbufs, [xnb], lambda: nc.vector.scalar_tensor_tensor(
                        out=xn[:ts, :], in0=x[:ts, :], scalar=st[:ts, 1:2], in1=g[:ts, :], op0=ALU.mult, op1=ALU.mult))
                else:
                    S.run("dve", [xb, stb] + gbufs, [xb], lambda: nc.vector.scalar_tensor_tensor(
                        out=x[:ts, :], in0=x[:ts, :], scalar=st[:ts, 1:2], in1=g[:ts, :], op0=ALU.mult, op1=ALU.mult))
                    S.run("dve", [xb] + gbufs, [xnb], lambda: nc.vector.tensor_tensor(
                        out=xn[:ts, :], in0=x[:ts, :], in1=sh[:ts, :], op=ALU.add))
                o, ob = o_rot.next()
                for g0 in range(0, FT, 8):
                    gn = min(8, FT - g0)
                    p, pb = p_rot.next()

                    def tr(p=p, g0=g0, gn=gn):
                        for j in range(gn):
                            ins = nc.tensor.transpose(out=p[:, j, :ts], in_=xn[:ts, (g0 + j) * 128:(g0 + j + 1) * 128],
                                                      identity=self.idb[:ts, :ts])
                        return ins
                    S.run("pe", [xnb, self.b_id], [pb], tr)
                    self.evac([pb], [ob], o[:, g0:g0 + gn, :ts], p[:, :gn, :ts])
                S.dma("sp", dstv[:, :, t0:t0 + ts], o[:, :, :ts], [ob], [dbuf])

    def transpose_pass(self, name, src, sbuf_src, T, F, dst, dbuf, scale_col=None, scale_buf=None):
        nc, S = self.nc, self.S
        FT = F // 128
        with self.scope() as es:
            x_rot = self.rot_sb(es, f"{name}_x", [128, F], BF16, 2)
            o_rot = self.rot_sb(es, f"{name}_o", [128, FT, 128], BF16, 2)
            p_rot = self.rot_ps(es, f"{name}_pt", [128, 8, 128], BF16, 3)
            dstv = dst.rearrange("(kt p) t -> p kt t", p=128)
            for t0 in range(0, T, 128):
                x, xb = x_rot.next()
                S.dma("sp", x[:, :], src[t0:t0 + 128, :], [sbuf_src], [xb])
                o, ob = o_rot.next()
                for g0 in range(0, FT, 8):
                    p, pb = p_rot.next()

                    def tr(p=p, g0=g0):
                        for j in range(8):
                            ins = nc.tensor.transpose(out=p[:, j, :], in_=x[:, (g0 + j) * 128:(g0 + j + 1) * 128],
                                                      identity=self.idb[:, :])
                        return ins
                    S.run("pe", [xb, self.b_id], [pb], tr)
                    if scale_col is None:
                        self.evac([pb], [ob], o[:, g0:g0 + 8, :], p[:, :, :])
                    else:
                        S.run("dve", [pb, scale_buf], [ob], lambda p=p, g0=g0: nc.vector.tensor_tensor(
                            out=o[:, g0:g0 + 8, :], in0=p[:, :, :],
                            in1=scale_col[:, g0:g0 + 8].unsqueeze(2).to_broadcast([128, 8, 128]), op=ALU.mult))
                S.dma("sp", dstv[:, :, t0:t0 + 128], o[:, :, :], [ob], [dbuf])

    def bload(self, t, src_row, bufs_r, buf_w, np_=128):
        self.S.dma("sp", t, src_row.partition_broadcast(np_), bufs_r, [buf_w])

    def phase_mod(self):
        nc, S = self.nc, self.S
        cvec = self.inp("cvec", [2, D])
        w_ada = self.inp("w_ada", [D, 6 * D])
        b_ada = self.inp("b_ada", [1, 6 * D])
        self.modv, self.b_modv = self.scratch("modv", [2, 6 * D], F32)
        with self.scope() as es:
            c32 = self.sb(es, "A_c32", [32, 2, 128], F32)
            b_c = Buf("c32")
            S.dma("sp", c32[:], cvec.rearrange("r (kt p) -> kt r p", p=128), [], [b_c])
            S.run("act", [b_c], [b_c], lambda: nc.scalar.activation(out=c32[:], in_=c32[:], func=AF.Silu))
            cT = self.sb(es, "A_cT", [128, 32, 2], BF16)
            b_cT = Buf("cT")
            with self.scope() as es2:
                pt = self.ps(es2, "A_pt", [128, 2, 32], F32)
                b_pt = Buf("pt")

                def tr():
                    for r in range(2):
                        ins = nc.tensor.transpose(out=pt[:, r, :], in_=c32[:, r, :], identity=self.idf[:32, :32])
                    return ins
                S.run("pe", [b_c, self.b_id], [b_pt], tr)
                S.run("dve", [b_pt], [b_cT], lambda: nc.vector.tensor_copy(
                    out=cT[:].rearrange("p k r -> p r k"), in_=pt[:]))
            bias = self.sb(es, "A_bias", [2, 6 * D], F32)
            b_bias = Buf("bias")
            self.bload(bias[:], b_ada[0:1, :], [], b_bias, 2)
            st = self.rot_sb(es, "A_st", [2, 512], F32, 3)

            def epi(p, r0, c0, rs, cs, pb):
                t, tb = st.next()
                S.run("dve", [pb, b_bias], [tb], lambda: nc.vector.tensor_tensor(
                    out=t[:rs, :cs], in0=p, in1=bias[:rs, c0:c0 + cs], op=ALU.add))
                S.dma("sp", self.modv[0:rs, c0:c0 + cs], t[:rs, :cs], [tb], [self.b_modv])
            self.mm_phase("A", None, None, 2, D, [dict(w=w_ada, N=6 * D, mode="tm", epi=epi)], act_pre=(cT, b_cT))

    def mod_tile(self, es, name, row, chunk, normvec=None, plus1=False):
        nc, S = self.nc, self.S
        t = self.sb(es, name, [128, D], F32)
        b = Buf(name)
        self.bload(t[:], self.modv[row:row + 1, chunk * D:(chunk + 1) * D], [self.b_modv], b)
        if normvec is not None:
            nv, bn = self.nv, self.b_nv
            self.bload(nv[:], normvec[0:1, :], [], bn)
            if plus1:
                S.run("dve", [b, bn], [b], lambda: nc.vector.scalar_tensor_tensor(
                    out=t[:], in0=t[:], scalar=1.0, in1=nv[:], op0=ALU.add, op1=ALU.mult))
            else:
                S.run("dve", [b, bn], [b], lambda: nc.vector.tensor_tensor(out=t[:], in0=t[:], in1=nv[:], op=ALU.mult))
        return t, b

    def phase_h(self):
        xs = self.inp("xs", [NTOK, D])
        n1 = self.inp("norm_pre_mix", [1, D])
        self.hT, self.b_hT = self.scratch("hT", [D, NTOK], BF16)
        with self.scope() as es:
            self.nv, self.b_nv = self.sb(es, "B_nv", [128, D], F32), Buf("nv")
            W1, bW1 = self.mod_tile(es, "B_W1", 0, 1, n1, True)
            SH1, bS1 = self.mod_tile(es, "B_SH1", 0, 0)
            W1c, bW1c = self.mod_tile(es, "B_W1c", 1, 1, n1, True)
            SH1c, bS1c = self.mod_tile(es, "B_SH1c", 1, 0)
            self.norm_T("B", xs, None, NTOK, D, lambda ti: W1c if ti < 2 else W1,
                        lambda ti: SH1c if ti < 2 else SH1, self.hT, self.b_hT, [bW1, bS1, bW1c, bS1c])

    def phase_proj(self):
        nc, S = self.nc, self.S
        w_in = self.inp("w_in", [D, DIN])
        w_krs = self.inp("w_krs", [D, 64])
        b_g = self.inp("b_gates", [1, 32])
        sc = self.scratch
        self.qT, self.b_qT = sc("qT", [2048, NOWN], BF16)
        self.kT, self.b_kT = sc("kT", [2048, NTOK], BF16)
        self.k_tm, self.b_k_tm = sc("k_tm", [NTOK, 2048], BF16)
        self.v_tm, self.b_v_tm = sc("v_tm", [NTOK, 4096], BF16)
        self.so, self.b_so = sc("so", [NOWN, 4096], BF16)
        self.zcq, self.b_zcq = sc("zcq", [NOWN, 1024], F32)
        self.sgaT, self.b_sgaT = sc("sgaT", [D, NOWN], BF16)
        self.sgbT, self.b_sgbT = sc("sgbT", [D, NOWN], BF16)
        self.zgT, self.b_zgT = sc("zgT", [32, NTOK], F32)
        self.zckv, self.b_zckv = sc("zckv", [NTOK, 512], F32)
        self.zkrT, self.b_zkrT = sc("zkrT", [64, NTOK], F32)
        self.zkrsT, self.b_zkrsT = sc("zkrsT", [64, NTOK], F32)
        with self.scope() as es:
            stb = self.rot_sb(es, "C_stb", [128, 512], BF16, 3)
            stf = self.rot_sb(es, "C_stf", [128, 512], F32, 3)
            bg = self.sb(es, "C_bg", [32, 1], F32)
            self.b_bias = Buf("bg")
            S.dma("sp", bg[:], b_g.rearrange("o g -> g o"), [], [self.b_bias])
            E = self.store_epi

            def all_jobs(tok_off):
                return [
                    dict(w=w_in[:, CK:CK + 2048], N=2048, mode="fm", epi=E(stb, self.kT, self.b_kT, BF16, col_off=tok_off, scale=0.0625)),
                    dict(w=w_in[:, CK:CK + 2048], N=2048, mode="tm", epi=E(stb, self.k_tm, self.b_k_tm, BF16, row_off=tok_off, scale=0.0625)),
                    dict(w=w_in[:, CV:CV + 4096], N=4096, mode="tm", epi=E(stb, self.v_tm, self.b_v_tm, BF16, row_off=tok_off)),
                    dict(w=w_in[:, CG:CG + 32], N=32, mode="fm", epi=E(stf, self.zgT, self.b_zgT, F32, col_off=tok_off,
                                                                     bias=lambda r0, rs: bg[r0:r0 + rs, 0:1])),
                    dict(w=w_in[:, CCKV:CCKV + 512], N=512, mode="tm", epi=E(stf, self.zckv, self.b_zckv, F32, row_off=tok_off)),
                    dict(w=w_in[:, CKR:CKR + 64], N=64, mode="fm", epi=E(stf, self.zkrT, self.b_zkrT, F32, col_off=tok_off)),
                    dict(w=w_krs, N=64, mode="fm", epi=E(stf, self.zkrsT, self.b_zkrsT, F32, col_off=tok_off)),
                ]
            own_jobs = [
                dict(w=w_in[:, CQ:CQ + 2048], N=2048, mode="fm", epi=E(stb, self.qT, self.b_qT, BF16)),
                dict(w=w_in[:, CO:CO + 4096], N=4096, mode="tm", epi=E(stb, self.so, self.b_so, BF16, func=AF.Sigmoid)),
                dict(w=w_in[:, CCQ:CCQ + 1024], N=1024, mode="tm", epi=E(stf, self.zcq, self.b_zcq, F32)),
                dict(w=w_in[:, CGA:CGA + 4096], N=4096, mode="fm", epi=E(stb, self.sgaT, self.b_sgaT, BF16, func=AF.Sigmoid)),
                dict(w=w_in[:, CGB:CGB + 4096], N=4096, mode="fm", epi=E(stb, self.sgbT, self.b_sgbT, BF16, func=AF.Sigmoid)),
            ]
            self.mm_phase("C0", self.hT[:, 0:OWN0], self.b_hT, OWN0, D, all_jobs(0))
            self.mm_phase("C1", self.hT[:, OWN0:OWN1], self.b_hT, NOWN, D, all_jobs(OWN0) + own_jobs)
            self.mm_phase("C2", self.hT[:, OWN1:NTOK], self.b_hT, NTOK - OWN1, D, all_jobs(OWN1))


DEINT = list(range(0, 64, 2)) + list(range(1, 64, 2))
SWAPI = list(range(1, 64, 2)) + list(range(0, 64, 2))
_cache = {}


def core_inputs(inputs, c, names):
    b, half = c // 2, c % 2
    out = {}
    f32 = np.float32

    def shared(key, fn):
        if key not in _cache:
            _cache[key] = np.ascontiguousarray(fn(), dtype=f32)
        return _cache[key]

    for n in names:
        if n == "xs":
            xc, xx = inputs["ctx"][b], inputs["x"][b]
            if half:
                xc, xx = xc[::-1], xx[::-1]
            out[n] = np.ascontiguousarray(np.concatenate([xc, xx], 0), dtype=f32)
        elif n == "cvec":
            out[n] = np.ascontiguousarray(np.stack([inputs["c"][b], inputs["c_ctx"]], 0), dtype=f32)
        elif n == "posinfo":
            out[n] = np.array([[2047.0, -1.0]] if half else [[0.0, 1.0]], f32)
        elif n == "w_in":
            def mk(half=half):
                w = np.array(inputs["w_in"][0], dtype=f32)
                if half:
                    w[:, CG:CG + 32] = np.concatenate([w[:, CG + 16:CG + 32], w[:, CG:CG + 16]], 1)
                w[:, CKR:CKR + 64] = w[:, CKR:CKR + 64][:, DEINT]
                return w
            out[n] = shared(("w_in", half), mk)
        elif n == "w_krs":
            out[n] = shared("w_krs", lambda: inputs["w_in"][0][:, CKR:CKR + 64][:, SWAPI])
        elif n == "b_gates":
            g = inputs["b_gates"][0]
            if half:
                g = np.concatenate([g[16:32], g[0:16]])
            out[n] = np.ascontiguousarray(g[None, :], dtype=f32)
        elif n in ("w_uqn", "w_uqr", "w_uqs"):
            w3 = inputs["w_uq"][0].reshape(1024, 32, 192)
            if n == "w_uqn":
                out[n] = shared(n, lambda: w3[:, :, :128].reshape(1024, 4096))
            else:
                idx = DEINT if n == "w_uqr" else SWAPI
                out[n] = shared(n, lambda: w3[:, :, 128:][:, :, idx].reshape(1024, 2048))
        elif n in ("w_ukn", "w_ukvv"):
            w3 = inputs["w_ukv"][0].reshape(512, 32, 256)
            out[n] = shared(n, lambda: (w3[:, :, :128] if n == "w_ukn" else w3[:, :, 128:]).reshape(512, 4096))
        elif n == "w_uq":
            def mk():
                w = np.array(inputs["w_uq"][0], dtype=f32).reshape(1024, 32, 192)
                w[:, :, 128:] = w[:, :, 128:][:, :, DEINT]
                return w.reshape(1024, 6144)
            out[n] = shared("w_uq", mk)
        elif n == "w_uqs":
            out[n] = shared("w_uqs", lambda: inputs["w_uq"][0].reshape(1024, 32, 192)[:, :, 128:][:, :, SWAPI].reshape(1024, 2048))
        elif n == "w_gu":
            def mk():
                w = inputs["w_gu"][0].reshape(NE, D, 2, 12, 128)
                return np.ascontiguousarray(w.transpose(0, 1, 3, 2, 4)).reshape(NE, D, 3072)
            out[n] = shared("w_gu", mk)
        elif n == "b_gu":
            out[n] = shared("b_gu", lambda: inputs["b_gu"][0].reshape(NE, 2, 12, 128).transpose(0, 2, 1, 3).reshape(NE, 3072))
        elif n in ("w_down", "b_down"):
            out[n] = shared(n, lambda n=n: inputs[n][0])
        elif n in ("b_ada", "norm_pre_mix", "norm_post_mix", "norm_pre_ffn", "norm_post_ffn", "m_out_norm",
                   "q_norm", "kv_norm", "router_b"):
            out[n] = shared(n, lambda n=n: np.asarray(inputs[n]).reshape(1, -1))
        else:
            out[n] = shared(n, lambda n=n: inputs[n][0])
    return out


def _scan(P, X, Y, bx, by, pr, lo, hi, desc, op):
    nc, S = P.nc, P.S
    n = hi - lo
    s = 1
    src, dst, bs, bd = X, Y, bx, by
    while s < n:
        if not desc:
            a_out, a_in0, a_in1 = (lo + s, hi), (lo + s, hi), (lo, hi - s)
            c_rng = (lo, lo + s)
        else:
            a_out, a_in0, a_in1 = (lo, hi - s), (lo, hi - s), (lo + s, hi)
            c_rng = (hi - s, hi)
        S.run("dve", [bs], [bd], lambda src=src, dst=dst: nc.vector.tensor_tensor(
            out=dst[pr, a_out[0]:a_out[1]], in0=src[pr, a_in0[0]:a_in0[1]], in1=src[pr, a_in1[0]:a_in1[1]], op=op))
        S.run("act", [bs], [bd], lambda src=src, dst=dst: nc.scalar.copy(
            out=dst[pr, c_rng[0]:c_rng[1]], in_=src[pr, c_rng[0]:c_rng[1]]))
        src, dst, bs, bd = dst, src, bd, bs
        s *= 2
    if src is not X:
        S.run("dve", [by], [bx], lambda: nc.vector.tensor_copy(out=X[pr, lo:hi], in_=Y[pr, lo:hi]))


def phase_mlstm(P, d1_only=False):
    nc, S = P.nc, P.S
    R = 40
    P1, P2 = slice(0, 8), slice(32, 40)
    NCH = NTOK // 64
    P.h1, P.b_h1 = P.scratch("h1", [NOWN, 4096], F32)
    P.ya_tm, P.b_ya_tm = P.scratch("ya_tm", [NOWN, 4096], BF16)
    with P.scope() as es:
        sb = lambda n, s, d=F32: P.sb(es, "M_" + n, s, d)
        A = sb("A", [R, NTOK]); NEGMU = sb("NEGMU", [R, NTOK]); SCL = sb("SCL", [R, NTOK])
        WKtm = sb("WKtm", [64, NCH, R]); EMTtm = sb("EMTtm", [64, NCH, R]); DEC = sb("DEC", [R, NCH])
        MASKD = sb("MASKD", [R, 8, 64]); NM = [sb("NM1", [64, 8, 64]), sb("NM2", [64, 8, 64])]
        bA, bNEGMU, bSCL, bWK, bEMT, bDEC, bMASKD, bNM = (Buf(n) for n in "A NEGMU SCL WK EMT DEC MASKD NM".split())
        with P.scope() as es2:
            t2 = lambda n, s, d=F32: P.sb(es2, "G_" + n, s, d)
            I = t2("I", [R, NTOK]); X = t2("X", [R, NTOK]); Y = t2("Y", [R, NTOK]); Bt = t2("B", [R, NTOK])
            MU = t2("MU", [R, NTOK]); T3 = t2("T3", [R, NTOK]); ME = t2("ME", [R, NCH]); MP = t2("MP", [R, NCH])
            bI, bX, bY, bB, bMU, bT3, bME, bMP = (Buf(n) for n in "I X Y B MU T3 ME MP".split())

            def z():
                for t in (I, X, Y, A, NEGMU, SCL, MU, T3, MASKD, NM[0]):
                    nc.gpsimd.memset(t[:], 0.0)
                return nc.gpsimd.memset(NM[1][:], 0.0)
            S.run("pool", [], [bI, bX, bY, bA, bNEGMU, bSCL, bMU, bT3, bMASKD, bNM], z)
            S.run("pool", [bNM], [bNM], lambda: nc.gpsimd.affine_select(
                out=NM[0][:], in_=NM[0][:], pattern=[[0, 8], [1, 64]], compare_op=ALU.is_ge, fill=-30000.0, base=0, channel_multiplier=-1))
            S.run("pool", [bNM], [bNM], lambda: nc.gpsimd.affine_select(
                out=NM[1][:], in_=NM[1][:], pattern=[[0, 8], [-1, 64]], compare_op=ALU.is_ge, fill=-30000.0, base=0, channel_multiplier=1))
            for pr, off in ((P1, 0), (P2, 32)):
                S.run("dve", [P.b_id, bMASKD], [bMASKD], lambda pr=pr, off=off: nc.vector.tensor_copy(
                    out=MASKD[pr], in_=P.idf[pr, off:off + 8].unsqueeze(2).to_broadcast([8, 8, 64])))
            S.dma("sp", I[P1, :], P.zgT[0:8, :], [P.b_zgT, bI], [bI])
            S.dma("sp", X[P1, :], P.zgT[8:16, :], [P.b_zgT, bX], [bX])
            S.dma("sp", I[P2, :], P.zgT[16:24, :], [P.b_zgT, bI], [bI])
            S.dma("sp", X[P2, :], P.zgT[24:32, :], [P.b_zgT, bX], [bX])
            S.run("act", [bX], [bX], lambda: nc.scalar.activation(out=X[:], in_=X[:], func=AF.Exp, scale=-1.0))
            S.run("act", [bX], [bX], lambda: nc.scalar.activation(out=X[:], in_=X[:], func=AF.Ln, bias=1.0, scale=1.0))
            S.run("dve", [bX], [bX], lambda: nc.vector.tensor_scalar(out=X[:], in0=X[:], scalar1=-1.0, scalar2=None, op0=ALU.mult))
            _scan(P, X, Y, bX, bY, P1, 0, OWN1, False, ALU.add)
            _scan(P, X, Y, bX, bY, P2, 0, OWN0, True, ALU.add)
            _scan(P, X, Y, bX, bY, P2, OWN0, NTOK, True, ALU.add)
            S.run("dve", [bX], [bX], lambda: nc.vector.tensor_scalar(
                out=X[P2, OWN0:NTOK], in0=X[P2, OWN0:NTOK], scalar1=X[P2, 0:1], scalar2=None, op0=ALU.add))
            S.run("dve", [bX], [bB], lambda: nc.vector.tensor_copy(out=Bt[:], in_=X[:]))
            S.run("dve", [bI, bB], [bA], lambda: nc.vector.tensor_tensor(out=A[:], in0=I[:], in1=Bt[:], op=ALU.subtract))
            S.run("dve", [bA], [bX], lambda: nc.vector.tensor_copy(out=X[:], in_=A[:]))
            _scan(P, X, Y, bX, bY, P1, 0, OWN1, False, ALU.max)
            _scan(P, X, Y, bX, bY, P2, 0, OWN0, True, ALU.max)
            _scan(P, X, Y, bX, bY, P2, OWN0, NTOK, True, ALU.max)
            S.run("dve", [bX], [bX], lambda: nc.vector.tensor_scalar(out=X[:], in0=X[:], scalar1=0.0, scalar2=None, op0=ALU.max))
            S.run("dve", [bX], [bX], lambda: nc.vector.tensor_scalar(
                out=X[P2, OWN0:NTOK], in0=X[P2, OWN0:NTOK], scalar1=X[P2, 0:1], scalar2=None, op0=ALU.max))
            S.run("dve", [bX], [bMU], lambda: nc.vector.tensor_copy(out=MU[:], in_=X[:]))
            S.run("dve", [bMU], [bNEGMU], lambda: nc.vector.tensor_scalar(out=NEGMU[:], in0=MU[:], scalar1=-1.0, scalar2=None, op0=ALU.mult))
            MUv = MU[:].rearrange("p (c j) -> p c j", j=64)
            S.run("dve", [bMU], [bME], lambda: nc.vector.tensor_copy(out=ME[:], in_=MUv[:, :, 0]))
            S.run("dve", [bMU, bME], [bME], lambda: nc.vector.tensor_copy(out=ME[P1, :], in_=MUv[P1, :, 63]))

            def mp():
                nc.vector.memset(MP[:], 0.0)
                nc.vector.tensor_copy(out=MP[P1, 1:NCH], in_=ME[P1, 0:NCH - 1])
                nc.vector.tensor_copy(out=MP[P2, 0:NCH - 1], in_=ME[P2, 1:NCH])
                nc.vector.memset(MP[P2, 3:4], 0.0)
                return nc.vector.tensor_copy(out=MP[P2, NCH - 1:NCH], in_=ME[P2, 0:1])
            S.run("dve", [bME], [bMP], mp)
            S.run("dve", [bMP, bMU], [bT3], lambda: nc.vector.tensor_tensor(
                out=T3[:].rearrange("p (c j) -> p c j", j=64), in0=MP[:].unsqueeze(2).to_broadcast([R, NCH, 64]), in1=MUv, op=ALU.subtract))
            S.run("act", [bT3], [bSCL], lambda: nc.scalar.activation(out=SCL[:], in_=T3[:], func=AF.Exp))
            S.run("dve", [bMP, bME], [bDEC], lambda: nc.vector.tensor_tensor(out=DEC[:], in0=MP[:], in1=ME[:], op=ALU.subtract))
            S.run("act", [bDEC], [bDEC], lambda: nc.scalar.activation(out=DEC[:], in_=DEC[:], func=AF.Exp))
            S.run("dve", [bA, bME], [bX], lambda: nc.vector.tensor_tensor(
                out=X[:].rearrange("p (c j) -> p c j", j=64), in0=A[:].rearrange("p (c j) -> p c j", j=64),
                in1=ME[:].unsqueeze(2).to_broadcast([R, NCH, 64]), op=ALU.subtract))
            S.run("act", [bX], [bX], lambda: nc.scalar.activation(out=X[:], in_=X[:], func=AF.Exp))
            S.run("dve", [bB, bMU], [bY], lambda: nc.vector.tensor_tensor(out=Y[:], in0=Bt[:], in1=MU[:], op=ALU.add))
            S.run("act", [bY], [bY], lambda: nc.scalar.activation(out=Y[:], in_=Y[:], func=AF.Exp, scale=-1.0))
            pt_rot = P.rot_ps(es2, "G_pt", [64, 12, R], F32, 2)
            for src, bsrc, dst, bdst in ((X, bX, WKtm, bWK), (Y, bY, EMTtm, bEMT)):
                for c0 in range(0, NCH, 12):
                    pt, bpt = pt_rot.next()

                    def tr(pt=pt, c0=c0, src=src):
                        for j in range(12):
                            ins = nc.tensor.transpose(out=pt[:, j, :], in_=src[:, (c0 + j) * 64:(c0 + j + 1) * 64],
                                                      identity=P.idf[:R, :R])
                        return ins
                    S.run("pe", [bsrc, P.b_id], [bpt], tr)
                    P.evac([bpt], [bdst], dst[:, c0:c0 + 12, :], pt[:])
        if d1_only:
            dbg, bdbg = P.scratch("d1dbg", [R, 3 * NTOK + NCH], F32)
            S.dma("sp", dbg[:, 0:NTOK], A[:], [bA], [bdbg])
            S.dma("sp", dbg[:, NTOK:2 * NTOK], NEGMU[:], [bNEGMU], [bdbg])
            S.dma("sp", dbg[:, 2 * NTOK:3 * NTOK], SCL[:], [bSCL], [bdbg])
            S.dma("sp", dbg[:, 3 * NTOK:3 * NTOK + NCH], DEC[:], [bDEC], [bdbg])
            dbg2, bdbg2 = P.scratch("d1dbg2", [64, 2 * NCH * R], F32)
            S.dma("sp", dbg2[:, 0:NCH * R], WKtm[:].rearrange("p c r -> p (c r)"), [bWK], [bdbg2])
            S.dma("sp", dbg2[:, NCH * R:], EMTtm[:].rearrange("p c r -> p (c r)"), [bEMT], [bdbg2])
            P.b_d1dbg, P.b_d1dbg2 = bdbg, bdbg2
            return
        CT = sb("CT", [128, 8, 2, 512]); CTb = sb("CTb", [128, 8, 2, 512], BF16)
        NT = sb("NT", [128, 8, 2]); NTb = sb("NTb", [128, 8, 2, 2], BF16)
        bCT, bCTb, bNT, bNTb = Buf("CT"), Buf("CTb"), Buf("NT"), Buf("NTb")
        kt_rot = P.rot_sb(es, "M_kt", [64, 2048], BF16, 2)
        v_rot = P.rot_sb(es, "M_v", [64, 8, 512], BF16, 2)
        q_rot = P.rot_sb(es, "M_q", [128, 16, 64], BF16, 2)
        k_rot = P.rot_sb(es, "M_k", [128, 16, 64], BF16, 2)
        h1_rot = P.rot_sb(es, "M_h1", [64, 8, 512], F32, 1)
        so_rot = P.rot_sb(es, "M_so", [64, 8, 512], BF16, 1)
        vs_rot = P.rot_sb(es, "M_vs", [64, 8, 512], BF16, 1)
        hst_rot = P.rot_sb(es, "M_hst", [64, 512], F32, 3)
        R1 = sb("R1", [R, 8, 64]); R2 = sb("R2", [R, 8, 64]); R3 = sb("R3", [R, 8])
        bR1, bR2, bR3 = Buf("R1"), Buf("R2"), Buf("R3")
        WT = sb("WT", [64, 512]); PT = sb("PT", [64, 512], BF16); QS = sb("QS", [128, 16, 64], BF16)
        bWT, bPT, bQS = Buf("WT"), Buf("PT"), Buf("QS")
        RD = sb("RD", [64, 8]); RD2 = sb("RD2", [64, 8]); DECs = sb("DECs", [128, 8]); WKb = sb("WKb", [64, 8, 2], BF16)
        bRD, bDECs, bWKb = Buf("RD"), Buf("DECs"), Buf("WKb")
        HS = sb("HS", [64, 8, 512]); SS = sb("SS", [64, 16]); YA = sb("YA", [64, 4096], BF16)
        junk = sb("junk", [64, 512], BF16)
        bHS, bSS, bYA, bjunk = Buf("HS"), Buf("SS"), Buf("YA"), Buf("junk")
        psD = P.ps(es, "M_psD", [64, 512]); psE = P.ps(es, "M_psE", [128, 512]); psS = P.ps(es, "M_psS", [64, 512])
        psM = P.ps(es, "M_psM", [128, 512])
        bpsD, bpsE, bpsS = Buf("psD"), Buf("psE"), Buf("psS")
        bDEN = bNUPS = bDECp = Buf("psM")
        num_rot = P.rot_ps(es, "M_num", [64, 512], F32, 2)
        upd_rot = P.rot_ps(es, "M_upd", [128, 512], F32, 2)
        qTv = P.qT.rearrange("(g p) t -> p g t", p=128)
        kTv = P.kT.rearrange("(g p) t -> p g t", p=128)
        for d in (0, 1):
            pb = 0 if d == 0 else 32
            pr = slice(pb, pb + 8)
            if d == 0:
                seq = [(c, c >= 4) for c in range(0, 20)]
            else:
                seq = [(c, False) for c in (3, 2, 1, 0)] + [(c, False) for c in range(35, 19, -1)] + [(c, True) for c in range(19, 3, -1)]

            lim = getattr(P, "lim", None)
            if lim is not None:
                if d not in lim["dirs"]:
                    continue
                seq = [x for x in seq if lim["sel"](x)]
            def zs():
                nc.gpsimd.memset(CT[:], 0.0)
                nc.gpsimd.memset(CTb[:], 0.0)
                nc.gpsimd.memset(NT[:], 0.0)
                return nc.gpsimd.memset(NTb[:], 0.0)
            S.run("pool", [], [bCT, bCTb, bNT, bNTb], zs)
            for si, (c, is_out) in enumerate(seq):
                last = si == len(seq) - 1
                n0 = c * 64
                o0 = n0 - OWN0
                cs = slice(n0, n0 + 64)
                KTc, bKT = kt_rot.next()
                Vc, bV = v_rot.next()
                S.dma("sp", KTc[:], P.k_tm[n0:n0 + 64, :], [P.b_k_tm], [bKT])
                S.dma("sp", Vc[:].rearrange("p h v -> p (h v)"), P.v_tm[n0:n0 + 64, :], [P.b_v_tm], [bV])
                if is_out:
                    Qc, bQ = q_rot.next()
                    Kc, bK = k_rot.next()
                    S.dma("sp", Qc[:], qTv[:, :, o0:o0 + 64], [P.b_qT], [bQ])
                    S.dma("sp", Kc[:], kTv[:, :, n0:n0 + 64], [P.b_kT], [bK])
                    if d == 1:
                        H1c, bH1 = h1_rot.next()
                        SOc, bSO = so_rot.next()
                        S.dma("sp", H1c[:].rearrange("p h v -> p (h v)"), P.h1[o0:o0 + 64, :], [P.b_h1], [bH1])
                        S.dma("sp", SOc[:].rearrange("p h v -> p (h v)"), P.so[o0:o0 + 64, :], [P.b_so], [bSO])
                    S.run("dve", [bNEGMU, bMASKD], [bR1], lambda: nc.vector.tensor_tensor(
                        out=R1[pr], in0=NEGMU[pr, cs].unsqueeze(1).to_broadcast([8, 8, 64]), in1=MASKD[pr], op=ALU.mult))
                    S.run("dve", [bSCL, bMASKD], [bR2], lambda: nc.vector.tensor_tensor(
                        out=R2[pr], in0=SCL[pr, cs].unsqueeze(1).to_broadcast([8, 8, 64]), in1=MASKD[pr], op=ALU.mult))

                    def dmm():
                        nc.tensor.matmul(psD[:, :], lhsT=P.onesf[pr, 0:64], rhs=R1[pr].rearrange("p h t -> p (h t)"), start=True, stop=False)
                        nc.tensor.matmul(psD[:, :], lhsT=A[pr, cs], rhs=MASKD[pr].rearrange("p h t -> p (h t)"), start=False, stop=False)
                        return nc.tensor.matmul(psD[:, :], lhsT=P.idf[0:64, 0:64], rhs=NM[d][:].rearrange("p h t -> p (h t)"), start=False, stop=True)
                    S.run("pe", [bR1, bA, bMASKD, bNM, P.b_id], [bpsD], dmm)
                    S.run("act", [bpsD], [bWT], lambda: nc.scalar.activation(out=WT[:], in_=psD[:], func=AF.Exp))
                    S.run("pe", [bR2, P.b_id], [bpsE], lambda: nc.tensor.matmul(
                        psE[:, :], lhsT=P.onesf[pr, 0:128], rhs=R2[pr].rearrange("p h t -> p (h t)"), start=True, stop=True))
                    S.run("dve", [bpsE, bQ], [bQS], lambda: nc.vector.tensor_tensor(
                        out=QS[:].rearrange("p (h k) t -> p h k t", k=2), in0=Qc[:].rearrange("p (h k) t -> p h k t", k=2),
                        in1=psE[:].rearrange("p (h t) -> p h t", t=64).unsqueeze(2).to_broadcast([128, 8, 2, 64]), op=ALU.mult))

                    def smm():
                        for h in range(8):
                            for kc in range(2):
                                ins = nc.tensor.matmul(psS[:, h * 64:(h + 1) * 64], lhsT=Kc[:, h * 2 + kc, :], rhs=Qc[:, h * 2 + kc, :],
                                                       start=(kc == 0), stop=(kc == 1))
                        return ins
                    S.run("pe", [bK, bQ], [bpsS], smm)
                    S.run("dve", [bpsS, bWT], [bPT], lambda: nc.vector.tensor_tensor(out=PT[:], in0=psS[:], in1=WT[:], op=ALU.mult))

                    def denmm():
                        for h in range(8):
                            nc.tensor.matmul(psM[0:64, 2 * h:2 * h + 2], lhsT=PT[:, h * 64:(h + 1) * 64], rhs=P.onesb[0:64, 0:2], start=True, stop=False)
                            for kc in range(2):
                                ins = nc.tensor.matmul(psM[0:64, 2 * h:2 * h + 2], lhsT=QS[:, h * 2 + kc, :], rhs=NTb[:, h, kc, :],
                                                       start=False, stop=(kc == 1))
                        return ins
                    S.run("pe", [bPT, bQS, bNTb], [bDEN], denmm)

                    S.run("dve", [bDEN], [bRD], lambda: nc.vector.tensor_copy(out=RD2[:], in_=psM[0:64, 0:16].rearrange("p (h t) -> p h t", t=2)[:, :, 0]))
                    S.run("dve", [bRD], [bRD], lambda: nc.vector.scalar_tensor_tensor(
                        out=RD[:], in0=RD2[:], scalar=-1.0, in1=RD2[:], op0=ALU.mult, op1=ALU.max))
                    S.run("dve", [bRD, bEMT], [bRD], lambda: nc.vector.tensor_tensor(out=RD[:], in0=RD[:], in1=EMTtm[:, c, pr], op=ALU.max))
                    S.run("dve", [bRD], [bRD], lambda: nc.vector.reciprocal(out=RD[:], in_=RD[:]))
                    for h in range(8):
                        pn, bpn = num_rot.next()

                        def nmm(pn=pn, h=h):
                            nc.tensor.matmul(pn[:, :], lhsT=PT[:, h * 64:(h + 1) * 64], rhs=Vc[:, h, :], start=True, stop=False)
                            for kc in range(2):
                                ins = nc.tensor.matmul(pn[:, :], lhsT=QS[:, h * 2 + kc, :], rhs=CTb[:, h, kc, :], start=False, stop=(kc == 1))
                            return ins
                        S.run("pe", [bPT, bV, bQS, bCTb], [bpn], nmm)
                        if d == 0:
                            t, tb = hst_rot.next()
                            S.run("act", [bpn, bRD], [tb], lambda t=t, pn=pn, h=h: nc.scalar.activation(
                                out=t[:], in_=pn[:], func=AF.Copy, scale=RD[:, h:h + 1]))
                            S.dma("sp", P.h1[o0:o0 + 64, h * 512:(h + 1) * 512], t[:], [tb], [P.b_h1])
                        else:
                            S.run("dve", [bpn, bRD, bH1], [bHS], lambda pn=pn, h=h: nc.vector.scalar_tensor_tensor(
                                out=HS[:, h, :], in0=pn[:], scalar=RD[:, h:h + 1], in1=H1c[:, h, :], op0=ALU.mult, op1=ALU.add))
                    if d == 1:
                        S.run("dve", [], [bSS], lambda: nc.vector.memset(SS[:], 0.0))

                        def sq():
                            for h in range(8):
                                ins = nc.scalar.activation(out=junk[:], in_=HS[:, h, :], func=AF.Square, accum_out=SS[:, h:h + 1])
                            return ins
                        S.run("act", [bHS, bSS], [bjunk, bSS], sq)
                        S.run("act", [bSS], [bSS], lambda: nc.scalar.activation(
                            out=SS[:, 8:16], in_=SS[:, 0:8], func=AF.Sqrt, bias=EPS, scale=1.0 / 512))
                        S.run("dve", [bSS], [bSS], lambda: nc.vector.reciprocal(out=SS[:, 8:16], in_=SS[:, 8:16]))
                        S.run("dve", [bHS, bSS], [bHS], lambda: nc.vector.tensor_tensor(
                            out=HS[:], in0=HS[:], in1=SS[:, 8:16].unsqueeze(2).to_broadcast([64, 8, 512]), op=ALU.mult))
                        S.run("dve", [bHS, bSO], [bYA], lambda: nc.vector.tensor_tensor(
                            out=YA[:], in0=HS[:].rearrange("p h v -> p (h v)"), in1=SOc[:].rearrange("p h v -> p (h v)"), op=ALU.mult))
                        S.dma("sp", P.ya_tm[o0:o0 + 64, :], YA[:], [bYA], [P.b_ya_tm])
                if last:
                    continue
                VS, bVS = vs_rot.next()
                S.run("dve", [bV, bWK], [bVS], lambda: nc.vector.tensor_tensor(
                    out=VS[:], in0=Vc[:], in1=WKtm[:, c, pr].unsqueeze(2).to_broadcast([64, 8, 512]), op=ALU.mult))
                S.run("dve", [bWK], [bWKb], lambda: nc.vector.tensor_copy(out=WKb[:], in_=WKtm[:, c, pr].unsqueeze(2).to_broadcast([64, 8, 2])))
                stage = (lim or {}).get("stage", 99)
                if stage < 2:
                    continue
                S.run("dve", [P.b_id, bDEC], [bR3], lambda: nc.vector.tensor_scalar(
                    out=R3[pr], in0=P.idf[pr, pb:pb + 8], scalar1=DEC[pr, c:c + 1], scalar2=None, op0=ALU.mult))
                S.run("pe", [bR3, P.b_id], [bDECp], lambda: nc.tensor.matmul(
                    psM[:, 32:40], lhsT=P.onesf[pr, 0:128], rhs=R3[pr], start=True, stop=True))
                S.run("act", [bDECp], [bDECs], lambda: nc.scalar.copy(out=DECs[:], in_=psM[:, 32:40]))

                if stage < 3:
                    continue

                def numm():
                    for h in range(8):
                        for kc in range(2):
                            ins = nc.tensor.matmul(psM[:, 64 + (h * 2 + kc) * 2:66 + (h * 2 + kc) * 2], lhsT=KTc[:, h * 256 + kc * 128:h * 256 + (kc + 1) * 128],
                                                   rhs=WKb[:, h, :], start=True, stop=True)
                    return ins
                S.run("pe", [bKT, bWKb], [bNUPS], numm)

                if stage < 4:
                    continue
                S.run("dve", [bDECs, bNTb], [bNT], lambda: nc.vector.tensor_tensor(
                    out=NT[:], in0=NT[:], in1=DECs[:].unsqueeze(2).to_broadcast([128, 8, 2]), op=ALU.mult))
                S.run("dve", [bNUPS, bNT], [bNT], lambda: nc.vector.tensor_tensor(
                    out=NT[:], in0=NT[:], in1=psM[:, 64:96].rearrange("p (h k t) -> p h k t", k=2, t=2)[:, :, :, 0], op=ALU.add))
                S.run("dve", [bNT], [bNTb], lambda: nc.vector.tensor_copy(out=NTb[:], in_=NT[:].unsqueeze(3).to_broadcast([128, 8, 2, 2])))
                if stage < 5:
                    continue
                for h in range(8):
                    for kc in range(2):
                        pu, bpu = upd_rot.next()
                        S.run("pe", [bKT, bVS], [bpu], lambda pu=pu, h=h, kc=kc: nc.tensor.matmul(
                            pu[:, :], lhsT=KTc[:, h * 256 + kc * 128:h * 256 + (kc + 1) * 128], rhs=VS[:, h, :], start=True, stop=True))
                        S.run("dve", [bpu, bDECs, bCTb], [bCT], lambda pu=pu, h=h, kc=kc: nc.vector.scalar_tensor_tensor(
                            out=CT[:, h, kc, :], in0=CT[:, h, kc, :], scalar=DECs[:, h:h + 1], in1=pu[:], op0=ALU.mult, op1=ALU.add))
                        S.run("act", [bCT], [bCTb], lambda h=h, kc=kc: nc.scalar.copy(out=CTb[:, h, kc, :], in_=CT[:, h, kc, :]))


Prog.phase_mlstm = phase_mlstm


def small_T(P, es, name, vec, n):
    nc, S = P.nc, P.S
    t = P.sb(es, name, [128, n], F32)
    b = Buf(name)
    with P.scope() as es2:
        r = P.sb(es2, name + "_r", [n, 128], F32)
        br = Buf(name + "r")
        S.dma("sp", r[:], vec.rearrange("o (k p) -> (o k) p", p=128), [], [br])
        pt = P.ps(es2, name + "_pt", [128, n], F32)
        bpt = Buf(name + "pt")
        S.run("pe", [br, P.b_id], [bpt], lambda: nc.tensor.transpose(out=pt[:, :], in_=r[:, :], identity=P.idf[:n, :n]))
        S.run("dve", [bpt], [b], lambda: nc.vector.tensor_copy(out=t[:], in_=pt[:]))
    return t, b


def phase_mla(P):
    nc, S = P.nc, P.S
    sc = P.scratch
    P.cqT, P.b_cqT = sc("cqT", [1024, NOWN], BF16)
    P.ckvT, P.b_ckvT = sc("ckvT", [512, NTOK], BF16)
    P.qnT, P.b_qnT = sc("qnT", [4096, NOWN], BF16)
    P.qrA, P.b_qrA = sc("qrA", [2048, NOWN], F32)
    P.qrB, P.b_qrB = sc("qrB", [2048, NOWN], F32)
    P.knT, P.b_knT = sc("knT", [4096, NTOK], BF16)
    P.v_a, P.b_v_a = sc("v_a", [NTOK, 4096], BF16)
    P.ybT, P.b_ybT = sc("ybT", [4096, NOWN], BF16)
    q_norm = P.inp("q_norm", [1, 1024]); kv_norm = P.inp("kv_norm", [1, 512])
    w_uqn = P.inp("w_uqn", [1024, 4096]); w_uqr = P.inp("w_uqr", [1024, 2048]); w_uqs = P.inp("w_uqs", [1024, 2048])
    w_ukn = P.inp("w_ukn", [512, 4096]); w_ukv = P.inp("w_ukvv", [512, 4096])
    posinfo = P.inp("posinfo", [1, 2])
    with P.scope() as es:
        QN = P.sb(es, "E_QN", [128, 1024]); bQN = Buf("QN")
        P.bload(QN[:], q_norm[0:1, :], [], bQN)
        P.norm_T("E1", P.zcq, P.b_zcq, NOWN, 1024, lambda ti: QN, None, P.cqT, P.b_cqT, [bQN])
    with P.scope() as es:
        KVN = P.sb(es, "E_KVN", [128, 512]); bKVN = Buf("KVN")
        P.bload(KVN[:], kv_norm[0:1, :], [], bKVN)
        P.norm_T("E2", P.zckv, P.b_zckv, NTOK, 512, lambda ti: KVN, None, P.ckvT, P.b_ckvT, [bKVN])
    with P.scope() as es:
        stb = P.rot_sb(es, "E_stb", [128, 512], BF16, 3)
        stf = P.rot_sb(es, "E_stf", [128, 512], F32, 3)
        E = P.store_epi
        P.mm_phase("E4", P.cqT, P.b_cqT, NOWN, 1024, [
            dict(w=w_uqn, N=4096, mode="fm", epi=E(stb, P.qnT, P.b_qnT, BF16)),
            dict(w=w_uqr, N=2048, mode="fm", epi=E(stf, P.qrA, P.b_qrA, F32)),
            dict(w=w_uqs, N=2048, mode="fm", epi=E(stf, P.qrB, P.b_qrB, F32))])
        P.mm_phase("E5", P.ckvT, P.b_ckvT, NTOK, 512, [
            dict(w=w_ukn, N=4096, mode="fm", epi=E(stb, P.knT, P.b_knT, BF16)),
            dict(w=w_ukv, N=4096, mode="tm", epi=E(stb, P.v_a, P.b_v_a, BF16))], TG=1152)
    with P.scope() as es:
        sb = lambda n, s, d=F32: P.sb(es, "R_" + n, s, d)
        CC = sb("CC", [64, NTOK]); SSn = sb("SS", [64, NTOK]); KR = sb("KR", [64, NTOK], BF16)
        bCC, bSS, bKR = Buf("CC"), Buf("SS"), Buf("KR")
        with P.scope() as es2:
            t2 = lambda n, s, d=F32: P.sb(es2, "RT_" + n, s, d)
            ii = t2("ii", [64, 2048], I32); tf = t2("tf", [64, 2048]); rw = t2("rw", [64, 2048]); cl = t2("cl", [64, 2048])
            ki = t2("ki", [64, 2048], I32); ang = t2("ang", [64, 2048]); tmp = t2("tmp", [64, 2048])
            pi_ = t2("pi", [64, 2]); pp = t2("pp", [64, 8]); ppi = t2("ppi", [64, 2], I32)
            b = {n: Buf(n) for n in "ii tf rw cl ki ang tmp pi pp ppi".split()}
            P.bload(pi_[:], posinfo[0:1, :], [], b["pi"], 64)
            S.run("pool", [], [b["ii"]], lambda: nc.gpsimd.iota(ii[:], pattern=[[1, 2048]], base=0, channel_multiplier=0))
            S.run("pool", [], [b["ppi"]], lambda: nc.gpsimd.iota(ppi[:, 0:1], pattern=[[0, 1]], base=0, channel_multiplier=1))
            R_ = lambda e, r, w, f: S.run(e, [b[x] for x in r], [b[x] for x in w], f)
            R_("dve", ["ii"], ["tf"], lambda: nc.vector.tensor_copy(out=tf[:], in_=ii[:]))
            R_("dve", ["tf", "pi"], ["tf"], lambda: nc.vector.tensor_scalar(out=tf[:], in0=tf[:], scalar1=pi_[:, 1:2], scalar2=pi_[:, 0:1], op0=ALU.mult, op1=ALU.add))
            R_("dve", ["tf"], ["tmp"], lambda: nc.vector.tensor_scalar(out=tmp[:], in0=tf[:], scalar1=-31.5, scalar2=1.0 / 64, op0=ALU.add, op1=ALU.mult))
            R_("dve", ["tmp"], ["ki"], lambda: nc.vector.tensor_copy(out=ki[:], in_=tmp[:]))
            R_("dve", ["ki"], ["rw"], lambda: nc.vector.tensor_copy(out=rw[:], in_=ki[:]))
            R_("dve", ["rw", "tf"], ["cl"], lambda: nc.vector.scalar_tensor_tensor(out=cl[:], in0=rw[:], scalar=-64.0, in1=tf[:], op0=ALU.mult, op1=ALU.add))
            R_("dve", ["ppi"], ["pp"], lambda: nc.vector.tensor_copy(out=pp[:, 0:1], in_=ppi[:, 0:1]))
            R_("dve", ["pp"], ["pp"], lambda: nc.vector.tensor_scalar(out=pp[:, 1:2], in0=pp[:, 0:1], scalar1=-15.5, scalar2=1.0 / 32, op0=ALU.add, op1=ALU.mult))
            R_("dve", ["pp"], ["ppi"], lambda: nc.vector.tensor_copy(out=ppi[:, 1:2], in_=pp[:, 1:2]))
            R_("dve", ["ppi"], ["pp"], lambda: nc.vector.tensor_copy(out=pp[:, 1:2], in_=ppi[:, 1:2]))
            R_("dve", ["pp"], ["pp"], lambda: nc.vector.scalar_tensor_tensor(out=pp[:, 2:3], in0=pp[:, 1:2], scalar=-32.0, in1=pp[:, 0:1], op0=ALU.mult, op1=ALU.add))
            R_("dve", ["pp"], ["pp"], lambda: nc.vector.tensor_scalar(out=pp[:, 3:4], in0=pp[:, 2:3], scalar1=15.5, scalar2=None, op0=ALU.is_lt))
            R_("dve", ["pp"], ["pp"], lambda: nc.vector.scalar_tensor_tensor(out=pp[:, 4:5], in0=pp[:, 3:4], scalar=16.0, in1=pp[:, 2:3], op0=ALU.mult, op1=ALU.add))
            R_("dve", ["pp"], ["pp"], lambda: nc.vector.tensor_scalar(out=pp[:, 4:5], in0=pp[:, 4:5], scalar1=-16.0, scalar2=None, op0=ALU.add))
            R_("act", ["pp"], ["pp"], lambda: nc.scalar.activation(out=pp[:, 5:6], in_=pp[:, 4:5], func=AF.Exp, scale=-float(np.log(10000.0)) / 16))
            R_("dve", ["pp"], ["pp"], lambda: nc.vector.tensor_scalar(out=pp[:, 6:7], in0=pp[:, 0:1], scalar1=31.5, scalar2=2.0, op0=ALU.is_gt, op1=ALU.mult))
            R_("dve", ["pp"], ["pp"], lambda: nc.vector.tensor_scalar(out=pp[:, 6:7], in0=pp[:, 6:7], scalar1=-1.0, scalar2=None, op0=ALU.add))
            R_("dve", ["rw", "cl"], ["tmp"], lambda: nc.vector.tensor_tensor(out=tmp[:], in0=rw[:], in1=cl[:], op=ALU.subtract))
            R_("dve", ["tmp", "cl", "pp"], ["ang"], lambda: nc.vector.scalar_tensor_tensor(out=ang[:], in0=tmp[:], scalar=pp[:, 3:4], in1=cl[:], op0=ALU.mult, op1=ALU.add))
            R_("dve", ["ang", "pp"], ["ang"], lambda: nc.vector.tensor_scalar(out=ang[:], in0=ang[:], scalar1=pp[:, 5:6], scalar2=None, op0=ALU.mult))

            def sin_of(shift, out_ap, post_scale):
                R_("dve", ["ang"], ["tmp"], lambda: nc.vector.tensor_scalar(out=tmp[:], in0=ang[:], scalar1=shift, scalar2=1.0 / (2 * np.pi), op0=ALU.add, op1=ALU.mult))
                R_("dve", ["tmp"], ["ki"], lambda: nc.vector.tensor_copy(out=ki[:], in_=tmp[:]))
                R_("dve", ["ki"], ["rw"], lambda: nc.vector.tensor_copy(out=rw[:], in_=ki[:]))
                R_("dve", ["rw", "ang"], ["tmp"], lambda: nc.vector.scalar_tensor_tensor(out=tmp[:], in0=rw[:], scalar=-2 * np.pi, in1=ang[:], op0=ALU.mult, op1=ALU.add))
                R_("dve", ["tmp"], ["tmp"], lambda: nc.vector.tensor_scalar(out=tmp[:], in0=tmp[:], scalar1=shift, scalar2=None, op0=ALU.add))
                R_("dve", ["tmp"], ["tmp"], lambda: nc.vector.tensor_scalar(out=tmp[:], in0=tmp[:], scalar1=3.1415925, scalar2=-3.1415925, op0=ALU.min, op1=ALU.max))
                if post_scale is None:
                    S.run("act", [b["tmp"]], [bCC], lambda: nc.scalar.activation(out=out_ap, in_=tmp[:], func=AF.Sin))
                else:
                    S.run("act", [b["tmp"]], [bSS], lambda: nc.scalar.activation(out=out_ap, in_=tmp[:], func=AF.Sin))
                    S.run("dve", [bSS, b["pp"]], [bSS], lambda: nc.vector.tensor_scalar(out=out_ap, in0=out_ap, scalar1=pp[:, 6:7], scalar2=None, op0=ALU.mult))
            S.run("dve", [], [bCC], lambda: nc.vector.memset(CC[:, 0:OWN0], 1.0))
            S.run("dve", [], [bSS], lambda: nc.vector.memset(SSn[:, 0:OWN0], 0.0))
            sin_of(float(np.pi / 2), CC[:, OWN0:NTOK], None)
            sin_of(0.0, SSn[:, OWN0:NTOK], True)
            za = t2("za", [64, NTOK]); zb = t2("zb", [64, NTOK]); bza, bzb = Buf("za"), Buf("zb")
            S.dma("sp", za[:], P.zkrT[:, :], [P.b_zkrT], [bza])
            S.dma("sp", zb[:], P.zkrsT[:, :], [P.b_zkrsT], [bzb])
            S.run("dve", [bza, bCC], [bza], lambda: nc.vector.tensor_tensor(out=za[:], in0=za[:], in1=CC[:], op=ALU.mult))
            S.run("dve", [bzb, bSS], [bzb], lambda: nc.vector.tensor_tensor(out=zb[:], in0=zb[:], in1=SSn[:], op=ALU.mult))
            S.run("dve", [bza, bzb], [bKR], lambda: nc.vector.tensor_tensor(out=KR[:], in0=za[:], in1=zb[:], op=ALU.add))
        kn_rot = P.rot_sb(es, "R_kn", [128, NTOK], BF16, 2)
        v_rot = P.rot_sb(es, "R_v", [128, 18, 128], BF16, 2)
        qn_rot = P.rot_sb(es, "R_qn", [128, NOWN], BF16, 2)
        qa_rot = P.rot_sb(es, "R_qa", [64, NOWN], F32, 2)
        qb_rot = P.rot_sb(es, "R_qb", [64, NOWN], F32, 2)
        qr_rot = P.rot_sb(es, "R_qr", [64, NOWN], BF16, 2)
        pt_rot = P.rot_sb(es, "R_pt", [128, 512], BF16, 3)
        y_rot = P.rot_sb(es, "R_y", [128, 512], BF16, 2)
        rl_rot = P.rot_sb(es, "R_rl", [128, 512], F32, 2)
        s_rot = P.rot_ps(es, "R_s", [128, 512], F32, 3)
        o_rot = P.rot_ps(es, "R_o", [128, 512], F32, 2)
        l_rot = P.rot_ps(es, "R_l", [128, 512], F32, 2)
        knv = P.knT.rearrange("(h p) t -> h p t", p=128)
        qnv = P.qnT.rearrange("(h p) t -> h p t", p=128)
        qav = P.qrA.rearrange("(h p) t -> h p t", p=64)
        qbv = P.qrB.rearrange("(h p) t -> h p t", p=64)
        for h in range(32):
            kn, bkn = kn_rot.next(); v, bv = v_rot.next(); qn, bqn = qn_rot.next()
            qa, bqa = qa_rot.next(); qb, bqb = qb_rot.next(); qr, bqr = qr_rot.next()
            S.dma("sp", kn[:], knv[h], [P.b_knT], [bkn])
            S.dma("sp", v[:], P.v_a[:, h * 128:(h + 1) * 128].rearrange("(kb p) d -> p kb d", p=128), [P.b_v_a], [bv])
            S.dma("sp", qn[:], qnv[h], [P.b_qnT], [bqn])
            S.dma("sp", qa[:], qav[h], [P.b_qrA], [bqa])
            S.dma("sp", qb[:], qbv[h], [P.b_qrB], [bqb])
            S.run("dve", [bqa, bCC], [bqa], lambda: nc.vector.tensor_tensor(out=qa[:], in0=qa[:], in1=CC[:, OWN0:OWN1], op=ALU.mult))
            S.run("dve", [bqb, bSS], [bqb], lambda: nc.vector.tensor_tensor(out=qb[:], in0=qb[:], in1=SSn[:, OWN0:OWN1], op=ALU.mult))
            S.run("dve", [bqa, bqb], [bqr], lambda: nc.vector.tensor_tensor(out=qr[:], in0=qa[:], in1=qb[:], op=ALU.add))
            for qt in range(2):
                qs_ = slice(qt * 512, (qt + 1) * 512)
                po, bpo = o_rot.next(); pl, bpl = l_rot.next()
                for kb in range(18):
                    ks_ = slice(kb * 128, (kb + 1) * 128)
                    ps_, bps = s_rot.next()

                    def smm(ps_=ps_, ks_=ks_):
                        nc.tensor.matmul(ps_[:, :], lhsT=kn[:, ks_], rhs=qn[:, qs_], start=True, stop=False)
                        return nc.tensor.matmul(ps_[:, :], lhsT=KR[:, ks_], rhs=qr[:, qs_], start=False, stop=True)
                    S.run("pe", [bkn, bqn, bKR, bqr], [bps], smm)
                    pt, bpt = pt_rot.next()
                    S.run("act", [bps], [bpt], lambda pt=pt, ps_=ps_: nc.scalar.activation(out=pt[:], in_=ps_[:], func=AF.Exp, scale=A_SCALE))

                    def pv(pt=pt, kb=kb):
                        nc.tensor.matmul(po[:, :], lhsT=v[:, kb, :], rhs=pt[:], start=(kb == 0), stop=(kb == 17))
                        return nc.tensor.matmul(pl[:, :], lhsT=P.onesb[:, :], rhs=pt[:], start=(kb == 0), stop=(kb == 17))
                    S.run("pe", [bv, bpt, P.b_id], [bpo, bpl], pv)
                rl, brl = rl_rot.next(); y, by = y_rot.next()
                S.run("dve", [bpl], [brl], lambda rl=rl: nc.vector.reciprocal(out=rl[:], in_=pl[:]))
                S.run("dve", [bpo, brl], [by], lambda y=y, rl=rl: nc.vector.tensor_tensor(out=y[:], in0=po[:], in1=rl[:], op=ALU.mult))
                S.dma("sp", P.ybT[h * 128:(h + 1) * 128, qs_], y[:], [by], [P.b_ybT])


Prog.phase_mla = phase_mla


def phase_merge(P):
    nc, S = P.nc, P.S
    sc = P.scratch
    P.yaT, P.b_yaT = sc("yaT", [D, NOWN], BF16)
    P.m1T, P.b_m1T = sc("m1T", [D, NOWN], F32)
    P.mT, P.b_mT = sc("mT", [D, NOWN], BF16)
    P.y, P.b_y = sc("y", [NOWN, D], F32)
    wa = P.inp("w_branch_a", [D, D]); wb = P.inp("w_branch_b", [D, D]); wo = P.inp("w_out", [D, D])
    mon = P.inp("m_out_norm", [1, D])
    with P.scope() as es:
        MON, bMON = small_T(P, es, "F_MON", mon, 32)
        P.transpose_pass("F0", P.ya_tm, P.b_ya_tm, NOWN, D, P.yaT, P.b_yaT, scale_col=MON, scale_buf=bMON)
    with P.scope() as es:
        g_rot = P.rot_sb(es, "F_g", [128, 512], BF16, 3)
        m_rot = P.rot_sb(es, "F_m", [128, 512], F32, 3)
        t_rot = P.rot_sb(es, "F_t", [128, 512], F32, 3)
        o_rot = P.rot_sb(es, "F_o", [128, 512], BF16, 3)

        def epi1(p, c0, t0, cs, ts, pb):
            g, bg = g_rot.next(); t, bt = t_rot.next()
            S.dma("sp", g[:cs, :ts], P.sgaT[c0:c0 + cs, t0:t0 + ts], [P.b_sgaT], [bg])
            S.run("dve", [pb, bg], [bt], lambda: nc.vector.tensor_tensor(out=t[:cs, :ts], in0=p, in1=g[:cs, :ts], op=ALU.mult))
            S.dma("sp", P.m1T[c0:c0 + cs, t0:t0 + ts], t[:cs, :ts], [bt], [P.b_m1T])

        def epi2(p, c0, t0, cs, ts, pb):
            g, bg = g_rot.next(); t, bt = t_rot.next(); m, bm = m_rot.next(); o, bo = o_rot.next()
            S.dma("sp", g[:cs, :ts], P.sgbT[c0:c0 + cs, t0:t0 + ts], [P.b_sgbT], [bg])
            S.dma("sp", m[:cs, :ts], P.m1T[c0:c0 + cs, t0:t0 + ts], [P.b_m1T], [bm])
            S.run("dve", [pb, bg], [bt], lambda: nc.vector.tensor_tensor(out=t[:cs, :ts], in0=p, in1=g[:cs, :ts], op=ALU.mult))
            S.run("dve", [bt, bm], [bo], lambda: nc.vector.tensor_tensor(out=o[:cs, :ts], in0=t[:cs, :ts], in1=m[:cs, :ts], op=ALU.add))
            S.dma("sp", P.mT[c0:c0 + cs, t0:t0 + ts], o[:cs, :ts], [bo], [P.b_mT])
        P.mm_phase("F1", P.yaT, P.b_yaT, NOWN, D, [dict(w=wa, N=D, mode="fm", epi=epi1)])
        P.mm_phase("F2", P.ybT, P.b_ybT, NOWN, D, [dict(w=wb, N=D, mode="fm", epi=epi2)])
        P.mm_phase("F3", P.mT, P.b_mT, NOWN, D, [dict(w=wo, N=D, mode="tm", epi=P.store_epi(t_rot, P.y, P.b_y, F32))])


def _rstd(P, x, xb, junk, bjunk, st, stb, F):
    nc, S = P.nc, P.S
    S.run("dve", [], [stb], lambda: nc.vector.memset(st[:, :], 0.0))
    S.run("act", [xb, stb], [bjunk, stb], lambda: nc.scalar.activation(out=junk[:, :], in_=x, func=AF.Square, accum_out=st[:, 0:1]))
    S.run("act", [stb], [stb], lambda: nc.scalar.activation(out=st[:, 1:2], in_=st[:, 0:1], func=AF.Sqrt, bias=EPS, scale=1.0 / F))
    S.run("dve", [stb], [stb], lambda: nc.vector.reciprocal(out=st[:, 1:2], in_=st[:, 1:2]))


def phase_ffn(P, experts=range(NE)):
    nc, S = P.nc, P.S
    sc = P.scratch
    xs = P.inp("xs", [NTOK, D])
    P.x1, P.b_x1 = sc("x1", [NOWN, D], F32)
    P.h2T, P.b_h2T = sc("h2T", [D, NOWN], BF16)
    P.gT, P.b_gT = sc("gT", [NE, NOWN], F32)
    P.fo, P.b_fo = sc("fo", [NOWN, D], F32)
    n2 = P.inp("norm_post_mix", [1, D]); n3 = P.inp("norm_pre_ffn", [1, D]); n4 = P.inp("norm_post_ffn", [1, D])
    rw = P.inp("router_w", [D, NE]); rb = P.inp("router_b", [1, NE])
    w_gu = P.inp("w_gu", [NE, D, 3072]); b_gu = P.inp("b_gu", [NE, 3072])
    w_dn = P.inp("w_down", [NE, FE, D]); b_dn = P.inp("b_down", [NE, D])
    out = P.nc.dram_tensor("out", [NOWN, D], F32, kind="ExternalOutput").ap()
    P.b_out = Buf("out")
    with P.scope() as es:
        P.nv, P.b_nv = P.sb(es, "PG_nv", [128, D]), Buf("nv")
        WG1, bWG1 = P.mod_tile(es, "PG_WG1", 0, 2, n2)
        W2, bW2 = P.mod_tile(es, "PG_W2", 0, 4, n3, True)
        SH2, bSH2 = P.mod_tile(es, "PG_SH2", 0, 3)
        RW = P.sb(es, "PG_RW", [128, 32, NE]); bRW = Buf("RW")
        S.dma("sp", RW[:], rw.rearrange("(kt p) e -> p kt e", p=128), [], [bRW])
        RB = P.sb(es, "PG_RB", [128, NE]); bRB = Buf("RB")
        P.bload(RB[:], rb[0:1, :], [], bRB)
        yt = P.sb(es, "PG_y", [128, D]); xt = P.sb(es, "PG_x", [128, D]); byt, bxt = Buf("yt"), Buf("xt")
        junk = P.sb(es, "PG_junk", [128, D], BF16); bjunk = Buf("junk")
        st = P.sb(es, "PG_st", [128, 4]); bst = Buf("st")
        hf = P.sb(es, "PG_hf", [128, 32, 128]); hb = P.sb(es, "PG_hb", [128, 32, 128], BF16); bhf, bhb = Buf("hf"), Buf("hb")
        L = P.sb(es, "PG_L", [128, NE]); E_ = P.sb(es, "PG_E", [128, NE]); MX = P.sb(es, "PG_MX", [128, 16])
        bL, bE, bMX = Buf("L"), Buf("E"), Buf("MX")
        GTs = P.sb(es, "PG_GT", [NE, 128]); bGTs = Buf("GTs")
        pt_rot = P.rot_ps(es, "PG_pt", [128, 4, 128], F32, 3)
        plg = P.ps(es, "PG_plg", [128, 512]); bplg = Buf("plg")
        h2v = P.h2T.rearrange("(kt p) t -> p kt t", p=128)
        for i in range(8):
            r0 = i * 128
            S.dma("sp", yt[:], P.y[r0:r0 + 128, :], [P.b_y], [byt])
            S.dma("sp", xt[:], xs[OWN0 + r0:OWN0 + r0 + 128, :], [], [bxt])
            _rstd(P, yt[:], byt, junk, bjunk, st, bst, D)
            S.run("dve", [byt, bst, bWG1], [byt], lambda: nc.vector.scalar_tensor_tensor(
                out=yt[:], in0=yt[:], scalar=st[:, 1:2], in1=WG1[:], op0=ALU.mult, op1=ALU.mult))
            S.run("dve", [byt, bxt], [bxt], lambda: nc.vector.tensor_tensor(out=xt[:], in0=yt[:], in1=xt[:], op=ALU.add))
            S.dma("sp", P.x1[r0:r0 + 128, :], xt[:], [bxt], [P.b_x1])
            _rstd(P, xt[:], bxt, junk, bjunk, st, bst, D)
            S.run("dve", [bxt, bst, bW2], [byt], lambda: nc.vector.scalar_tensor_tensor(
                out=yt[:], in0=xt[:], scalar=st[:, 1:2], in1=W2[:], op0=ALU.mult, op1=ALU.mult))
            S.run("dve", [byt, bSH2], [byt], lambda: nc.vector.tensor_tensor(out=yt[:], in0=yt[:], in1=SH2[:], op=ALU.add))
            for g0 in range(0, 32, 4):
                p, pb = pt_rot.next()

                def tr(p=p, g0=g0):
                    for j in range(4):
                        ins = nc.tensor.transpose(out=p[:, j, :], in_=yt[:, (g0 + j) * 128:(g0 + j + 1) * 128], identity=P.idf[:, :])
                    return ins
                S.run("pe", [byt, P.b_id], [pb], tr)
                S.run("act", [pb], [bhf], lambda p=p, g0=g0: nc.scalar.copy(out=hf[:, g0:g0 + 4, :], in_=p[:]))
                S.run("dve", [bhf], [bhb], lambda g0=g0: nc.vector.tensor_copy(out=hb[:, g0:g0 + 4, :], in_=hf[:, g0:g0 + 4, :]))
            S.dma("sp", h2v[:, :, r0:r0 + 128], hb[:], [bhb], [P.b_h2T])

            def lg():
                for kt in range(32):
                    ins = nc.tensor.matmul(plg[:, 0:NE], lhsT=hf[:, kt, :], rhs=RW[:, kt, :], start=(kt == 0), stop=(kt == 31))
                return ins
            S.run("pe", [bhf, bRW], [bplg], lg)
            S.run("dve", [bplg, bRB], [bL], lambda: nc.vector.tensor_tensor(out=L[:], in0=plg[:, 0:NE], in1=RB[:], op=ALU.add))
            S.run("dve", [bL], [bMX], lambda: nc.vector.max(out=MX[:, 0:8], in_=L[:]))
            S.run("dve", [bMX], [bMX], lambda: nc.vector.tensor_scalar(out=MX[:, 8:9], in0=MX[:, 0:1], scalar1=-1.0, scalar2=None, op0=ALU.mult))
            S.run("act", [bL, bMX], [bE], lambda: nc.scalar.activation(out=E_[:], in_=L[:], func=AF.Exp, bias=MX[:, 8:9], scale=1.0))
            S.run("dve", [bL, bMX], [bL], lambda: nc.vector.tensor_scalar(out=L[:], in0=L[:], scalar1=MX[:, 3:4], scalar2=None, op0=ALU.is_ge))
            S.run("dve", [bE, bL], [bE], lambda: nc.vector.tensor_tensor(out=E_[:], in0=E_[:], in1=L[:], op=ALU.mult))
            S.run("dve", [bE], [bMX], lambda: nc.vector.reduce_sum(out=MX[:, 9:10], in_=E_[:], axis=AX.X))
            S.run("dve", [bMX], [bMX], lambda: nc.vector.reciprocal(out=MX[:, 9:10], in_=MX[:, 9:10]))
            S.run("dve", [bE, bMX], [bE], lambda: nc.vector.tensor_scalar(out=E_[:], in0=E_[:], scalar1=MX[:, 9:10], scalar2=None, op0=ALU.mult))
            S.run("pe", [bE, P.b_id], [bplg], lambda: nc.tensor.transpose(out=plg[0:NE, 128:256], in_=E_[:, :], identity=P.idf[:, :]))
            S.run("dve", [bplg], [bGTs], lambda: nc.vector.tensor_copy(out=GTs[:], in_=plg[0:NE, 128:256]))
            S.dma("sp", P.gT[:, r0:r0 + 128], GTs[:], [bGTs], [P.b_gT])
    if getattr(P, "stop_after_g", False):
        return
    TH = 512
    with P.scope() as es:
        GT = P.sb(es, "H_GT", [NE, NOWN]); bGT = Buf("GT")
        S.dma("sp", GT[:], P.gT[:, :], [P.b_gT], [bGT])
        BD = P.sb(es, "H_BD", [NE, D]); bBD = Buf("BD")
        S.dma("sp", BD[:], b_dn[:, :], [], [bBD])
        BG = P.sb(es, "H_BG", [128, NE * 24]); bBG = Buf("BG")
        with P.scope() as es2:
            r_ = P.sb(es2, "H_bgr", [128, 128]); br_ = Buf("bgr")
            pp_ = P.ps(es2, "H_bgp", [128, 128]); bpp = Buf("bgp")
            bgv = b_gu.rearrange("e (r p) -> (e r) p", p=128)
            for blk in range(6):
                S.dma("sp", r_[:], bgv[blk * 128:(blk + 1) * 128, :], [], [br_])
                S.run("pe", [br_, P.b_id], [bpp], lambda: nc.tensor.transpose(out=pp_[:, :], in_=r_[:, :], identity=P.idf[:, :]))
                S.run("dve", [bpp], [bBG], lambda blk=blk: nc.vector.tensor_copy(out=BG[:, blk * 128:(blk + 1) * 128], in_=pp_[:, :]))
        act = P.sb(es, "H_act", [128, 32, TH], BF16); bact = Buf("act")
        FACC = P.sb(es, "H_facc", [128, TH // 128, D]); bF = [Buf(f"F{j}") for j in range(TH // 128)]
        actT = P.sb(es, "H_actT", [128, 12, TH], BF16); bactT = Buf("actT")
        GB = P.rot_sb(es, "H_GB", [128, TH], F32, 2)
        wg_rot = P.rot_sb(es, "H_wg", [128, 32, 256], BF16, 2)
        wd_rot = P.rot_sb(es, "H_wd", [128, 12, 512], BF16, 2)
        G1 = P.sb(es, "H_G1", [128, TH]); SG = P.sb(es, "H_SG", [128, TH]); L1 = P.sb(es, "H_L1", [128, TH])
        bG1, bSG, bL1 = Buf("G1"), Buf("SG"), Buf("L1")
        pg_rot = P.rot_ps(es, "H_pg", [128, 512], F32, 2)
        pl_rot = P.rot_ps(es, "H_pl", [128, 512], F32, 2)
        pd_rot = P.rot_ps(es, "H_pd", [128, 512], F32, 3)
        h2v = P.h2T.rearrange("(kt p) t -> p kt t", p=128)
        for half in range(NOWN // TH):
            t0 = half * TH
            S.dma("sp", act[:], h2v[:, :, t0:t0 + TH], [P.b_h2T], [bact])
            for j in range(TH // 128):
                for cb in range(8):
                    pd, bpd = pd_rot.next()
                    S.run("pe", [bGT, bBD], [bpd], lambda pd=pd, j=j, cb=cb: nc.tensor.matmul(
                        pd[:, :], lhsT=GT[:, t0 + j * 128:t0 + (j + 1) * 128], rhs=BD[:, cb * 512:(cb + 1) * 512], start=True, stop=True))
                    P.evac([bpd], [bF[j]], FACC[:, j, cb * 512:(cb + 1) * 512], pd[:, :])
            for e in experts:
                gb, bgb = GB.next()
                S.dma("sp", gb[:], P.gT[e:e + 1, t0:t0 + TH].partition_broadcast(128), [P.b_gT], [bgb])
                wgv = w_gu[e].rearrange("(kt p) n -> p kt n", p=128)
                for c in range(12):
                    wg, bwg = wg_rot.next()
                    S.dma("pq", wg[:], wgv[:, :, c * 256:(c + 1) * 256], [], [bwg])
                    pg, bpg = pg_rot.next(); pl, bpl = pl_rot.next()

                    def gmm(pg=pg, wg=wg):
                        for kt in range(32):
                            ins = nc.tensor.matmul(pg[:, :TH], lhsT=wg[:, kt, 0:128], rhs=act[:, kt, :], start=(kt == 0), stop=(kt == 31))
                        return ins

                    def lmm(pl=pl, wg=wg):
                        for kt in range(32):
                            ins = nc.tensor.matmul(pl[:, :TH], lhsT=wg[:, kt, 128:256], rhs=act[:, kt, :], start=(kt == 0), stop=(kt == 31))
                        return ins
                    S.run("pe", [bact, bwg], [bpg], gmm)
                    S.run("pe", [bact, bwg], [bpl], lmm)
                    col = e * 24 + c * 2
                    S.run("dve", [bpg, bBG], [bG1], lambda pg=pg, col=col: nc.vector.tensor_scalar(
                        out=G1[:], in0=pg[:, :TH], scalar1=BG[:, col:col + 1], scalar2=7.0, op0=ALU.add, op1=ALU.min))
                    S.run("act", [bG1], [bSG], lambda: nc.scalar.activation(out=SG[:], in_=G1[:], func=AF.Sigmoid, scale=1.702))
                    S.run("dve", [bpl, bBG], [bL1], lambda pl=pl, col=col: nc.vector.tensor_scalar(
                        out=L1[:], in0=pl[:, :TH], scalar1=BG[:, col + 1:col + 2], scalar2=7.0, op0=ALU.add, op1=ALU.min))
                    S.run("dve", [bL1], [bL1], lambda: nc.vector.tensor_scalar(out=L1[:], in0=L1[:], scalar1=-7.0, scalar2=1.0, op0=ALU.max, op1=ALU.add))
                    S.run("dve", [bG1, bSG], [bG1], lambda: nc.vector.tensor_tensor(out=G1[:], in0=G1[:], in1=SG[:], op=ALU.mult))
                    S.run("dve", [bL1, bgb], [bL1], lambda gb=gb: nc.vector.tensor_tensor(out=L1[:], in0=L1[:], in1=gb[:], op=ALU.mult))
                    S.run("dve", [bG1, bL1], [bactT], lambda c=c: nc.vector.tensor_tensor(out=actT[:, c, :], in0=G1[:], in1=L1[:], op=ALU.mult))
                wdv = w_dn[e].rearrange("(c p) n -> p c n", p=128)
                for cb in range(8):
                    wd, bwd = wd_rot.next()
                    S.dma("pq", wd[:], wdv[:, :, cb * 512:(cb + 1) * 512], [], [bwd])
                    for j in range(TH // 128):
                        pd, bpd = pd_rot.next()

                        def dmm(pd=pd, wd=wd, j=j):
                            for c in range(12):
                                ins = nc.tensor.matmul(pd[:, :], lhsT=actT[:, c, j * 128:(j + 1) * 128], rhs=wd[:, c, :], start=(c == 0), stop=(c == 11))
                            return ins
                        S.run("pe", [bactT, bwd], [bpd], dmm)
                        S.run("dve", [bpd, bF[j]], [bF[j]], lambda pd=pd, j=j, cb=cb: nc.vector.tensor_tensor(
                            out=FACC[:, j, cb * 512:(cb + 1) * 512], in0=FACC[:, j, cb * 512:(cb + 1) * 512], in1=pd[:, :], op=ALU.add))
            for j in range(TH // 128):
                S.dma("sp", P.fo[t0 + j * 128:t0 + (j + 1) * 128, :], FACC[:, j, :], [bF[j]], [P.b_fo])
    with P.scope() as es:
        P.nv, P.b_nv = P.sb(es, "I_nv", [128, D]), Buf("nv")
        WG2, bWG2 = P.mod_tile(es, "I_WG2", 0, 5, n4)
        f_rot = P.rot_sb(es, "I_f", [128, D], F32, 2)
        x_rot = P.rot_sb(es, "I_x", [128, D], F32, 2)
        junk = P.sb(es, "I_junk", [128, D], BF16); bjunk = Buf("junk")
        st_rot = P.rot_sb(es, "I_st", [128, 4], F32, 2)
        for i in range(8):
            r0 = i * 128
            ft, bft = f_rot.next(); xt, bxt = x_rot.next(); st, bst = st_rot.next()
            S.dma("sp", ft[:], P.fo[r0:r0 + 128, :], [P.b_fo], [bft])
            S.dma("sp", xt[:], P.x1[r0:r0 + 128, :], [P.b_x1], [bxt])
            _rstd(P, ft[:], bft, junk, bjunk, st, bst, D)
            S.run("dve", [bft, bst, bWG2], [bft], lambda: nc.vector.scalar_tensor_tensor(
                out=ft[:], in0=ft[:], scalar=st[:, 1:2], in1=WG2[:], op0=ALU.mult, op1=ALU.mult))
            S.run("dve", [bft, bxt], [bxt], lambda: nc.vector.tensor_tensor(out=xt[:], in0=ft[:], in1=xt[:], op=ALU.add))
            S.dma("sp", out[r0:r0 + 128, :], xt[:], [bxt], [P.b_out])


Prog.phase_merge = phase_merge
Prog.phase_ffn = phase_ffn


def build_full(debug=()):
    P = Prog(debug=debug)
    with ExitStack() as es:
        P.consts(es)
        P.phase_mod(); P.phase_h(); P.phase_proj(); P.phase_mlstm(); P.phase_mla(); P.phase_merge(); P.phase_ffn()
        P.S.finish([P.b_out] + [getattr(P, "b_" + n) for n in debug])
    return P


_prog = None


def kernel(**inputs):
    global _prog
    if _prog is None:
        _prog = build_full()
    P = _prog
    names = list(P.inputs)
    in_maps = [core_inputs(inputs, c, names) for c in range(8)]
    res = run_bass_kernel_spmd(P.nc, in_maps, core_ids=list(range(8)))
    out = np.empty((4, 2048, D), np.float32)
    for c in range(8):
        b, half = c // 2, c % 2
        o = np.asarray(res.results[c]["out"], dtype=np.float32)
        if half == 0:
            out[b, 0:1024] = o
        else:
            out[b, 1024:2048] = o[::-1]
    return out
```

```python
import numpy as np
from contextlib import ExitStack, contextmanager
import concourse.bass as bass
import concourse.mybir as mybir
from concourse.bass_utils import run_bass_kernel_spmd

F32 = mybir.dt.float32
BF16 = mybir.dt.bfloat16
I32 = mybir.dt.int32
ALU = mybir.AluOpType
AF = mybir.ActivationFunctionType
AX = mybir.AxisListType

D = 4096
NTOK = 2304
OWN0, OWN1 = 256, 1280
NOWN = 1024
EPS = 1e-6
NE = 32
FE = 1536
A_SCALE = 192 ** -0.5
CQ, CK, CV, CO, CG, CCQ, CCKV, CKR, CGA, CGB = 0, 2048, 4096, 8192, 12288, 12320, 13344, 13856, 13920, 18016
DIN = 22112


class Buf:
    __slots__ = ("name", "w", "r")

    def __init__(self, name=""):
        self.name = name
        self.w = None
        self.r = []


class Sched:
    DMAK = 6
    ROT = 28000

    def __init__(self, nc):
        self.nc = nc
        self.eng = {"pe": nc.tensor, "act": nc.scalar, "dve": nc.vector, "pool": nc.gpsimd, "sp": nc.sync}
        self.sem, self.cnt, self.nsem = {}, {}, 0
        for e in ("pe", "act", "dve", "pool"):
            self._fresh(e)
        self.q_issuer = {"sp": "sp", "pq": "pool"}
        self.qsem, self.qcnt, self.qi = {}, {}, {}
        for q in self.q_issuer:
            self.qsem[q] = [self._alloc(f"{q}{j}") for j in range(self.DMAK)]
            self.qcnt[q] = [0] * self.DMAK
            self.qi[q] = 0
        self.waited = {e: {} for e in self.eng}

    def _alloc(self, name):
        self.nsem += 1
        return self.nc.alloc_semaphore(name=f"s_{name}_{self.nsem}")

    def _fresh(self, e):
        self.sem[e] = self._alloc(e)
        self.cnt[e] = 0

    def _wait(self, ename, dep):
        sem, val, _ = dep
        w = self.waited[ename]
        key = id(sem)
        if w.get(key, (None, 0))[1] >= val:
            return
        w[key] = (sem, val)
        self.eng[ename].wait_ge(sem, val)

    def _deps(self, ename, reads, writes, is_dma):
        for b in reads:
            for d in b.w or ():
                self._wait(ename, d)
        for b in writes:
            for d in b.w or ():
                if is_dma or d[2] != ename:
                    self._wait(ename, d)
            for d in b.r:
                if is_dma or d[2] != ename:
                    self._wait(ename, d)

    def _update(self, tok, reads, writes):
        for b in reads:
            b.r.append(tok)
        for b in writes:
            if tok[2] == "dma":
                b.w = [d for d in (b.w or ()) if d[2] == "dma"][-(2 * self.DMAK - 1):] + [tok]
            else:
                b.w = [tok]
            b.r = []

    def run(self, ename, reads, writes, fn):
        if self.cnt[ename] >= self.ROT:
            self._fresh(ename)
        self._deps(ename, reads, writes, False)
        ins = fn()
        self.cnt[ename] += 1
        ins.then_inc(self.sem[ename], 1)
        tok = (self.sem[ename], self.cnt[ename], ename)
        self._update(tok, reads, writes)
        return tok

    def dma(self, q, out, in_, reads, writes, **kw):
        issuer = self.q_issuer[q]
        j = self.qi[q]
        self.qi[q] = (j + 1) % self.DMAK
        if self.qcnt[q][j] >= self.ROT:
            self._wait(issuer, (self.qsem[q][j], self.qcnt[q][j], "dma"))
            self.qsem[q][j] = self._alloc(f"{q}{j}")
            self.qcnt[q][j] = 0
        sem = self.qsem[q][j]
        if self.qcnt[q][j] > 0:
            self._wait(issuer, (sem, self.qcnt[q][j], "dma"))
        self._deps(issuer, reads, writes, True)
        eng = self.nc.sync if q == "sp" else self.nc.gpsimd
        eng.dma_start(out=out, in_=in_, **kw).then_inc(sem, 16)
        self.qcnt[q][j] += 16
        tok = (sem, self.qcnt[q][j], "dma")
        self._update(tok, reads, writes)
        return tok

    def barrier(self):
        toks = [(self.sem[e], self.cnt[e], e) for e in ("pe", "act", "dve", "pool") if self.cnt[e] > 0]
        for q in self.q_issuer:
            for j in range(self.DMAK):
                if self.qcnt[q][j] > 0:
                    toks.append((self.qsem[q][j], self.qcnt[q][j], "dma"))
        for e in self.eng:
            for t in toks:
                self._wait(e, t)

    def finish(self, bufs):
        for b in bufs:
            for d in b.w or ():
                self._wait("sp", d)


class Rot:
    def __init__(self, tiles, name):
        self.tiles = tiles
        self.bufs = [Buf(f"{name}{i}") for i in range(len(tiles))]
        self.i = 0

    def next(self):
        j = self.i % len(self.tiles)
        self.i += 1
        return self.tiles[j], self.bufs[j]


class Prog:
    def __init__(self, debug=()):
        self.nc = bass.Bass("TRN2", target_bir_lowering=False)
        self.S = Sched(self.nc)
        self.inputs = {}
        self.debug = set(debug)
        self.outs = []
        self.k = 0

    @contextmanager
    def scope(self):
        with ExitStack() as es:
            yield es
            self.S.barrier()

    def inp(self, name, shape, dtype=F32):
        if name not in self.inputs:
            self.inputs[name] = self.nc.dram_tensor(name, list(shape), dtype, kind="ExternalInput").ap()
        return self.inputs[name]

    def scratch(self, name, shape, dtype):
        if name in self.debug:
            self.outs.append(name)
            return self.nc.dram_tensor(name, list(shape), dtype, kind="ExternalOutput").ap(), Buf(name)
        return self.nc.dram_tensor(name, list(shape), dtype).ap(), Buf(name)

    def sb(self, es, name, shape, dtype=F32):
        return es.enter_context(self.nc.sbuf_tensor(name, list(shape), dtype))

    def ps(self, es, name, shape, dtype=F32):
        return es.enter_context(self.nc.psum_tensor(name, list(shape), dtype))

    def rot_sb(self, es, name, shape, dtype, n):
        return Rot([self.sb(es, f"{name}{i}", shape, dtype) for i in range(n)], name)

    def rot_ps(self, es, name, shape, dtype, n):
        return Rot([self.ps(es, f"{name}{i}", shape, dtype) for i in range(n)], name)

    def evac(self, reads, writes, out, in_):
        nc = self.nc
        self.k += 1
        if self.k % 2 == 0:
            return self.S.run("act", reads, writes, lambda: nc.scalar.copy(out=out, in_=in_))
        return self.S.run("dve", reads, writes, lambda: nc.vector.tensor_copy(out=out, in_=in_))

    def consts(self, es):
        nc, S = self.nc, self.S
        self.idf = self.sb(es, "idf", [128, 128], F32)
        self.idb = self.sb(es, "idb", [128, 128], BF16)
        self.b_id = Buf("ident")
        self.onesb = self.sb(es, "onesb", [128, 128], BF16)
        self.onesf = self.sb(es, "onesf", [128, 128], F32)

        def mk():
            nc.gpsimd.memset(self.onesb[:], 1.0)
            nc.gpsimd.memset(self.onesf[:], 1.0)
            return nc.gpsimd.memset(self.idf[:], 1.0)
        S.run("pool", [], [self.b_id], mk)
        S.run("pool", [self.b_id], [self.b_id], lambda: nc.gpsimd.affine_select(
            out=self.idf[:], in_=self.idf[:], pattern=[[-1, 128]], compare_op=ALU.is_equal, fill=0.0, base=0, channel_multiplier=1))
        S.run("dve", [self.b_id], [self.b_id], lambda: nc.vector.tensor_copy(out=self.idb[:], in_=self.idf[:]))

    def mm_phase(self, name, act, actbuf, T, K, jobs, TG=1024, act_pre=None):
        nc, S = self.nc, self.S
        KT = K // 128
        with self.scope() as es:
            ngroups = (T + TG - 1) // TG
            if act_pre is None:
                actv = act.rearrange("(kt p) t -> p kt t", p=128)
                a_rot = self.rot_sb(es, f"{name}_a", [128, KT, TG], BF16, 2 if ngroups > 1 else 1)
            NBmax = max(j.get("NB", 512) for j in jobs)
            w_rot = self.rot_sb(es, f"{name}_w", [128, KT, NBmax], BF16, 2)
            p_rot = self.rot_ps(es, f"{name}_ps", [128, 512], F32, 4)
            for g0 in range(0, T, TG):
                tg = min(TG, T - g0)
                if act_pre is None:
                    asb, ab = a_rot.next()
                    S.dma("sp", asb[:, :, :tg], actv[:, :, g0:g0 + tg], [actbuf] if actbuf else [], [ab])
                else:
                    asb, ab = act_pre
                for job in jobs:
                    N, mode, epi, NB = job["N"], job["mode"], job["epi"], job.get("NB", 512)
                    wv = job["w"].rearrange("(kt p) n -> p kt n", p=128)
                    for n0 in range(0, N, NB):
                        nb = min(NB, N - n0)
                        wsb, wb = w_rot.next()
                        S.dma("pq", wsb[:, :, :nb], wv[:, :, n0:n0 + nb], [], [wb])
                        if mode == "tm":
                            for t0 in range(0, tg, 128):
                                ts = min(128, tg - t0)
                                p, pb = p_rot.next()

                                def mm(p=p, t0=t0, ts=ts, wsb=wsb, nb=nb):
                                    for kt in range(KT):
                                        ins = nc.tensor.matmul(p[:ts, :nb], lhsT=asb[:, kt, t0:t0 + ts],
                                                               rhs=wsb[:, kt, :nb], start=(kt == 0), stop=(kt == KT - 1))
                                    return ins
                                S.run("pe", [ab, wb], [pb], mm)
                                epi(p[:ts, :nb], g0 + t0, n0, ts, nb, pb)
                        else:
                            for c0 in range(0, nb, 128):
                                cs = min(128, nb - c0)
                                for t0 in range(0, tg, 512):
                                    ts = min(512, tg - t0)
                                    p, pb = p_rot.next()

                                    def mm(p=p, c0=c0, cs=cs, t0=t0, ts=ts, wsb=wsb):
                                        for kt in range(KT):
                                            ins = nc.tensor.matmul(p[:cs, :ts], lhsT=wsb[:, kt, c0:c0 + cs],
                                                                   rhs=asb[:, kt, t0:t0 + ts],
                                                                   start=(kt == 0), stop=(kt == KT - 1))
                                        return ins
                                    S.run("pe", [ab, wb], [pb], mm)
                                    epi(p[:cs, :ts], n0 + c0, g0 + t0, cs, ts, pb)

    def store_epi(self, st, dst, dbuf, dtype, row_off=0, col_off=0, func=None, scale=None, bias=None):
        nc, S = self.nc, self.S

        def epi(p, r0, c0, rs, cs, pb):
            t, tb = st.next()
            o = t[:rs, :cs]
            if func is not None or bias is not None:
                kw = {}
                if bias is not None:
                    kw["bias"] = bias(r0, rs)
                S.run("act", [pb] + ([self.b_bias] if bias is not None else []), [tb],
                      lambda: nc.scalar.activation(out=o, in_=p, func=func or AF.Identity,
                                                   scale=1.0 if scale is None else scale, **kw))
            elif scale is not None:
                self.k += 1
                if self.k % 2 == 0:
                    S.run("act", [pb], [tb], lambda: nc.scalar.mul(out=o, in_=p, mul=scale))
                else:
                    S.run("dve", [pb], [tb], lambda: nc.vector.tensor_scalar(out=o, in0=p, scalar1=scale, scalar2=None, op0=ALU.mult))
            else:
                self.evac([pb], [tb], o, p)
            S.dma("sp", dst[row_off + r0:row_off + r0 + rs, col_off + c0:col_off + c0 + cs], o, [tb], [dbuf])
        return epi

    def norm_T(self, name, src, sbuf_src, T, F, gain_of_tile, shift_of_tile, dst, dbuf, gbufs, src_row0=0):
        nc, S = self.nc, self.S
        FT = F // 128
        with self.scope() as es:
            x_rot = self.rot_sb(es, f"{name}_x", [128, F], F32, 2)
            junk = self.sb(es, f"{name}_junk", [128, F], BF16)
            b_junk = Buf("junk")
            xn_rot = self.rot_sb(es, f"{name}_xn", [128, F], BF16, 2)
            o_rot = self.rot_sb(es, f"{name}_o", [128, FT, 128], BF16, 2)
            st_rot = self.rot_sb(es, f"{name}_st", [128, 2], F32, 2)
            p_rot = self.rot_ps(es, f"{name}_pt", [128, 8, 128], BF16, 3)
            dstv = dst.rearrange("(kt p) t -> p kt t", p=128)
            for ti, t0 in enumerate(range(0, T, 128)):
                ts = min(128, T - t0)
                x, xb = x_rot.next()
                S.dma("sp", x[:ts, :], src[src_row0 + t0:src_row0 + t0 + ts, :], [sbuf_src] if sbuf_src else [], [xb])
                st, stb = st_rot.next()
                S.run("dve", [], [stb], lambda: nc.vector.memset(st[:, :], 0.0))
                S.run("act", [xb], [b_junk, stb], lambda: nc.scalar.activation(
                    out=junk[:ts, :], in_=x[:ts, :], func=AF.Square, accum_out=st[:ts, 0:1]))
                S.run("act", [stb], [stb], lambda: nc.scalar.activation(
                    out=st[:ts, 1:2], in_=st[:ts, 0:1], func=AF.Sqrt, bias=EPS, scale=1.0 / F))
                S.run("dve", [stb], [stb], lambda: nc.vector.reciprocal(out=st[:ts, 1:2], in_=st[:ts, 1:2]))
                xn, xnb = xn_rot.next()
                g = gain_of_tile(ti)
                sh = shift_of_tile(ti) if shift_of_tile else None
                if sh is None:
                    S.run("dve", [xb, stb] + gbufs, [xnb], lambda: nc.vector.scalar_tensor_tensor(
                        out=xn[:ts, :], in0=x[:ts, :], scalar=st[:ts, 1:2], in1=g[:ts, :], op0=ALU.mult, op1=ALU.mult))
                else:
                    S.run("dve", [xb, stb] + gbufs, [xb], lambda: nc.vector.scalar_tensor_tensor(
                        out=x[:ts, :], in0=x[:ts, :], scalar=st[:ts, 1:2], in1=g[:ts, :], op0=ALU.mult, op1=ALU.mult))
                    S.run("dve", [xb] + gbufs, [xnb], lambda: nc.vector.tensor_tensor(
                        out=xn[:ts, :], in0=x[:ts, :], in1=sh[:ts, :], op=ALU.add))
                o, ob = o_rot.next()
                for g0 in range(0, FT, 8):
                    gn = min(8, FT - g0)
                    p, pb = p_rot.next()

                    def tr(p=p, g0=g0, gn=gn):
                        for j in range(gn):
                            ins = nc.tensor.transpose(out=p[:, j, :ts], in_=xn[:ts, (g0 + j) * 128:(g0 + j + 1) * 128],
                                                      identity=self.idb[:ts, :ts])
                        return ins
                    S.run("pe", [xnb, self.b_id], [pb], tr)
                    self.evac([pb], [ob], o[:, g0:g0 + gn, :ts], p[:, :gn, :ts])
                S.dma("sp", dstv[:, :, t0:t0 + ts], o[:, :, :ts], [ob], [dbuf])

    def transpose_pass(self, name, src, sbuf_src, T, F, dst, dbuf, scale_col=None, scale_buf=None):
        nc, S = self.nc, self.S
        FT = F // 128
        with self.scope() as es:
            x_rot = self.rot_sb(es, f"{name}_x", [128, F], BF16, 2)
            o_rot = self.rot_sb(es, f"{name}_o", [128, FT, 128], BF16, 2)
            p_rot = self.rot_ps(es, f"{name}_pt", [128, 8, 128], BF16, 3)
            dstv = dst.rearrange("(kt p) t -> p kt t", p=128)
            for t0 in range(0, T, 128):
                x, xb = x_rot.next()
                S.dma("sp", x[:, :], src[t0:t0 + 128, :], [sbuf_src], [xb])
                o, ob = o_rot.next()
                for g0 in range(0, FT, 8):
                    p, pb = p_rot.next()

                    def tr(p=p, g0=g0):
                        for j in range(8):
                            ins = nc.tensor.transpose(out=p[:, j, :], in_=x[:, (g0 + j) * 128:(g0 + j + 1) * 128],
                                                      identity=self.idb[:, :])
                        return ins
                    S.run("pe", [xb, self.b_id], [pb], tr)
                    if scale_col is None:
                        self.evac([pb], [ob], o[:, g0:g0 + 8, :], p[:, :, :])
                    else:
                        S.run("dve", [pb, scale_buf], [ob], lambda p=p, g0=g0: nc.vector.tensor_tensor(
                            out=o[:, g0:g0 + 8, :], in0=p[:, :, :],
                            in1=scale_col[:, g0:g0 + 8].unsqueeze(2).to_broadcast([128, 8, 128]), op=ALU.mult))
                S.dma("sp", dstv[:, :, t0:t0 + 128], o[:, :, :], [ob], [dbuf])

    def bload(self, t, src_row, bufs_r, buf_w, np_=128):
        self.S.dma("sp", t, src_row.partition_broadcast(np_), bufs_r, [buf_w])

    def phase_mod(self):
        nc, S = self.nc, self.S
        cvec = self.inp("cvec", [2, D])
        w_ada = self.inp("w_ada", [D, 6 * D])
        b_ada = self.inp("b_ada", [1, 6 * D])
        self.modv, self.b_modv = self.scratch("modv", [2, 6 * D], F32)
        with self.scope() as es:
            c32 = self.sb(es, "A_c32", [32, 2, 128], F32)
            b_c = Buf("c32")
            S.dma("sp", c32[:], cvec.rearrange("r (kt p) -> kt r p", p=128), [], [b_c])
            S.run("act", [b_c], [b_c], lambda: nc.scalar.activation(out=c32[:], in_=c32[:], func=AF.Silu))
            cT = self.sb(es, "A_cT", [128, 32, 2], BF16)
            b_cT = Buf("cT")
            with self.scope() as es2:
                pt = self.ps(es2, "A_pt", [128, 2, 32], F32)
                b_pt = Buf("pt")

                def tr():
                    for r in range(2):
                        ins = nc.tensor.transpose(out=pt[:, r, :], in_=c32[:, r, :], identity=self.idf[:32, :32])
                    return ins
                S.run("pe", [b_c, self.b_id], [b_pt], tr)
                S.run("dve", [b_pt], [b_cT], lambda: nc.vector.tensor_copy(
                    out=cT[:].rearrange("p k r -> p r k"), in_=pt[:]))
            bias = self.sb(es, "A_bias", [2, 6 * D], F32)
            b_bias = Buf("bias")
            self.bload(bias[:], b_ada[0:1, :], [], b_bias, 2)
            st = self.rot_sb(es, "A_st", [2, 512], F32, 3)

            def epi(p, r0, c0, rs, cs, pb):
                t, tb = st.next()
                S.run("dve", [pb, b_bias], [tb], lambda: nc.vector.tensor_tensor(
                    out=t[:rs, :cs], in0=p, in1=bias[:rs, c0:c0 + cs], op=ALU.add))
                S.dma("sp", self.modv[0:rs, c0:c0 + cs], t[:rs, :cs], [tb], [self.b_modv])
            self.mm_phase("A", None, None, 2, D, [dict(w=w_ada, N=6 * D, mode="tm", epi=epi)], act_pre=(cT, b_cT))

    def mod_tile(self, es, name, row, chunk, normvec=None, plus1=False):
        nc, S = self.nc, self.S
        t = self.sb(es, name, [128, D], F32)
        b = Buf(name)
        self.bload(t[:], self.modv[row:row + 1, chunk * D:(chunk + 1) * D], [self.b_modv], b)
        if normvec is not None:
            nv, bn = self.nv, self.b_nv
            self.bload(nv[:], normvec[0:1, :], [], bn)
            if plus1:
                S.run("dve", [b, bn], [b], lambda: nc.vector.scalar_tensor_tensor(
                    out=t[:], in0=t[:], scalar=1.0, in1=nv[:], op0=ALU.add, op1=ALU.mult))
            else:
                S.run("dve", [b, bn], [b], lambda: nc.vector.tensor_tensor(out=t[:], in0=t[:], in1=nv[:], op=ALU.mult))
        return t, b

    def phase_h(self):
        xs = self.inp("xs", [NTOK, D])
        n1 = self.inp("norm_pre_mix", [1, D])
        self.hT, self.b_hT = self.scratch("hT", [D, NTOK], BF16)
        with self.scope() as es:
            self.nv, self.b_nv = self.sb(es, "B_nv", [128, D], F32), Buf("nv")
            W1, bW1 = self.mod_tile(es, "B_W1", 0, 1, n1, True)
            SH1, bS1 = self.mod_tile(es, "B_SH1", 0, 0)
            W1c, bW1c = self.mod_tile(es, "B_W1c", 1, 1, n1, True)
            SH1c, bS1c = self.mod_tile(es, "B_SH1c", 1, 0)
            self.norm_T("B", xs, None, NTOK, D, lambda ti: W1c if ti < 2 else W1,
                        lambda ti: SH1c if ti < 2 else SH1, self.hT, self.b_hT, [bW1, bS1, bW1c, bS1c])

    def phase_proj(self):
        nc, S = self.nc, self.S
        w_in = self.inp("w_in", [D, DIN])
        w_krs = self.inp("w_krs", [D, 64])
        b_g = self.inp("b_gates", [1, 32])
        sc = self.scratch
        self.qT, self.b_qT = sc("qT", [2048, NOWN], BF16)
        self.kT, self.b_kT = sc("kT", [2048, NTOK], BF16)
        self.k_tm, self.b_k_tm = sc("k_tm", [NTOK, 2048], BF16)
        self.v_tm, self.b_v_tm = sc("v_tm", [NTOK, 4096], BF16)
        self.so, self.b_so = sc("so", [NOWN, 4096], BF16)
        self.zcq, self.b_zcq = sc("zcq", [NOWN, 1024], F32)
        self.sgaT, self.b_sgaT = sc("sgaT", [D, NOWN], BF16)
        self.sgbT, self.b_sgbT = sc("sgbT", [D, NOWN], BF16)
        self.zgT, self.b_zgT = sc("zgT", [32, NTOK], F32)
        self.zckv, self.b_zckv = sc("zckv", [NTOK, 512], F32)
        self.zkrT, self.b_zkrT = sc("zkrT", [64, NTOK], F32)
        self.zkrsT, self.b_zkrsT = sc("zkrsT", [64, NTOK], F32)
        with self.scope() as es:
            stb = self.rot_sb(es, "C_stb", [128, 512], BF16, 3)
            stf = self.rot_sb(es, "C_stf", [128, 512], F32, 3)
            bg = self.sb(es, "C_bg", [32, 1], F32)
            self.b_bias = Buf("bg")
            S.dma("sp", bg[:], b_g.rearrange("o g -> g o"), [], [self.b_bias])
            E = self.store_epi

            def all_jobs(tok_off):
                return [
                    dict(w=w_in[:, CK:CK + 2048], N=2048, mode="fm", epi=E(stb, self.kT, self.b_kT, BF16, col_off=tok_off, scale=0.0625)),
                    dict(w=w_in[:, CK:CK + 2048], N=2048, mode="tm", epi=E(stb, self.k_tm, self.b_k_tm, BF16, row_off=tok_off, scale=0.0625)),
                    dict(w=w_in[:, CV:CV + 4096], N=4096, mode="tm", epi=E(stb, self.v_tm, self.b_v_tm, BF16, row_off=tok_off)),
                    dict(w=w_in[:, CG:CG + 32], N=32, mode="fm", epi=E(stf, self.zgT, self.b_zgT, F32, col_off=tok_off,
                                                                     bias=lambda r0, rs: bg[r0:r0 + rs, 0:1])),
                    dict(w=w_in[:, CCKV:CCKV + 512], N=512, mode="tm", epi=E(stf, self.zckv, self.b_zckv, F32, row_off=tok_off)),
                    dict(w=w_in[:, CKR:CKR + 64], N=64, mode="fm", epi=E(stf, self.zkrT, self.b_zkrT, F32, col_off=tok_off)),
                    dict(w=w_krs, N=64, mode="fm", epi=E(stf, self.zkrsT, self.b_zkrsT, F32, col_off=tok_off)),
                ]
            own_jobs = [
                dict(w=w_in[:, CQ:CQ + 2048], N=2048, mode="fm", epi=E(stb, self.qT, self.b_qT, BF16)),
                dict(w=w_in[:, CO:CO + 4096], N=4096, mode="tm", epi=E(stb, self.so, self.b_so, BF16, func=AF.Sigmoid)),
                dict(w=w_in[:, CCQ:CCQ + 1024], N=1024, mode="tm", epi=E(stf, self.zcq, self.b_zcq, F32)),
                dict(w=w_in[:, CGA:CGA + 4096], N=4096, mode="fm", epi=E(stb, self.sgaT, self.b_sgaT, BF16, func=AF.Sigmoid)),
                dict(w=w_in[:, CGB:CGB + 4096], N=4096, mode="fm", epi=E(stb, self.sgbT, self.b_sgbT, BF16, func=AF.Sigmoid)),
            ]
            self.mm_phase("C0", self.hT[:, 0:OWN0], self.b_hT, OWN0, D, all_jobs(0))
            self.mm_phase("C1", self.hT[:, OWN0:OWN1], self.b_hT, NOWN, D, all_jobs(OWN0) + own_jobs)
            self.mm_phase("C2", self.hT[:, OWN1:NTOK], self.b_hT, NTOK - OWN1, D, all_jobs(OWN1))


DEINT = list(range(0, 64, 2)) + list(range(1, 64, 2))
SWAPI = list(range(1, 64, 2)) + list(range(0, 64, 2))
_cache = {}


def core_inputs(inputs, c, names):
    b, half = c // 2, c % 2
    out = {}
    f32 = np.float32

    def shared(key, fn):
        if key not in _cache:
            _cache[key] = np.ascontiguousarray(fn(), dtype=f32)
        return _cache[key]

    for n in names:
        if n == "xs":
            xc, xx = inputs["ctx"][b], inputs["x"][b]
            if half:
                xc, xx = xc[::-1], xx[::-1]
            out[n] = np.ascontiguousarray(np.concatenate([xc, xx], 0), dtype=f32)
        elif n == "cvec":
            out[n] = np.ascontiguousarray(np.stack([inputs["c"][b], inputs["c_ctx"]], 0), dtype=f32)
        elif n == "posinfo":
            out[n] = np.array([[2047.0, -1.0]] if half else [[0.0, 1.0]], f32)
        elif n == "w_in":
            def mk(half=half):
                w = np.array(inputs["w_in"][0], dtype=f32)
                if half:
                    w[:, CG:CG + 32] = np.concatenate([w[:, CG + 16:CG + 32], w[:, CG:CG + 16]], 1)
                w[:, CKR:CKR + 64] = w[:, CKR:CKR + 64][:, DEINT]
                return w
            out[n] = shared(("w_in", half), mk)
        elif n == "w_krs":
            out[n] = shared("w_krs", lambda: inputs["w_in"][0][:, CKR:CKR + 64][:, SWAPI])
        elif n == "b_gates":
            g = inputs["b_gates"][0]
            if half:
                g = np.concatenate([g[16:32], g[0:16]])
            out[n] = np.ascontiguousarray(g[None, :], dtype=f32)
        elif n in ("w_uqn", "w_uqr", "w_uqs"):
            w3 = inputs["w_uq"][0].reshape(1024, 32, 192)
            if n == "w_uqn":
                out[n] = shared(n, lambda: w3[:, :, :128].reshape(1024, 4096))
            else:
                idx = DEINT if n == "w_uqr" else SWAPI
                out[n] = shared(n, lambda: w3[:, :, 128:][:, :, idx].reshape(1024, 2048))
        elif n in ("w_ukn", "w_ukvv"):
            w3 = inputs["w_ukv"][0].reshape(512, 32, 256)
            out[n] = shared(n, lambda: (w3[:, :, :128] if n == "w_ukn" else w3[:, :, 128:]).reshape(512, 4096))
        elif n == "w_uq":
            def mk():
                w = np.array(inputs["w_uq"][0], dtype=f32).reshape(1024, 32, 192)
                w[:, :, 128:] = w[:, :, 128:][:, :, DEINT]
                return w.reshape(1024, 6144)
            out[n] = shared("w_uq", mk)
        elif n == "w_uqs":
            out[n] = shared("w_uqs", lambda: inputs["w_uq"][0].reshape(1024, 32, 192)[:, :, 128:][:, :, SWAPI].reshape(1024, 2048))
        elif n == "w_gu":
            def mk():
                w = inputs["w_gu"][0].reshape(NE, D, 2, 12, 128)
                return np.ascontiguousarray(w.transpose(0, 1, 3, 2, 4)).reshape(NE, D, 3072)
            out[n] = shared("w_gu", mk)
        elif n == "b_gu":
            out[n] = shared("b_gu", lambda: inputs["b_gu"][0].reshape(NE, 2, 12, 128).transpose(0, 2, 1, 3).reshape(NE, 3072))
        elif n in ("w_down", "b_down"):
            out[n] = shared(n, lambda n=n: inputs[n][0])
        elif n in ("b_ada", "norm_pre_mix", "norm_post_mix", "norm_pre_ffn", "norm_post_ffn", "m_out_norm",
                   "q_norm", "kv_norm", "router_b"):
            out[n] = shared(n, lambda n=n: np.asarray(inputs[n]).reshape(1, -1))
        else:
            out[n] = shared(n, lambda n=n: inputs[n][0])
    return out


def _scan(P, X, Y, bx, by, pr, lo, hi, desc, op):
    nc, S = P.nc, P.S
    n = hi - lo
    s = 1
    src, dst, bs, bd = X, Y, bx, by
    while s < n:
        if not desc:
            a_out, a_in0, a_in1 = (lo + s, hi), (lo + s, hi), (lo, hi - s)
            c_rng = (lo, lo + s)
        else:
            a_out, a_in0, a_in1 = (lo, hi - s), (lo, hi - s), (lo + s, hi)
            c_rng = (hi - s, hi)
        S.run("dve", [bs], [bd], lambda src=src, dst=dst: nc.vector.tensor_tensor(
            out=dst[pr, a_out[0]:a_out[1]], in0=src[pr, a_in0[0]:a_in0[1]], in1=src[pr, a_in1[0]:a_in1[1]], op=op))
        S.run("act", [bs], [bd], lambda src=src, dst=dst: nc.scalar.copy(
            out=dst[pr, c_rng[0]:c_rng[1]], in_=src[pr, c_rng[0]:c_rng[1]]))
        src, dst, bs, bd = dst, src, bd, bs
        s *= 2
    if src is not X:
        S.run("dve", [by], [bx], lambda: nc.vector.tensor_copy(out=X[pr, lo:hi], in_=Y[pr, lo:hi]))


def phase_mlstm(P, d1_only=False):
    nc, S = P.nc, P.S
    R = 40
    P1, P2 = slice(0, 8), slice(32, 40)
    NCH = NTOK // 64
    P.h1, P.b_h1 = P.scratch("h1", [NOWN, 4096], F32)
    P.ya_tm, P.b_ya_tm = P.scratch("ya_tm", [NOWN, 4096], BF16)
    with P.scope() as es:
        sb = lambda n, s, d=F32: P.sb(es, "M_" + n, s, d)
        A = sb("A", [R, NTOK]); NEGMU = sb("NEGMU", [R, NTOK]); SCL = sb("SCL", [R, NTOK])
        WKtm = sb("WKtm", [64, NCH, R]); EMTtm = sb("EMTtm", [64, NCH, R]); DEC = sb("DEC", [R, NCH])
        MASKD = sb("MASKD", [R, 8, 64]); NM = [sb("NM1", [64, 8, 64]), sb("NM2", [64, 8, 64])]
        bA, bNEGMU, bSCL, bWK, bEMT, bDEC, bMASKD, bNM = (Buf(n) for n in "A NEGMU SCL WK EMT DEC MASKD NM".split())
        with P.scope() as es2:
            t2 = lambda n, s, d=F32: P.sb(es2, "G_" + n, s, d)
            I = t2("I", [R, NTOK]); X = t2("X", [R, NTOK]); Y = t2("Y", [R, NTOK]); Bt = t2("B", [R, NTOK])
            MU = t2("MU", [R, NTOK]); T3 = t2("T3", [R, NTOK]); ME = t2("ME", [R, NCH]); MP = t2("MP", [R, NCH])
            bI, bX, bY, bB, bMU, bT3, bME, bMP = (Buf(n) for n in "I X Y B MU T3 ME MP".split())

            def z():
                for t in (I, X, Y, A, NEGMU, SCL, MU, T3, MASKD, NM[0]):
                    nc.gpsimd.memset(t[:], 0.0)
                return nc.gpsimd.memset(NM[1][:], 0.0)
            S.run("pool", [], [bI, bX, bY, bA, bNEGMU, bSCL, bMU, bT3, bMASKD, bNM], z)
            S.run("pool", [bNM], [bNM], lambda: nc.gpsimd.affine_select(
                out=NM[0][:], in_=NM[0][:], pattern=[[0, 8], [1, 64]], compare_op=ALU.is_ge, fill=-30000.0, base=0, channel_multiplier=-1))
            S.run("pool", [bNM], [bNM], lambda: nc.gpsimd.affine_select(
                out=NM[1][:], in_=NM[1][:], pattern=[[0, 8], [-1, 64]], compare_op=ALU.is_ge, fill=-30000.0, base=0, channel_multiplier=1))
            for pr, off in ((P1, 0), (P2, 32)):
                S.run("dve", [P.b_id, bMASKD], [bMASKD], lambda pr=pr, off=off: nc.vector.tensor_copy(
                    out=MASKD[pr], in_=P.idf[pr, off:off + 8].unsqueeze(2).to_broadcast([8, 8, 64])))
            S.dma("sp", I[P1, :], P.zgT[0:8, :], [P.b_zgT, bI], [bI])
            S.dma("sp", X[P1, :], P.zgT[8:16, :], [P.b_zgT, bX], [bX])
            S.dma("sp", I[P2, :], P.zgT[16:24, :], [P.b_zgT, bI], [bI])
            S.dma("sp", X[P2, :], P.zgT[24:32, :], [P.b_zgT, bX], [bX])
            S.run("act", [bX], [bX], lambda: nc.scalar.activation(out=X[:], in_=X[:], func=AF.Exp, scale=-1.0))
            S.run("act", [bX], [bX], lambda: nc.scalar.activation(out=X[:], in_=X[:], func=AF.Ln, bias=1.0, scale=1.0))
            S.run("dve", [bX], [bX], lambda: nc.vector.tensor_scalar(out=X[:], in0=X[:], scalar1=-1.0, scalar2=None, op0=ALU.mult))
            _scan(P, X, Y, bX, bY, P1, 0, OWN1, False, ALU.add)
            _scan(P, X, Y, bX, bY, P2, 0, OWN0, True, ALU.add)
            _scan(P, X, Y, bX, bY, P2, OWN0, NTOK, True, ALU.add)
            S.run("dve", [bX], [bX], lambda: nc.vector.tensor_scalar(
                out=X[P2, OWN0:NTOK], in0=X[P2, OWN0:NTOK], scalar1=X[P2, 0:1], scalar2=None, op0=ALU.add))
            S.run("dve", [bX], [bB], lambda: nc.vector.tensor_copy(out=Bt[:], in_=X[:]))
            S.run("dve", [bI, bB], [bA], lambda: nc.vector.tensor_tensor(out=A[:], in0=I[:], in1=Bt[:], op=ALU.subtract))
            S.run("dve", [bA], [bX], lambda: nc.vector.tensor_copy(out=X[:], in_=A[:]))
            _scan(P, X, Y, bX, bY, P1, 0, OWN1, False, ALU.max)
            _scan(P, X, Y, bX, bY, P2, 0, OWN0, True, ALU.max)
            _scan(P, X, Y, bX, bY, P2, OWN0, NTOK, True, ALU.max)
            S.run("dve", [bX], [bX], lambda: nc.vector.tensor_scalar(out=X[:], in0=X[:], scalar1=0.0, scalar2=None, op0=ALU.max))
            S.run("dve", [bX], [bX], lambda: nc.vector.tensor_scalar(
                out=X[P2, OWN0:NTOK], in0=X[P2, OWN0:NTOK], scalar1=X[P2, 0:1], scalar2=None, op0=ALU.max))
            S.run("dve", [bX], [bMU], lambda: nc.vector.tensor_copy(out=MU[:], in_=X[:]))
            S.run("dve", [bMU], [bNEGMU], lambda: nc.vector.tensor_scalar(out=NEGMU[:], in0=MU[:], scalar1=-1.0, scalar2=None, op0=ALU.mult))
            MUv = MU[:].rearrange("p (c j) -> p c j", j=64)
            S.run("dve", [bMU], [bME], lambda: nc.vector.tensor_copy(out=ME[:], in_=MUv[:, :, 0]))
            S.run("dve", [bMU, bME], [bME], lambda: nc.vector.tensor_copy(out=ME[P1, :], in_=MUv[P1, :, 63]))

            def mp():
                nc.vector.memset(MP[:], 0.0)
                nc.vector.tensor_copy(out=MP[P1, 1:NCH], in_=ME[P1, 0:NCH - 1])
                nc.vector.tensor_copy(out=MP[P2, 0:NCH - 1], in_=ME[P2, 1:NCH])
                nc.vector.memset(MP[P2, 3:4], 0.0)
                return nc.vector.tensor_copy(out=MP[P2, NCH - 1:NCH], in_=ME[P2, 0:1])
            S.run("dve", [bME], [bMP], mp)
            S.run("dve", [bMP, bMU], [bT3], lambda: nc.vector.tensor_tensor(
                out=T3[:].rearrange("p (c j) -> p c j", j=64), in0=MP[:].unsqueeze(2).to_broadcast([R, NCH, 64]), in1=MUv, op=ALU.subtract))
            S.run("act", [bT3], [bSCL], lambda: nc.scalar.activation(out=SCL[:], in_=T3[:], func=AF.Exp))
            S.run("dve", [bMP, bME], [bDEC], lambda: nc.vector.tensor_tensor(out=DEC[:], in0=MP[:], in1=ME[:], op=ALU.subtract))
            S.run("act", [bDEC], [bDEC], lambda: nc.scalar.activation(out=DEC[:], in_=DEC[:], func=AF.Exp))
            S.run("dve", [bA, bME], [bX], lambda: nc.vector.tensor_tensor(
                out=X[:].rearrange("p (c j) -> p c j", j=64), in0=A[:].rearrange("p (c j) -> p c j", j=64),
                in1=ME[:].unsqueeze(2).to_broadcast([R, NCH, 64]), op=ALU.subtract))
            S.run("act", [bX], [bX], lambda: nc.scalar.activation(out=X[:], in_=X[:], func=AF.Exp))
            S.run("dve", [bB, bMU], [bY], lambda: nc.vector.tensor_tensor(out=Y[:], in0=Bt[:], in1=MU[:], op=ALU.add))
            S.run("act", [bY], [bY], lambda: nc.scalar.activation(out=Y[:], in_=Y[:], func=AF.Exp, scale=-1.0))
            pt_rot = P.rot_ps(es2, "G_pt", [64, 12, R], F32, 2)
            for src, bsrc, dst, bdst in ((X, bX, WKtm, bWK), (Y, bY, EMTtm, bEMT)):
                for c0 in range(0, NCH, 12):
                    pt, bpt = pt_rot.next()

                    def tr(pt=pt, c0=c0, src=src):
                        for j in range(12):
                            ins = nc.tensor.transpose(out=pt[:, j, :], in_=src[:, (c0 + j) * 64:(c0 + j + 1) * 64],
                                                      identity=P.idf[:R, :R])
                        return ins
                    S.run("pe", [bsrc, P.b_id], [bpt], tr)
                    P.evac([bpt], [bdst], dst[:, c0:c0 + 12, :], pt[:])
        if d1_only:
            dbg, bdbg = P.scratch("d1dbg", [R, 3 * NTOK + NCH], F32)
            S.dma("sp", dbg[:, 0:NTOK], A[:], [bA], [bdbg])
            S.dma("sp", dbg[:, NTOK:2 * NTOK], NEGMU[:], [bNEGMU], [bdbg])
            S.dma("sp", dbg[:, 2 * NTOK:3 * NTOK], SCL[:], [bSCL], [bdbg])
            S.dma("sp", dbg[:, 3 * NTOK:3 * NTOK + NCH], DEC[:], [bDEC], [bdbg])
            dbg2, bdbg2 = P.scratch("d1dbg2", [64, 2 * NCH * R], F32)
            S.dma("sp", dbg2[:, 0:NCH * R], WKtm[:].rearrange("p c r -> p (c r)"), [bWK], [bdbg2])
            S.dma("sp", dbg2[:, NCH * R:], EMTtm[:].rearrange("p c r -> p (c r)"), [bEMT], [bdbg2])
            P.b_d1dbg, P.b_d1dbg2 = bdbg, bdbg2
            return
        CT = sb("CT", [128, 8, 2, 512]); CTb = sb("CTb", [128, 8, 2, 512], BF16)
        NT = sb("NT", [128, 8, 2]); NTb = sb("NTb", [128, 8, 2, 2], BF16)
        bCT, bCTb, bNT, bNTb = Buf("CT"), Buf("CTb"), Buf("NT"), Buf("NTb")
        kt_rot = P.rot_sb(es, "M_kt", [64, 2048], BF16, 2)
        v_rot = P.rot_sb(es, "M_v", [64, 8, 512], BF16, 2)
        q_rot = P.rot_sb(es, "M_q", [128, 16, 64], BF16, 2)
        k_rot = P.rot_sb(es, "M_k", [128, 16, 64], BF16, 2)
        h1_rot = P.rot_sb(es, "M_h1", [64, 8, 512], F32, 1)
        so_rot = P.rot_sb(es, "M_so", [64, 8, 512], BF16, 1)
        vs_rot = P.rot_sb(es, "M_vs", [64, 8, 512], BF16, 1)
        hst_rot = P.rot_sb(es, "M_hst", [64, 512], F32, 3)
        R1 = sb("R1", [R, 8, 64]); R2 = sb("R2", [R, 8, 64]); R3 = sb("R3", [R, 8])
        bR1, bR2, bR3 = Buf("R1"), Buf("R2"), Buf("R3")
        WT = sb("WT", [64, 512]); PT = sb("PT", [64, 512], BF16); QS = sb("QS", [128, 16, 64], BF16)
        bWT, bPT, bQS = Buf("WT"), Buf("PT"), Buf("QS")
        RD = sb("RD", [64, 8]); RD2 = sb("RD2", [64, 8]); DECs = sb("DECs", [128, 8]); WKb = sb("WKb", [64, 8, 2], BF16)
        bRD, bDECs, bWKb = Buf("RD"), Buf("DECs"), Buf("WKb")
        HS = sb("HS", [64, 8, 512]); SS = sb("SS", [64, 16]); YA = sb("YA", [64, 4096], BF16)
        junk = sb("junk", [64, 512], BF16)
        bHS, bSS, bYA, bjunk = Buf("HS"), Buf("SS"), Buf("YA"), Buf("junk")
        psD = P.ps(es, "M_psD", [64, 512]); psE = P.ps(es, "M_psE", [128, 512]); psS = P.ps(es, "M_psS", [64, 512])
        psM = P.ps(es, "M_psM", [128, 512])
        bpsD, bpsE, bpsS = Buf("psD"), Buf("psE"), Buf("psS")
        bDEN = bNUPS = bDECp = Buf("psM")
        num_rot = P.rot_ps(es, "M_num", [64, 512], F32, 2)
        upd_rot = P.rot_ps(es, "M_upd", [128, 512], F32, 2)
        qTv = P.qT.rearrange("(g p) t -> p g t", p=128)
        kTv = P.kT.rearrange("(g p) t -> p g t", p=128)
        for d in (0, 1):
            pb = 0 if d == 0 else 32
            pr = slice(pb, pb + 8)
            if d == 0:
                seq = [(c, c >= 4) for c in range(0, 20)]
            else:
                seq = [(c, False) for c in (3, 2, 1, 0)] + [(c, False) for c in range(35, 19, -1)] + [(c, True) for c in range(19, 3, -1)]

            lim = getattr(P, "lim", None)
            if lim is not None:
                if d not in lim["dirs"]:
                    continue
                seq = [x for x in seq if lim["sel"](x)]
            def zs():
                nc.gpsimd.memset(CT[:], 0.0)
                nc.gpsimd.memset(CTb[:], 0.0)
                nc.gpsimd.memset(NT[:], 0.0)
                return nc.gpsimd.memset(NTb[:], 0.0)
            S.run("pool", [], [bCT, bCTb, bNT, bNTb], zs)
            for si, (c, is_out) in enumerate(seq):
                last = si == len(seq) - 1
                n0 = c * 64
                o0 = n0 - OWN0
                cs = slice(n0, n0 + 64)
                KTc, bKT = kt_rot.next()
                Vc, bV = v_rot.next()
                S.dma("sp", KTc[:], P.k_tm[n0:n0 + 64, :], [P.b_k_tm], [bKT])
                S.dma("sp", Vc[:].rearrange("p h v -> p (h v)"), P.v_tm[n0:n0 + 64, :], [P.b_v_tm], [bV])
                if is_out:
                    Qc, bQ = q_rot.next()
                    Kc, bK = k_rot.next()
                    S.dma("sp", Qc[:], qTv[:, :, o0:o0 + 64], [P.b_qT], [bQ])
                    S.dma("sp", Kc[:], kTv[:, :, n0:n0 + 64], [P.b_kT], [bK])
                    if d == 1:
                        H1c, bH1 = h1_rot.next()
                        SOc, bSO = so_rot.next()
                        S.dma("sp", H1c[:].rearrange("p h v -> p (h v)"), P.h1[o0:o0 + 64, :], [P.b_h1], [bH1])
                        S.dma("sp", SOc[:].rearrange("p h v -> p (h v)"), P.so[o0:o0 + 64, :], [P.b_so], [bSO])
                    S.run("dve", [bNEGMU, bMASKD], [bR1], lambda: nc.vector.tensor_tensor(
                        out=R1[pr], in0=NEGMU[pr, cs].unsqueeze(1).to_broadcast([8, 8, 64]), in1=MASKD[pr], op=ALU.mult))
                    S.run("dve", [bSCL, bMASKD], [bR2], lambda: nc.vector.tensor_tensor(
                        out=R2[pr], in0=SCL[pr, cs].unsqueeze(1).to_broadcast([8, 8, 64]), in1=MASKD[pr], op=ALU.mult))

                    def dmm():
                        nc.tensor.matmul(psD[:, :], lhsT=P.onesf[pr, 0:64], rhs=R1[pr].rearrange("p h t -> p (h t)"), start=True, stop=False)
                        nc.tensor.matmul(psD[:, :], lhsT=A[pr, cs], rhs=MASKD[pr].rearrange("p h t -> p (h t)"), start=False, stop=False)
                        return nc.tensor.matmul(psD[:, :], lhsT=P.idf[0:64, 0:64], rhs=NM[d][:].rearrange("p h t -> p (h t)"), start=False, stop=True)
                    S.run("pe", [bR1, bA, bMASKD, bNM, P.b_id], [bpsD], dmm)
                    S.run("act", [bpsD], [bWT], lambda: nc.scalar.activation(out=WT[:], in_=psD[:], func=AF.Exp))
                    S.run("pe", [bR2, P.b_id], [bpsE], lambda: nc.tensor.matmul(
                        psE[:, :], lhsT=P.onesf[pr, 0:128], rhs=R2[pr].rearrange("p h t -> p (h t)"), start=True, stop=True))
                    S.run("dve", [bpsE, bQ], [bQS], lambda: nc.vector.tensor_tensor(
                        out=QS[:].rearrange("p (h k) t -> p h k t", k=2), in0=Qc[:].rearrange("p (h k) t -> p h k t", k=2),
                        in1=psE[:].rearrange("p (h t) -> p h t", t=64).unsqueeze(2).to_broadcast([128, 8, 2, 64]), op=ALU.mult))

                    def smm():
                        for h in range(8):
                            for kc in range(2):
                                ins = nc.tensor.matmul(psS[:, h * 64:(h + 1) * 64], lhsT=Kc[:, h * 2 + kc, :], rhs=Qc[:, h * 2 + kc, :],
                                                       start=(kc == 0), stop=(kc == 1))
                        return ins
                    S.run("pe", [bK, bQ], [bpsS], smm)
                    S.run("dve", [bpsS, bWT], [bPT], lambda: nc.vector.tensor_tensor(out=PT[:], in0=psS[:], in1=WT[:], op=ALU.mult))

                    def denmm():
                        for h in range(8):
                            nc.tensor.matmul(psM[0:64, 2 * h:2 * h + 2], lhsT=PT[:, h * 64:(h + 1) * 64], rhs=P.onesb[0:64, 0:2], start=True, stop=False)
                            for kc in range(2):
                                ins = nc.tensor.matmul(psM[0:64, 2 * h:2 * h + 2], lhsT=QS[:, h * 2 + kc, :], rhs=NTb[:, h, kc, :],
                                                       start=False, stop=(kc == 1))
                        return ins
                    S.run("pe", [bPT, bQS, bNTb], [bDEN], denmm)

                    S.run("dve", [bDEN], [bRD], lambda: nc.vector.tensor_copy(out=RD2[:], in_=psM[0:64, 0:16].rearrange("p (h t) -> p h t", t=2)[:, :, 0]))
                    S.run("dve", [bRD], [bRD], lambda: nc.vector.scalar_tensor_tensor(
                        out=RD[:], in0=RD2[:], scalar=-1.0, in1=RD2[:], op0=ALU.mult, op1=ALU.max))
                    S.run("dve", [bRD, bEMT], [bRD], lambda: nc.vector.tensor_tensor(out=RD[:], in0=RD[:], in1=EMTtm[:, c, pr], op=ALU.max))
                    S.run("dve", [bRD], [bRD], lambda: nc.vector.reciprocal(out=RD[:], in_=RD[:]))
                    for h in range(8):
                        pn, bpn = num_rot.next()

                        def nmm(pn=pn, h=h):
                            nc.tensor.matmul(pn[:, :], lhsT=PT[:, h * 64:(h + 1) * 64], rhs=Vc[:, h, :], start=True, stop=False)
                            for kc in range(2):
                                ins = nc.tensor.matmul(pn[:, :], lhsT=QS[:, h * 2 + kc, :], rhs=CTb[:, h, kc, :], start=False, stop=(kc == 1))
                            return ins
                        S.run("pe", [bPT, bV, bQS, bCTb], [bpn], nmm)
                        if d == 0:
                            t, tb = hst_rot.next()
                            S.run("act", [bpn, bRD], [tb], lambda t=t, pn=pn, h=h: nc.scalar.activation(
                                out=t[:], in_=pn[:], func=AF.Copy, scale=RD[:, h:h + 1]))
                            S.dma("sp", P.h1[o0:o0 + 64, h * 512:(h + 1) * 512], t[:], [tb], [P.b_h1])
                        else:
                            S.run("dve", [bpn, bRD, bH1], [bHS], lambda pn=pn, h=h: nc.vector.scalar_tensor_tensor(
                                out=HS[:, h, :], in0=pn[:], scalar=RD[:, h:h + 1], in1=H1c[:, h, :], op0=ALU.mult, op1=ALU.add))
                    if d == 1:
                        S.run("dve", [], [bSS], lambda: nc.vector.memset(SS[:], 0.0))

                        def sq():
                            for h in range(8):
                                ins = nc.scalar.activation(out=junk[:], in_=HS[:, h, :], func=AF.Square, accum_out=SS[:, h:h + 1])
                            return ins
                        S.run("act", [bHS, bSS], [bjunk, bSS], sq)
                        S.run("act", [bSS], [bSS], lambda: nc.scalar.activation(
                            out=SS[:, 8:16], in_=SS[:, 0:8], func=AF.Sqrt, bias=EPS, scale=1.0 / 512))
                        S.run("dve", [bSS], [bSS], lambda: nc.vector.reciprocal(out=SS[:, 8:16], in_=SS[:, 8:16]))
                        S.run("dve", [bHS, bSS], [bHS], lambda: nc.vector.tensor_tensor(
                            out=HS[:], in0=HS[:], in1=SS[:, 8:16].unsqueeze(2).to_broadcast([64, 8, 512]), op=ALU.mult))
                        S.run("dve", [bHS, bSO], [bYA], lambda: nc.vector.tensor_tensor(
                            out=YA[:], in0=HS[:].rearrange("p h v -> p (h v)"), in1=SOc[:].rearrange("p h v -> p (h v)"), op=ALU.mult))
                        S.dma("sp", P.ya_tm[o0:o0 + 64, :], YA[:], [bYA], [P.b_ya_tm])
                if last:
                    continue
                VS, bVS = vs_rot.next()
                S.run("dve", [bV, bWK], [bVS], lambda: nc.vector.tensor_tensor(
                    out=VS[:], in0=Vc[:], in1=WKtm[:, c, pr].unsqueeze(2).to_broadcast([64, 8, 512]), op=ALU.mult))
                S.run("dve", [bWK], [bWKb], lambda: nc.vector.tensor_copy(out=WKb[:], in_=WKtm[:, c, pr].unsqueeze(2).to_broadcast([64, 8, 2])))
                stage = (lim or {}).get("stage", 99)
                if stage < 2:
                    continue
                S.run("dve", [P.b_id, bDEC], [bR3], lambda: nc.vector.tensor_scalar(
                    out=R3[pr], in0=P.idf[pr, pb:pb + 8], scalar1=DEC[pr, c:c + 1], scalar2=None, op0=ALU.mult))
                S.run("pe", [bR3, P.b_id], [bDECp], lambda: nc.tensor.matmul(
                    psM[:, 32:40], lhsT=P.onesf[pr, 0:128], rhs=R3[pr], start=True, stop=True))
                S.run("act", [bDECp], [bDECs], lambda: nc.scalar.copy(out=DECs[:], in_=psM[:, 32:40]))

                if stage < 3:
                    continue

                def numm():
                    for h in range(8):
                        for kc in range(2):
                            ins = nc.tensor.matmul(psM[:, 64 + (h * 2 + kc) * 2:66 + (h * 2 + kc) * 2], lhsT=KTc[:, h * 256 + kc * 128:h * 256 + (kc + 1) * 128],
                                                   rhs=WKb[:, h, :], start=True, stop=True)
                    return ins
                S.run("pe", [bKT, bWKb], [bNUPS], numm)

                if stage < 4:
                    continue
                S.run("dve", [bDECs, bNTb], [bNT], lambda: nc.vector.tensor_tensor(
                    out=NT[:], in0=NT[:], in1=DECs[:].unsqueeze(2).to_broadcast([128, 8, 2]), op=ALU.mult))
                S.run("dve", [bNUPS, bNT], [bNT], lambda: nc.vector.tensor_tensor(
                    out=NT[:], in0=NT[:], in1=psM[:, 64:96].rearrange("p (h k t) -> p h k t", k=2, t=2)[:, :, :, 0], op=ALU.add))
                S.run("dve", [bNT], [bNTb], lambda: nc.vector.tensor_copy(out=NTb[:], in_=NT[:].unsqueeze(3).to_broadcast([128, 8, 2, 2])))
                if stage < 5:
                    continue
                for h in range(8):
                    for kc in range(2):
                        pu, bpu = upd_rot.next()
                        S.run("pe", [bKT, bVS], [bpu], lambda pu=pu, h=h, kc=kc: nc.tensor.matmul(
                            pu[:, :], lhsT=KTc[:, h * 256 + kc * 128:h * 256 + (kc + 1) * 128], rhs=VS[:, h, :], start=True, stop=True))
                        S.run("dve", [bpu, bDECs, bCTb], [bCT], lambda pu=pu, h=h, kc=kc: nc.vector.scalar_tensor_tensor(
                            out=CT[:, h, kc, :], in0=CT[:, h, kc, :], scalar=DECs[:, h:h + 1], in1=pu[:], op0=ALU.mult, op1=ALU.add))
                        S.run("act", [bCT], [bCTb], lambda h=h, kc=kc: nc.scalar.copy(out=CTb[:, h, kc, :], in_=CT[:, h, kc, :]))


Prog.phase_mlstm = phase_mlstm


def small_T(P, es, name, vec, n):
    nc, S = P.nc, P.S
    t = P.sb(es, name, [128, n], F32)
    b = Buf(name)
    with P.scope() as es2:
        r = P.sb(es2, name + "_r", [n, 128], F32)
        br = Buf(name + "r")
        S.dma("sp", r[:], vec.rearrange("o (k p) -> (o k) p", p=128), [], [br])
        pt = P.ps(es2, name + "_pt", [128, n], F32)
        bpt = Buf(name + "pt")
        S.run("pe", [br, P.b_id], [bpt], lambda: nc.tensor.transpose(out=pt[:, :], in_=r[:, :], identity=P.idf[:n, :n]))
        S.run("dve", [bpt], [b], lambda: nc.vector.tensor_copy(out=t[:], in_=pt[:]))
    return t, b


def phase_mla(P):
    nc, S = P.nc, P.S
    sc = P.scratch
    P.cqT, P.b_cqT = sc("cqT", [1024, NOWN], BF16)
    P.ckvT, P.b_ckvT = sc("ckvT", [512, NTOK], BF16)
    P.qnT, P.b_qnT = sc("qnT", [4096, NOWN], BF16)
    P.qrA, P.b_qrA = sc("qrA", [2048, NOWN], F32)
    P.qrB, P.b_qrB = sc("qrB", [2048, NOWN], F32)
    P.knT, P.b_knT = sc("knT", [4096, NTOK], BF16)
    P.v_a, P.b_v_a = sc("v_a", [NTOK, 4096], BF16)
    P.ybT, P.b_ybT = sc("ybT", [4096, NOWN], BF16)
    q_norm = P.inp("q_norm", [1, 1024]); kv_norm = P.inp("kv_norm", [1, 512])
    w_uqn = P.inp("w_uqn", [1024, 4096]); w_uqr = P.inp("w_uqr", [1024, 2048]); w_uqs = P.inp("w_uqs", [1024, 2048])
    w_ukn = P.inp("w_ukn", [512, 4096]); w_ukv = P.inp("w_ukvv", [512, 4096])
    posinfo = P.inp("posinfo", [1, 2])
    with P.scope() as es:
        QN = P.sb(es, "E_QN", [128, 1024]); bQN = Buf("QN")
        P.bload(QN[:], q_norm[0:1, :], [], bQN)
        P.norm_T("E1", P.zcq, P.b_zcq, NOWN, 1024, lambda ti: QN, None, P.cqT, P.b_cqT, [bQN])
    with P.scope() as es:
        KVN = P.sb(es, "E_KVN", [128, 512]); bKVN = Buf("KVN")
        P.bload(KVN[:], kv_norm[0:1, :], [], bKVN)
        P.norm_T("E2", P.zckv, P.b_zckv, NTOK, 512, lambda ti: KVN, None, P.ckvT, P.b_ckvT, [bKVN])
    with P.scope() as es:
        stb = P.rot_sb(es, "E_stb", [128, 512], BF16, 3)
        stf = P.rot_sb(es, "E_stf", [128, 512], F32, 3)
        E = P.store_epi
        P.mm_phase("E4", P.cqT, P.b_cqT, NOWN, 1024, [
            dict(w=w_uqn, N=4096, mode="fm", epi=E(stb, P.qnT, P.b_qnT, BF16)),
            dict(w=w_uqr, N=2048, mode="fm", epi=E(stf, P.qrA, P.b_qrA, F32)),
            dict(w=w_uqs, N=2048, mode="fm", epi=E(stf, P.qrB, P.b_qrB, F32))])
        P.mm_phase("E5", P.ckvT, P.b_ckvT, NTOK, 512, [
            dict(w=w_ukn, N=4096, mode="fm", epi=E(stb, P.knT, P.b_knT, BF16)),
            dict(w=w_ukv, N=4096, mode="tm", epi=E(stb, P.v_a, P.b_v_a, BF16))], TG=1152)
    with P.scope() as es:
        sb = lambda n, s, d=F32: P.sb(es, "R_" + n, s, d)
        CC = sb("CC", [64, NTOK]); SSn = sb("SS", [64, NTOK]); KR = sb("KR", [64, NTOK], BF16)
        bCC, bSS, bKR = Buf("CC"), Buf("SS"), Buf("KR")
        with P.scope() as es2:
            t2 = lambda n, s, d=F32: P.sb(es2, "RT_" + n, s, d)
            ii = t2("ii", [64, 2048], I32); tf = t2("tf", [64, 2048]); rw = t2("rw", [64, 2048]); cl = t2("cl", [64, 2048])
            ki = t2("ki", [64, 2048], I32); ang = t2("ang", [64, 2048]); tmp = t2("tmp", [64, 2048])
            pi_ = t2("pi", [64, 2]); pp = t2("pp", [64, 8]); ppi = t2("ppi", [64, 2], I32)
            b = {n: Buf(n) for n in "ii tf rw cl ki ang tmp pi pp ppi".split()}
            P.bload(pi_[:], posinfo[0:1, :], [], b["pi"], 64)
            S.run("pool", [], [b["ii"]], lambda: nc.gpsimd.iota(ii[:], pattern=[[1, 2048]], base=0, channel_multiplier=0))
            S.run("pool", [], [b["ppi"]], lambda: nc.gpsimd.iota(ppi[:, 0:1], pattern=[[0, 1]], base=0, channel_multiplier=1))
            R_ = lambda e, r, w, f: S.run(e, [b[x] for x in r], [b[x] for x in w], f)
            R_("dve", ["ii"], ["tf"], lambda: nc.vector.tensor_copy(out=tf[:], in_=ii[:]))
            R_("dve", ["tf", "pi"], ["tf"], lambda: nc.vector.tensor_scalar(out=tf[:], in0=tf[:], scalar1=pi_[:, 1:2], scalar2=pi_[:, 0:1], op0=ALU.mult, op1=ALU.add))
            R_("dve", ["tf"], ["tmp"], lambda: nc.vector.tensor_scalar(out=tmp[:], in0=tf[:], scalar1=-31.5, scalar2=1.0 / 64, op0=ALU.add, op1=ALU.mult))
            R_("dve", ["tmp"], ["ki"], lambda: nc.vector.tensor_copy(out=ki[:], in_=tmp[:]))
            R_("dve", ["ki"], ["rw"], lambda: nc.vector.tensor_copy(out=rw[:], in_=ki[:]))
            R_("dve", ["rw", "tf"], ["cl"], lambda: nc.vector.scalar_tensor_tensor(out=cl[:], in0=rw[:], scalar=-64.0, in1=tf[:], op0=ALU.mult, op1=ALU.add))
            R_("dve", ["ppi"], ["pp"], lambda: nc.vector.tensor_copy(out=pp[:, 0:1], in_=ppi[:, 0:1]))
            R_("dve", ["pp"], ["pp"], lambda: nc.vector.tensor_scalar(out=pp[:, 1:2], in0=pp[:, 0:1], scalar1=-15.5, scalar2=1.0 / 32, op0=ALU.add, op1=ALU.mult))
            R_("dve", ["pp"], ["ppi"], lambda: nc.vector.tensor_copy(out=ppi[:, 1:2], in_=pp[:, 1:2]))
            R_("dve", ["ppi"], ["pp"], lambda: nc.vector.tensor_copy(out=pp[:, 1:2], in_=ppi[:, 1:2]))
            R_("dve", ["pp"], ["pp"], lambda: nc.vector.scalar_tensor_tensor(out=pp[:, 2:3], in0=pp[:, 1:2], scalar=-32.0, in1=pp[:, 0:1], op0=ALU.mult, op1=ALU.add))
            R_("dve", ["pp"], ["pp"], lambda: nc.vector.tensor_scalar(out=pp[:, 3:4], in0=pp[:, 2:3], scalar1=15.5, scalar2=None, op0=ALU.is_lt))
            R_("dve", ["pp"], ["pp"], lambda: nc.vector.scalar_tensor_tensor(out=pp[:, 4:5], in0=pp[:, 3:4], scalar=16.0, in1=pp[:, 2:3], op0=ALU.mult, op1=ALU.add))
            R_("dve", ["pp"], ["pp"], lambda: nc.vector.tensor_scalar(out=pp[:, 4:5], in0=pp[:, 4:5], scalar1=-16.0, scalar2=None, op0=ALU.add))
            R_("act", ["pp"], ["pp"], lambda: nc.scalar.activation(out=pp[:, 5:6], in_=pp[:, 4:5], func=AF.Exp, scale=-float(np.log(10000.0)) / 16))
            R_("dve", ["pp"], ["pp"], lambda: nc.vector.tensor_scalar(out=pp[:, 6:7], in0=pp[:, 0:1], scalar1=31.5, scalar2=2.0, op0=ALU.is_gt, op1=ALU.mult))
            R_("dve", ["pp"], ["pp"], lambda: nc.vector.tensor_scalar(out=pp[:, 6:7], in0=pp[:, 6:7], scalar1=-1.0, scalar2=None, op0=ALU.add))
            R_("dve", ["rw", "cl"], ["tmp"], lambda: nc.vector.tensor_tensor(out=tmp[:], in0=rw[:], in1=cl[:], op=ALU.subtract))
            R_("dve", ["tmp", "cl", "pp"], ["ang"], lambda: nc.vector.scalar_tensor_tensor(out=ang[:], in0=tmp[:], scalar=pp[:, 3:4], in1=cl[:], op0=ALU.mult, op1=ALU.add))
            R_("dve", ["ang", "pp"], ["ang"], lambda: nc.vector.tensor_scalar(out=ang[:], in0=ang[:], scalar1=pp[:, 5:6], scalar2=None, op0=ALU.mult))

            def sin_of(shift, out_ap, post_scale):
                R_("dve", ["ang"], ["tmp"], lambda: nc.vector.tensor_scalar(out=tmp[:], in0=ang[:], scalar1=shift, scalar2=1.0 / (2 * np.pi), op0=ALU.add, op1=ALU.mult))
                R_("dve", ["tmp"], ["ki"], lambda: nc.vector.tensor_copy(out=ki[:], in_=tmp[:]))
                R_("dve", ["ki"], ["rw"], lambda: nc.vector.tensor_copy(out=rw[:], in_=ki[:]))
                R_("dve", ["rw", "ang"], ["tmp"], lambda: nc.vector.scalar_tensor_tensor(out=tmp[:], in0=rw[:], scalar=-2 * np.pi, in1=ang[:], op0=ALU.mult, op1=ALU.add))
                R_("dve", ["tmp"], ["tmp"], lambda: nc.vector.tensor_scalar(out=tmp[:], in0=tmp[:], scalar1=shift, scalar2=None, op0=ALU.add))
                R_("dve", ["tmp"], ["tmp"], lambda: nc.vector.tensor_scalar(out=tmp[:], in0=tmp[:], scalar1=3.1415925, scalar2=-3.1415925, op0=ALU.min, op1=ALU.max))
                if post_scale is None:
                    S.run("act", [b["tmp"]], [bCC], lambda: nc.scalar.activation(out=out_ap, in_=tmp[:], func=AF.Sin))
                else:
                    S.run("act", [b["tmp"]], [bSS], lambda: nc.scalar.activation(out=out_ap, in_=tmp[:], func=AF.Sin))
                    S.run("dve", [bSS, b["pp"]], [bSS], lambda: nc.vector.tensor_scalar(out=out_ap, in0=out_ap, scalar1=pp[:, 6:7], scalar2=None, op0=ALU.mult))
            S.run("dve", [], [bCC], lambda: nc.vector.memset(CC[:, 0:OWN0], 1.0))
            S.run("dve", [], [bSS], lambda: nc.vector.memset(SSn[:, 0:OWN0], 0.0))
            sin_of(float(np.pi / 2), CC[:, OWN0:NTOK], None)
            sin_of(0.0, SSn[:, OWN0:NTOK], True)
            za = t2("za", [64, NTOK]); zb = t2("zb", [64, NTOK]); bza, bzb = Buf("za"), Buf("zb")
            S.dma("sp", za[:], P.zkrT[:, :], [P.b_zkrT], [bza])
            S.dma("sp", zb[:], P.zkrsT[:, :], [P.b_zkrsT], [bzb])
            S.run("dve", [bza, bCC], [bza], lambda: nc.vector.tensor_tensor(out=za[:], in0=za[:], in1=CC[:], op=ALU.mult))
            S.run("dve", [bzb, bSS], [bzb], lambda: nc.vector.tensor_tensor(out=zb[:], in0=zb[:], in1=SSn[:], op=ALU.mult))
            S.run("dve", [bza, bzb], [bKR], lambda: nc.vector.tensor_tensor(out=KR[:], in0=za[:], in1=zb[:], op=ALU.add))
        kn_rot = P.rot_sb(es, "R_kn", [128, NTOK], BF16, 2)
        v_rot = P.rot_sb(es, "R_v", [128, 18, 128], BF16, 2)
        qn_rot = P.rot_sb(es, "R_qn", [128, NOWN], BF16, 2)
        qa_rot = P.rot_sb(es, "R_qa", [64, NOWN], F32, 2)
        qb_rot = P.rot_sb(es, "R_qb", [64, NOWN], F32, 2)
        qr_rot = P.rot_sb(es, "R_qr", [64, NOWN], BF16, 2)
        pt_rot = P.rot_sb(es, "R_pt", [128, 512], BF16, 3)
        y_rot = P.rot_sb(es, "R_y", [128, 512], BF16, 2)
        rl_rot = P.rot_sb(es, "R_rl", [128, 512], F32, 2)
        s_rot = P.rot_ps(es, "R_s", [128, 512], F32, 3)
        o_rot = P.rot_ps(es, "R_o", [128, 512], F32, 2)
        l_rot = P.rot_ps(es, "R_l", [128, 512], F32, 2)
        knv = P.knT.rearrange("(h p) t -> h p t", p=128)
        qnv = P.qnT.rearrange("(h p) t -> h p t", p=128)
        qav = P.qrA.rearrange("(h p) t -> h p t", p=64)
        qbv = P.qrB.rearrange("(h p) t -> h p t", p=64)
        for h in range(32):
            kn, bkn = kn_rot.next(); v, bv = v_rot.next(); qn, bqn = qn_rot.next()
            qa, bqa = qa_rot.next(); qb, bqb = qb_rot.next(); qr, bqr = qr_rot.next()
            S.dma("sp", kn[:], knv[h], [P.b_knT], [bkn])
            S.dma("sp", v[:], P.v_a[:, h * 128:(h + 1) * 128].rearrange("(kb p) d -> p kb d", p=128), [P.b_v_a], [bv])
            S.dma("sp", qn[:], qnv[h], [P.b_qnT], [bqn])
            S.dma("sp", qa[:], qav[h], [P.b_qrA], [bqa])
            S.dma("sp", qb[:], qbv[h], [P.b_qrB], [bqb])
            S.run("dve", [bqa, bCC], [bqa], lambda: nc.vector.tensor_tensor(out=qa[:], in0=qa[:], in1=CC[:, OWN0:OWN1], op=ALU.mult))
            S.run("dve", [bqb, bSS], [bqb], lambda: nc.vector.tensor_tensor(out=qb[:], in0=qb[:], in1=SSn[:, OWN0:OWN1], op=ALU.mult))
            S.run("dve", [bqa, bqb], [bqr], lambda: nc.vector.tensor_tensor(out=qr[:], in0=qa[:], in1=qb[:], op=ALU.add))
            for qt in range(2):
                qs_ = slice(qt * 512, (qt + 1) * 512)
                po, bpo = o_rot.next(); pl, bpl = l_rot.next()
                for kb in range(18):
                    ks_ = slice(kb * 128, (kb + 1) * 128)
                    ps_, bps = s_rot.next()

                    def smm(ps_=ps_, ks_=ks_):
                        nc.tensor.matmul(ps_[:, :], lhsT=kn[:, ks_], rhs=qn[:, qs_], start=True, stop=False)
                        return nc.tensor.matmul(ps_[:, :], lhsT=KR[:, ks_], rhs=qr[:, qs_], start=False, stop=True)
                    S.run("pe", [bkn, bqn, bKR, bqr], [bps], smm)
                    pt, bpt = pt_rot.next()
                    S.run("act", [bps], [bpt], lambda pt=pt, ps_=ps_: nc.scalar.activation(out=pt[:], in_=ps_[:], func=AF.Exp, scale=A_SCALE))

                    def pv(pt=pt, kb=kb):
                        nc.tensor.matmul(po[:, :], lhsT=v[:, kb, :], rhs=pt[:], start=(kb == 0), stop=(kb == 17))
                        return nc.tensor.matmul(pl[:, :], lhsT=P.onesb[:, :], rhs=pt[:], start=(kb == 0), stop=(kb == 17))
                    S.run("pe", [bv, bpt, P.b_id], [bpo, bpl], pv)
                rl, brl = rl_rot.next(); y, by = y_rot.next()
                S.run("dve", [bpl], [brl], lambda rl=rl: nc.vector.reciprocal(out=rl[:], in_=pl[:]))
                S.run("dve", [bpo, brl], [by], lambda y=y, rl=rl: nc.vector.tensor_tensor(out=y[:], in0=po[:], in1=rl[:], op=ALU.mult))
                S.dma("sp", P.ybT[h * 128:(h + 1) * 128, qs_], y[:], [by], [P.b_ybT])


Prog.phase_mla = phase_mla


def phase_merge(P):
    nc, S = P.nc, P.S
    sc = P.scratch
    P.yaT, P.b_yaT = sc("yaT", [D, NOWN], BF16)
    P.m1T, P.b_m1T = sc("m1T", [D, NOWN], F32)
    P.mT, P.b_mT = sc("mT", [D, NOWN], BF16)
    P.y, P.b_y = sc("y", [NOWN, D], F32)
    wa = P.inp("w_branch_a", [D, D]); wb = P.inp("w_branch_b", [D, D]); wo = P.inp("w_out", [D, D])
    mon = P.inp("m_out_norm", [1, D])
    with P.scope() as es:
        MON, bMON = small_T(P, es, "F_MON", mon, 32)
        P.transpose_pass("F0", P.ya_tm, P.b_ya_tm, NOWN, D, P.yaT, P.b_yaT, scale_col=MON, scale_buf=bMON)
    with P.scope() as es:
        g_rot = P.rot_sb(es, "F_g", [128, 512], BF16, 3)
        m_rot = P.rot_sb(es, "F_m", [128, 512], F32, 3)
        t_rot = P.rot_sb(es, "F_t", [128, 512], F32, 3)
        o_rot = P.rot_sb(es, "F_o", [128, 512], BF16, 3)

        def epi1(p, c0, t0, cs, ts, pb):
            g, bg = g_rot.next(); t, bt = t_rot.next()
            S.dma("sp", g[:cs, :ts], P.sgaT[c0:c0 + cs, t0:t0 + ts], [P.b_sgaT], [bg])
            S.run("dve", [pb, bg], [bt], lambda: nc.vector.tensor_tensor(out=t[:cs, :ts], in0=p, in1=g[:cs, :ts], op=ALU.mult))
            S.dma("sp", P.m1T[c0:c0 + cs, t0:t0 + ts], t[:cs, :ts], [bt], [P.b_m1T])

        def epi2(p, c0, t0, cs, ts, pb):
            g, bg = g_rot.next(); t, bt = t_rot.next(); m, bm = m_rot.next(); o, bo = o_rot.next()
            S.dma("sp", g[:cs, :ts], P.sgbT[c0:c0 + cs, t0:t0 + ts], [P.b_sgbT], [bg])
            S.dma("sp", m[:cs, :ts], P.m1T[c0:c0 + cs, t0:t0 + ts], [P.b_m1T], [bm])
            S.run("dve", [pb, bg], [bt], lambda: nc.vector.tensor_tensor(out=t[:cs, :ts], in0=p, in1=g[:cs, :ts], op=ALU.mult))
            S.run("dve", [bt, bm], [bo], lambda: nc.vector.tensor_tensor(out=o[:cs, :ts], in0=t[:cs, :ts], in1=m[:cs, :ts], op=ALU.add))
            S.dma("sp", P.mT[c0:c0 + cs, t0:t0 + ts], o[:cs, :ts], [bo], [P.b_mT])
        P.mm_phase("F1", P.yaT, P.b_yaT, NOWN, D, [dict(w=wa, N=D, mode="fm", epi=epi1)])
        P.mm_phase("F2", P.ybT, P.b_ybT, NOWN, D, [dict(w=wb, N=D, mode="fm", epi=epi2)])
        P.mm_phase("F3", P.mT, P.b_mT, NOWN, D, [dict(w=wo, N=D, mode="tm", epi=P.store_epi(t_rot, P.y, P.b_y, F32))])


def _rstd(P, x, xb, junk, bjunk, st, stb, F):
    nc, S = P.nc, P.S
    S.run("dve", [], [stb], lambda: nc.vector.memset(st[:, :], 0.0))
    S.run("act", [xb, stb], [bjunk, stb], lambda: nc.scalar.activation(out=junk[:, :], in_=x, func=AF.Square, accum_out=st[:, 0:1]))
    S.run("act", [stb], [stb], lambda: nc.scalar.activation(out=st[:, 1:2], in_=st[:, 0:1], func=AF.Sqrt, bias=EPS, scale=1.0 / F))
    S.run("dve", [stb], [stb], lambda: nc.vector.reciprocal(out=st[:, 1:2], in_=st[:, 1:2]))


def phase_ffn(P, experts=range(NE)):
    nc, S = P.nc, P.S
    sc = P.scratch
    xs = P.inp("xs", [NTOK, D])
    P.x1, P.b_x1 = sc("x1", [NOWN, D], F32)
    P.h2T, P.b_h2T = sc("h2T", [D, NOWN], BF16)
    P.gT, P.b_gT = sc("gT", [NE, NOWN], F32)
    P.fo, P.b_fo = sc("fo", [NOWN, D], F32)
    n2 = P.inp("norm_post_mix", [1, D]); n3 = P.inp("norm_pre_ffn", [1, D]); n4 = P.inp("norm_post_ffn", [1, D])
    rw = P.inp("router_w", [D, NE]); rb = P.inp("router_b", [1, NE])
    w_gu = P.inp("w_gu", [NE, D, 3072]); b_gu = P.inp("b_gu", [NE, 3072])
    w_dn = P.inp("w_down", [NE, FE, D]); b_dn = P.inp("b_down", [NE, D])
    out = P.nc.dram_tensor("out", [NOWN, D], F32, kind="ExternalOutput").ap()
    P.b_out = Buf("out")
    with P.scope() as es:
        P.nv, P.b_nv = P.sb(es, "PG_nv", [128, D]), Buf("nv")
        WG1, bWG1 = P.mod_tile(es, "PG_WG1", 0, 2, n2)
        W2, bW2 = P.mod_tile(es, "PG_W2", 0, 4, n3, True)
        SH2, bSH2 = P.mod_tile(es, "PG_SH2", 0, 3)
        RW = P.sb(es, "PG_RW", [128, 32, NE]); bRW = Buf("RW")
        S.dma("sp", RW[:], rw.rearrange("(kt p) e -> p kt e", p=128), [], [bRW])
        RB = P.sb(es, "PG_RB", [128, NE]); bRB = Buf("RB")
        P.bload(RB[:], rb[0:1, :], [], bRB)
        yt = P.sb(es, "PG_y", [128, D]); xt = P.sb(es, "PG_x", [128, D]); byt, bxt = Buf("yt"), Buf("xt")
        junk = P.sb(es, "PG_junk", [128, D], BF16); bjunk = Buf("junk")
        st = P.sb(es, "PG_st", [128, 4]); bst = Buf("st")
        hf = P.sb(es, "PG_hf", [128, 32, 128]); hb = P.sb(es, "PG_hb", [128, 32, 128], BF16); bhf, bhb = Buf("hf"), Buf("hb")
        L = P.sb(es, "PG_L", [128, NE]); E_ = P.sb(es, "PG_E", [128, NE]); MX = P.sb(es, "PG_MX", [128, 16])
        bL, bE, bMX = Buf("L"), Buf("E"), Buf("MX")
        GTs = P.sb(es, "PG_GT", [NE, 128]); bGTs = Buf("GTs")
        pt_rot = P.rot_ps(es, "PG_pt", [128, 4, 128], F32, 3)
        plg = P.ps(es, "PG_plg", [128, 512]); bplg = Buf("plg")
        h2v = P.h2T.rearrange("(kt p) t -> p kt t", p=128)
        for i in range(8):
            r0 = i * 128
            S.dma("sp", yt[:], P.y[r0:r0 + 128, :], [P.b_y], [byt])
            S.dma("sp", xt[:], xs[OWN0 + r0:OWN0 + r0 + 128, :], [], [bxt])
            _rstd(P, yt[:], byt, junk, bjunk, st, bst, D)
            S.run("dve", [byt, bst, bWG1], [byt], lambda: nc.vector.scalar_tensor_tensor(
                out=yt[:], in0=yt[:], scalar=st[:, 1:2], in1=WG1[:], op0=ALU.mult, op1=ALU.mult))
            S.run("dve", [byt, bxt], [bxt], lambda: nc.vector.tensor_tensor(out=xt[:], in0=yt[:], in1=xt[:], op=ALU.add))
            S.dma("sp", P.x1[r0:r0 + 128, :], xt[:], [bxt], [P.b_x1])
            _rstd(P, xt[:], bxt, junk, bjunk, st, bst, D)
            S.run("dve", [bxt, bst, bW2], [byt], lambda: nc.vector.scalar_tensor_tensor(
                out=yt[:], in0=xt[:], scalar=st[:, 1:2], in1=W2[:], op0=ALU.mult, op1=ALU.mult))
            S.run("dve", [byt, bSH2], [byt], lambda: nc.vector.tensor_tensor(out=yt[:], in0=yt[:], in1=SH2[:], op=ALU.add))
            for g0 in range(0, 32, 4):
                p, pb = pt_rot.next()

                def tr(p=p, g0=g0):
                    for j in range(4):
                        ins = nc.tensor.transpose(out=p[:, j, :], in_=yt[:, (g0 + j) * 128:(g0 + j + 1) * 128], identity=P.idf[:, :])
                    return ins
                S.run("pe", [byt, P.b_id], [pb], tr)
                S.run("act", [pb], [bhf], lambda p=p, g0=g0: nc.scalar.copy(out=hf[:, g0:g0 + 4, :], in_=p[:]))
                S.run("dve", [bhf], [bhb], lambda g0=g0: nc.vector.tensor_copy(out=hb[:, g0:g0 + 4, :], in_=hf[:, g0:g0 + 4, :]))
            S.dma("sp", h2v[:, :, r0:r0 + 128], hb[:], [bhb], [P.b_h2T])

            def lg():
                for kt in range(32):
                    ins = nc.tensor.matmul(plg[:, 0:NE], lhsT=hf[:, kt, :], rhs=RW[:, kt, :], start=(kt == 0), stop=(kt == 31))
                return ins
            S.run("pe", [bhf, bRW], [bplg], lg)
            S.run("dve", [bplg, bRB], [bL], lambda: nc.vector.tensor_tensor(out=L[:], in0=plg[:, 0:NE], in1=RB[:], op=ALU.add))
            S.run("dve", [bL], [bMX], lambda: nc.vector.max(out=MX[:, 0:8], in_=L[:]))
            S.run("dve", [bMX], [bMX], lambda: nc.vector.tensor_scalar(out=MX[:, 8:9], in0=MX[:, 0:1], scalar1=-1.0, scalar2=None, op0=ALU.mult))
            S.run("act", [bL, bMX], [bE], lambda: nc.scalar.activation(out=E_[:], in_=L[:], func=AF.Exp, bias=MX[:, 8:9], scale=1.0))
            S.run("dve", [bL, bMX], [bL], lambda: nc.vector.tensor_scalar(out=L[:], in0=L[:], scalar1=MX[:, 3:4], scalar2=None, op0=ALU.is_ge))
            S.run("dve", [bE, bL], [bE], lambda: nc.vector.tensor_tensor(out=E_[:], in0=E_[:], in1=L[:], op=ALU.mult))
            S.run("dve", [bE], [bMX], lambda: nc.vector.reduce_sum(out=MX[:, 9:10], in_=E_[:], axis=AX.X))
            S.run("dve", [bMX], [bMX], lambda: nc.vector.reciprocal(out=MX[:, 9:10], in_=MX[:, 9:10]))
            S.run("dve", [bE, bMX], [bE], lambda: nc.vector.tensor_scalar(out=E_[:], in0=E_[:], scalar1=MX[:, 9:10], scalar2=None, op0=ALU.mult))
            S.run("pe", [bE, P.b_id], [bplg], lambda: nc.tensor.transpose(out=plg[0:NE, 128:256], in_=E_[:, :], identity=P.idf[:, :]))
            S.run("dve", [bplg], [bGTs], lambda: nc.vector.tensor_copy(out=GTs[:], in_=plg[0:NE, 128:256]))
            S.dma("sp", P.gT[:, r0:r0 + 128], GTs[:], [bGTs], [P.b_gT])
    if getattr(P, "stop_after_g", False):
        return
    TH = NOWN
    NJ = TH // 128
    bfo = [[Buf(f"fo{j}_{cb}") for cb in range(8)] for j in range(NJ)]
    P.bfo_all = [b for row in bfo for b in row]
    with P.scope() as es:
        GT = P.sb(es, "H_GT", [NE, NOWN]); bGT = Buf("GT")
        S.dma("sp", GT[:], P.gT[:, :], [P.b_gT], [bGT])
        BD = P.sb(es, "H_BD", [NE, D]); bBD = Buf("BD")
        S.dma("sp", BD[:], b_dn[:, :], [], [bBD])
        BG = P.sb(es, "H_BG", [128, NE * 24]); bBG = Buf("BG")
        with P.scope() as es2:
            r_ = P.sb(es2, "H_bgr", [128, 128]); br_ = Buf("bgr")
            pp_ = P.ps(es2, "H_bgp", [128, 128]); bpp = Buf("bgp")
            bgv = b_gu.rearrange("e (r p) -> (e r) p", p=128)
            for blk in range(6):
                S.dma("sp", r_[:], bgv[blk * 128:(blk + 1) * 128, :], [], [br_])
                S.run("pe", [br_, P.b_id], [bpp], lambda: nc.tensor.transpose(out=pp_[:, :], in_=r_[:, :], identity=P.idf[:, :]))
                S.run("dve", [bpp], [bBG], lambda blk=blk: nc.vector.tensor_copy(out=BG[:, blk * 128:(blk + 1) * 128], in_=pp_[:, :]))
        act = P.sb(es, "H_act", [128, 32, TH], BF16); bact = Buf("act")
        actT = P.sb(es, "H_actT", [128, 12, TH], BF16); bactT = Buf("actT")
        GB = P.rot_sb(es, "H_GB", [128, TH], F32, 2)
        wg_rot = P.rot_sb(es, "H_wg", [128, 32, 256], BF16, 2)
        wd_rot = P.rot_sb(es, "H_wd", [128, 12, 512], BF16, 2)
        st_rot = P.rot_sb(es, "H_st", [128, 512], F32, 4)
        G1 = P.sb(es, "H_G1", [128, 512]); SG = P.sb(es, "H_SG", [128, 512]); L1 = P.sb(es, "H_L1", [128, 512])
        bG1, bSG, bL1 = Buf("G1"), Buf("SG"), Buf("L1")
        pg_rot = P.rot_ps(es, "H_pg", [128, 512], F32, 2)
        pl_rot = P.rot_ps(es, "H_pl", [128, 512], F32, 2)
        pd_rot = P.rot_ps(es, "H_pd", [128, 512], F32, 3)
        h2v = P.h2T.rearrange("(kt p) t -> p kt t", p=128)
        S.dma("sp", act[:], h2v[:, :, 0:TH], [P.b_h2T], [bact])
        for j in range(NJ):
            for cb in range(8):
                pd, bpd = pd_rot.next()
                S.run("pe", [bGT, bBD], [bpd], lambda pd=pd, j=j, cb=cb: nc.tensor.matmul(
                    pd[:, :], lhsT=GT[:, j * 128:(j + 1) * 128], rhs=BD[:, cb * 512:(cb + 1) * 512], start=True, stop=True))
                stt, bst = st_rot.next()
                P.evac([bpd], [bst], stt[:, :], pd[:, :])
                S.dma("sp", P.fo[j * 128:(j + 1) * 128, cb * 512:(cb + 1) * 512], stt[:, :], [bst], [bfo[j][cb]])
        blocks = []
        for e in experts:
            blocks += [("g", e, c) for c in range(12)] + [("d", e, cb) for cb in range(8)]
        loaded = {}

        def load(k):
            kind, e, x = blocks[k]
            if kind == "g":
                w, bw = wg_rot.next()
                S.dma("pq", w[:], w_gu[e].rearrange("(kt p) n -> p kt n", p=128)[:, :, x * 256:(x + 1) * 256], [], [bw])
            else:
                w, bw = wd_rot.next()
                S.dma("pq", w[:], w_dn[e].rearrange("(c p) n -> p c n", p=128)[:, :, x * 512:(x + 1) * 512], [], [bw])
            loaded[k] = (w, bw)
        load(0)
        gb = bgb = None
        for k, (kind, e, x) in enumerate(blocks):
            if k + 1 < len(blocks):
                load(k + 1)
            w, bw = loaded.pop(k)
            if kind == "g":
                c = x
                if c == 0:
                    gb, bgb = GB.next()
                    S.dma("sp", gb[:], P.gT[e:e + 1, 0:TH].partition_broadcast(128), [P.b_gT], [bgb])
                col = e * 24 + c * 2
                for hf_ in range(TH // 512):
                    ts_ = slice(hf_ * 512, (hf_ + 1) * 512)
                    pg, bpg = pg_rot.next(); pl, bpl = pl_rot.next()

                    def gmm(pg=pg, w=w, ts_=ts_):
                        for kt in range(32):
                            ins = nc.tensor.matmul(pg[:, :], lhsT=w[:, kt, 0:128], rhs=act[:, kt, ts_], start=(kt == 0), stop=(kt == 31))
                        return ins

                    def lmm(pl=pl, w=w, ts_=ts_):
                        for kt in range(32):
                            ins = nc.tensor.matmul(pl[:, :], lhsT=w[:, kt, 128:256], rhs=act[:, kt, ts_], start=(kt == 0), stop=(kt == 31))
                        return ins
                    S.run("pe", [bact, bw], [bpg], gmm)
                    S.run("pe", [bact, bw], [bpl], lmm)
                    S.run("dve", [bpg, bBG], [bG1], lambda pg=pg, col=col: nc.vector.tensor_scalar(
                        out=G1[:], in0=pg[:, :], scalar1=BG[:, col:col + 1], scalar2=7.0, op0=ALU.add, op1=ALU.min))
                    S.run("act", [bG1], [bSG], lambda: nc.scalar.activation(out=SG[:], in_=G1[:], func=AF.Sigmoid, scale=1.702))
                    S.run("dve", [bpl, bBG], [bL1], lambda pl=pl, col=col: nc.vector.tensor_scalar(
                        out=L1[:], in0=pl[:, :], scalar1=BG[:, col + 1:col + 2], scalar2=7.0, op0=ALU.add, op1=ALU.min))
                    S.run("dve", [bL1], [bL1], lambda: nc.vector.tensor_scalar(out=L1[:], in0=L1[:], scalar1=-7.0, scalar2=1.0, op0=ALU.max, op1=ALU.add))
                    S.run("dve", [bG1, bSG], [bG1], lambda: nc.vector.tensor_tensor(out=G1[:], in0=G1[:], in1=SG[:], op=ALU.mult))
                    S.run("dve", [bL1, bgb], [bL1], lambda gb=gb, ts_=ts_: nc.vector.tensor_tensor(out=L1[:], in0=L1[:], in1=gb[:, ts_], op=ALU.mult))
                    S.run("dve", [bG1, bL1], [bactT], lambda c=c, ts_=ts_: nc.vector.tensor_tensor(out=actT[:, c, ts_], in0=G1[:], in1=L1[:], op=ALU.mult))
            else:
                cb = x
                for j in range(NJ):
                    pd, bpd = pd_rot.next()

                    def dmm(pd=pd, w=w, j=j):
                        for c in range(12):
                            ins = nc.tensor.matmul(pd[:, :], lhsT=actT[:, c, j * 128:(j + 1) * 128], rhs=w[:, c, :], start=(c == 0), stop=(c == 11))
                        return ins
                    S.run("pe", [bactT, bw], [bpd], dmm)
                    stt, bst = st_rot.next()
                    P.evac([bpd], [bst], stt[:, :], pd[:, :])
                    S.dma("pq", P.fo[j * 128:(j + 1) * 128, cb * 512:(cb + 1) * 512], stt[:, :], [bst], [bfo[j][cb]], accum_op=ALU.add)
    with P.scope() as es:
        P.nv, P.b_nv = P.sb(es, "I_nv", [128, D]), Buf("nv")
        WG2, bWG2 = P.mod_tile(es, "I_WG2", 0, 5, n4)
        f_rot = P.rot_sb(es, "I_f", [128, D], F32, 2)
        x_rot = P.rot_sb(es, "I_x", [128, D], F32, 2)
        junk = P.sb(es, "I_junk", [128, D], BF16); bjunk = Buf("junk")
        st_rot = P.rot_sb(es, "I_st", [128, 4], F32, 2)
        for i in range(8):
            r0 = i * 128
            ft, bft = f_rot.next(); xt, bxt = x_rot.next(); st, bst = st_rot.next()
            S.dma("sp", ft[:], P.fo[r0:r0 + 128, :], P.bfo_all, [bft])
            S.dma("sp", xt[:], P.x1[r0:r0 + 128, :], [P.b_x1], [bxt])
            _rstd(P, ft[:], bft, junk, bjunk, st, bst, D)
            S.run("dve", [bft, bst, bWG2], [bft], lambda: nc.vector.scalar_tensor_tensor(
                out=ft[:], in0=ft[:], scalar=st[:, 1:2], in1=WG2[:], op0=ALU.mult, op1=ALU.mult))
            S.run("dve", [bft, bxt], [bxt], lambda: nc.vector.tensor_tensor(out=xt[:], in0=ft[:], in1=xt[:], op=ALU.add))
            S.dma("sp", out[r0:r0 + 128, :], xt[:], [bxt], [P.b_out])


Prog.phase_merge = phase_merge
Prog.phase_ffn = phase_ffn


def build_full(debug=()):
    P = Prog(debug=debug)
    with ExitStack() as es:
        P.consts(es)
        P.phase_mod(); P.phase_h(); P.phase_proj(); P.phase_mlstm(); P.phase_mla(); P.phase_merge(); P.phase_ffn()
        P.S.finish([P.b_out] + [getattr(P, "b_" + n) for n in debug])
    return P


_prog = None


def kernel(**inputs):
    global _prog
    if _prog is None:
        _prog = build_full()
    P = _prog
    names = list(P.inputs)
    in_maps = [core_inputs(inputs, c, names) for c in range(8)]
    res = run_bass_kernel_spmd(P.nc, in_maps, core_ids=list(range(8)))
    out = np.empty((4, 2048, D), np.float32)
    for c in range(8):
        b, half = c // 2, c % 2
        o = np.asarray(res.results[c]["out"], dtype=np.float32)
        if half == 0:
            out[b, 0:1024] = o
        else:
            out[b, 1024:2048] = o[::-1]
    return out
```

```python
import numpy as np
from contextlib import ExitStack, contextmanager
import concourse.bass as bass
import concourse.mybir as mybir
from concourse.bass_utils import run_bass_kernel_spmd

F32 = mybir.dt.float32
BF16 = mybir.dt.bfloat16
I32 = mybir.dt.int32
ALU = mybir.AluOpType
AF = mybir.ActivationFunctionType
AX = mybir.AxisListType

D = 4096
NTOK = 2304
OWN0, OWN1 = 256, 1280
NOWN = 1024
EPS = 1e-6
NE = 32
FE = 1536
A_SCALE = 192 ** -0.5
CQ, CK, CV, CO, CG, CCQ, CCKV, CKR, CGA, CGB = 0, 2048, 4096, 8192, 12288, 12320, 13344, 13856, 13920, 18016
DIN = 22112


class Buf:
    __slots__ = ("name", "w", "r")

    def __init__(self, name=""):
        self.name = name
        self.w = None
        self.r = []


class Sched:
    DMAK = 6
    ROT = 28000

    def __init__(self, nc):
        self.nc = nc
        self.eng = {"pe": nc.tensor, "act": nc.scalar, "dve": nc.vector, "pool": nc.gpsimd, "sp": nc.sync}
        self.sem, self.cnt, self.nsem = {}, {}, 0
        for e in ("pe", "act", "dve", "pool"):
            self._fresh(e)
        self.q_issuer = {"sp": "sp", "pq": "pool"}
        self.qsem, self.qcnt, self.qi = {}, {}, {}
        for q in self.q_issuer:
            self.qsem[q] = [self._alloc(f"{q}{j}") for j in range(self.DMAK)]
            self.qcnt[q] = [0] * self.DMAK
            self.qi[q] = 0
        self.waited = {e: {} for e in self.eng}

    def _alloc(self, name):
        self.nsem += 1
        return self.nc.alloc_semaphore(name=f"s_{name}_{self.nsem}")

    def _fresh(self, e):
        self.sem[e] = self._alloc(e)
        self.cnt[e] = 0

    def _wait(self, ename, dep):
        sem, val, _ = dep
        w = self.waited[ename]
        key = id(sem)
        if w.get(key, (None, 0))[1] >= val:
            return
        w[key] = (sem, val)
        self.eng[ename].wait_ge(sem, val)

    def _deps(self, ename, reads, writes, is_dma):
        for b in reads:
            for d in b.w or ():
                self._wait(ename, d)
        for b in writes:
            for d in b.w or ():
                if is_dma or d[2] != ename:
                    self._wait(ename, d)
            for d in b.r:
                if is_dma or d[2] != ename:
                    self._wait(ename, d)

    def _update(self, tok, reads, writes):
        for b in reads:
            b.r.append(tok)
        for b in writes:
            if tok[2] == "dma":
                b.w = [d for d in (b.w or ()) if d[2] == "dma"][-(2 * self.DMAK - 1):] + [tok]
            else:
                b.w = [tok]
            b.r = []

    def run(self, ename, reads, writes, fn):
        if self.cnt[ename] >= self.ROT:
            self._fresh(ename)
        self._deps(ename, reads, writes, False)
        ins = fn()
        self.cnt[ename] += 1
        ins.then_inc(self.sem[ename], 1)
        tok = (self.sem[ename], self.cnt[ename], ename)
        self._update(tok, reads, writes)
        return tok

    def dma(self, q, out, in_, reads, writes, **kw):
        issuer = self.q_issuer[q]
        j = self.qi[q]
        self.qi[q] = (j + 1) % self.DMAK
        if self.qcnt[q][j] >= self.ROT:
            self._wait(issuer, (self.qsem[q][j], self.qcnt[q][j], "dma"))
            self.qsem[q][j] = self._alloc(f"{q}{j}")
            self.qcnt[q][j] = 0
        sem = self.qsem[q][j]
        if self.qcnt[q][j] > 0:
            self._wait(issuer, (sem, self.qcnt[q][j], "dma"))
        self._deps(issuer, reads, writes, True)
        eng = self.nc.sync if q == "sp" else self.nc.gpsimd
        eng.dma_start(out=out, in_=in_, **kw).then_inc(sem, 16)
        self.qcnt[q][j] += 16
        tok = (sem, self.qcnt[q][j], "dma")
        self._update(tok, reads, writes)
        return tok

    def barrier(self):
        toks = [(self.sem[e], self.cnt[e], e) for e in ("pe", "act", "dve", "pool") if self.cnt[e] > 0]
        for q in self.q_issuer:
            for j in range(self.DMAK):
                if self.qcnt[q][j] > 0:
                    toks.append((self.qsem[q][j], self.qcnt[q][j], "dma"))
        for e in self.eng:
            for t in toks:
                self._wait(e, t)

    def finish(self, bufs):
        for b in bufs:
            for d in b.w or ():
                self._wait("sp", d)


class Rot:
    def __init__(self, tiles, name):
        self.tiles = tiles
        self.bufs = [Buf(f"{name}{i}") for i in range(len(tiles))]
        self.i = 0

    def next(self):
        j = self.i % len(self.tiles)
        self.i += 1
        return self.tiles[j], self.bufs[j]


class Prog:
    def __init__(self, debug=()):
        self.nc = bass.Bass("TRN2", target_bir_lowering=False)
        self.S = Sched(self.nc)
        self.inputs = {}
        self.debug = set(debug)
        self.outs = []
        self.k = 0

    @contextmanager
    def scope(self):
        with ExitStack() as es:
            yield es
            self.S.barrier()

    def inp(self, name, shape, dtype=F32):
        if name not in self.inputs:
            self.inputs[name] = self.nc.dram_tensor(name, list(shape), dtype, kind="ExternalInput").ap()
        return self.inputs[name]

    def scratch(self, name, shape, dtype):
        if name in self.debug:
            self.outs.append(name)
            return self.nc.dram_tensor(name, list(shape), dtype, kind="ExternalOutput").ap(), Buf(name)
        return self.nc.dram_tensor(name, list(shape), dtype).ap(), Buf(name)

    def sb(self, es, name, shape, dtype=F32):
        return es.enter_context(self.nc.sbuf_tensor(name, list(shape), dtype))

    def ps(self, es, name, shape, dtype=F32):
        return es.enter_context(self.nc.psum_tensor(name, list(shape), dtype))

    def rot_sb(self, es, name, shape, dtype, n):
        return Rot([self.sb(es, f"{name}{i}", shape, dtype) for i in range(n)], name)

    def rot_ps(self, es, name, shape, dtype, n):
        return Rot([self.ps(es, f"{name}{i}", shape, dtype) for i in range(n)], name)

    def evac(self, reads, writes, out, in_):
        nc = self.nc
        self.k += 1
        if self.k % 2 == 0:
            return self.S.run("act", reads, writes, lambda: nc.scalar.copy(out=out, in_=in_))
        return self.S.run("dve", reads, writes, lambda: nc.vector.tensor_copy(out=out, in_=in_))

    def consts(self, es):
        nc, S = self.nc, self.S
        self.idf = self.sb(es, "idf", [128, 128], F32)
        self.idb = self.sb(es, "idb", [128, 128], BF16)
        self.b_id = Buf("ident")
        self.onesb = self.sb(es, "onesb", [128, 128], BF16)
        self.onesf = self.sb(es, "onesf", [128, 128], F32)

        def mk():
            nc.gpsimd.memset(self.onesb[:], 1.0)
            nc.gpsimd.memset(self.onesf[:], 1.0)
            return nc.gpsimd.memset(self.idf[:], 1.0)
        S.run("pool", [], [self.b_id], mk)
        S.run("pool", [self.b_id], [self.b_id], lambda: nc.gpsimd.affine_select(
            out=self.idf[:], in_=self.idf[:], pattern=[[-1, 128]], compare_op=ALU.is_equal, fill=0.0, base=0, channel_multiplier=1))
        S.run("dve", [self.b_id], [self.b_id], lambda: nc.vector.tensor_copy(out=self.idb[:], in_=self.idf[:]))

    def mm_phase(self, name, act, actbuf, T, K, jobs, TG=1024, act_pre=None):
        nc, S = self.nc, self.S
        KT = K // 128
        with self.scope() as es:
            ngroups = (T + TG - 1) // TG
            if act_pre is None:
                actv = act.rearrange("(kt p) t -> p kt t", p=128)
                a_rot = self.rot_sb(es, f"{name}_a", [128, KT, TG], BF16, 2 if ngroups > 1 else 1)
            NBmax = max(j.get("NB", 512) for j in jobs)
            w_rot = self.rot_sb(es, f"{name}_w", [128, KT, NBmax], BF16, 2)
            p_rot = self.rot_ps(es, f"{name}_ps", [128, 512], F32, 4)
            for g0 in range(0, T, TG):
                tg = min(TG, T - g0)
                if act_pre is None:
                    asb, ab = a_rot.next()
                    S.dma("sp", asb[:, :, :tg], actv[:, :, g0:g0 + tg], [actbuf] if actbuf else [], [ab])
                else:
                    asb, ab = act_pre
                for job in jobs:
                    N, mode, epi, NB = job["N"], job["mode"], job["epi"], job.get("NB", 512)
                    wv = job["w"].rearrange("(kt p) n -> p kt n", p=128)
                    for n0 in range(0, N, NB):
                        nb = min(NB, N - n0)
                        wsb, wb = w_rot.next()
                        S.dma("pq", wsb[:, :, :nb], wv[:, :, n0:n0 + nb], [], [wb])
                        if mode == "tm":
                            for t0 in range(0, tg, 128):
                                ts = min(128, tg - t0)
                                p, pb = p_rot.next()

                                def mm(p=p, t0=t0, ts=ts, wsb=wsb, nb=nb):
                                    for kt in range(KT):
                                        ins = nc.tensor.matmul(p[:ts, :nb], lhsT=asb[:, kt, t0:t0 + ts],
                                                               rhs=wsb[:, kt, :nb], start=(kt == 0), stop=(kt == KT - 1))
                                    return ins
                                S.run("pe", [ab, wb], [pb], mm)
                                epi(p[:ts, :nb], g0 + t0, n0, ts, nb, pb)
                        else:
                            for c0 in range(0, nb, 128):
                                cs = min(128, nb - c0)
                                for t0 in range(0, tg, 512):
                                    ts = min(512, tg - t0)
                                    p, pb = p_rot.next()

                                    def mm(p=p, c0=c0, cs=cs, t0=t0, ts=ts, wsb=wsb):
                                        for kt in range(KT):
                                            ins = nc.tensor.matmul(p[:cs, :ts], lhsT=wsb[:, kt, c0:c0 + cs],
                                                                   rhs=asb[:, kt, t0:t0 + ts],
                                                                   start=(kt == 0), stop=(kt == KT - 1))
                                        return ins
                                    S.run("pe", [ab, wb], [pb], mm)
                                    epi(p[:cs, :ts], n0 + c0, g0 + t0, cs, ts, pb)

    def store_epi(self, st, dst, dbuf, dtype, row_off=0, col_off=0, func=None, scale=None, bias=None):
        nc, S = self.nc, self.S

        def epi(p, r0, c0, rs, cs, pb):
            t, tb = st.next()
            o = t[:rs, :cs]
            if func is not None or bias is not None:
                kw = {}
                if bias is not None:
                    kw["bias"] = bias(r0, rs)
                S.run("act", [pb] + ([self.b_bias] if bias is not None else []), [tb],
                      lambda: nc.scalar.activation(out=o, in_=p, func=func or AF.Identity,
                                                   scale=1.0 if scale is None else scale, **kw))
            elif scale is not None:
                self.k += 1
                if self.k % 2 == 0:
                    S.run("act", [pb], [tb], lambda: nc.scalar.mul(out=o, in_=p, mul=scale))
                else:
                    S.run("dve", [pb], [tb], lambda: nc.vector.tensor_scalar(out=o, in0=p, scalar1=scale, scalar2=None, op0=ALU.mult))
            else:
                self.evac([pb], [tb], o, p)
            S.dma("sp", dst[row_off + r0:row_off + r0 + rs, col_off + c0:col_off + c0 + cs], o, [tb], [dbuf])
        return epi

    def norm_T(self, name, src, sbuf_src, T, F, gain_of_tile, shift_of_tile, dst, dbuf, gbufs, src_row0=0):
        nc, S = self.nc, self.S
        FT = F // 128
        with self.scope() as es:
            x_rot = self.rot_sb(es, f"{name}_x", [128, F], F32, 2)
            junk = self.sb(es, f"{name}_junk", [128, F], BF16)
            b_junk = Buf("junk")
            xn_rot = self.rot_sb(es, f"{name}_xn", [128, F], BF16, 2)
            o_rot = self.rot_sb(es, f"{name}_o", [128, FT, 128], BF16, 2)
            st_rot = self.rot_sb(es, f"{name}_st", [128, 2], F32, 2)
            p_rot = self.rot_ps(es, f"{name}_pt", [128, 8, 128], BF16, 3)
            dstv = dst.rearrange("(kt p) t -> p kt t", p=128)
            for ti, t0 in enumerate(range(0, T, 128)):
                ts = min(128, T - t0)
                x, xb = x_rot.next()
                S.dma("sp", x[:ts, :], src[src_row0 + t0:src_row0 + t0 + ts, :], [sbuf_src] if sbuf_src else [], [xb])
                st, stb = st_rot.next()
                S.run("dve", [], [stb], lambda: nc.vector.memset(st[:, :], 0.0))
                S.run("act", [xb], [b_junk, stb], lambda: nc.scalar.activation(
                    out=junk[:ts, :], in_=x[:ts, :], func=AF.Square, accum_out=st[:ts, 0:1]))
                S.run("act", [stb], [stb], lambda: nc.scalar.activation(
                    out=st[:ts, 1:2], in_=st[:ts, 0:1], func=AF.Sqrt, bias=EPS, scale=1.0 / F))
                S.run("dve", [stb], [stb], lambda: nc.vector.reciprocal(out=st[:ts, 1:2], in_=st[:ts, 1:2]))
                xn, xnb = xn_rot.next()
                g = gain_of_tile(ti)
                sh = shift_of_tile(ti) if shift_of_tile else None
                if sh is None:
                    S.run("dve", [xb, stb] + gbufs, [xnb], lambda: nc.vector.scalar_tensor_tensor(
                        out=xn[:ts, :], in0=x[:ts, :], scalar=st[:ts, 1:2], in1=g[:ts, :], op0=ALU.mult, op1=ALU.mult))
                else:
                    S.run("dve", [xb, stb] + gbufs, [xb], lambda: nc.vector.scalar_tensor_tensor(
                        out=x[:ts, :], in0=x[:ts, :], scalar=st[:ts, 1:2], in1=g[:ts, :], op0=ALU.mult, op1=ALU.mult))
                    S.run("dve", [xb] + gbufs, [xnb], lambda: nc.vector.tensor_tensor(
                        out=xn[:ts, :], in0=x[:ts, :], in1=sh[:ts, :], op=ALU.add))
                o, ob = o_rot.next()
                for g0 in range(0, FT, 8):
                    gn = min(8, FT - g0)
                    p, pb = p_rot.next()

                    def tr(p=p, g0=g0, gn=gn):
                        for j in range(gn):
                            ins = nc.tensor.transpose(out=p[:, j, :ts], in_=xn[:ts, (g0 + j) * 128:(g0 + j + 1) * 128],
                                                      identity=self.idb[:ts, :ts])
                        return ins
                    S.run("pe", [xnb, self.b_id], [pb], tr)
                    self.evac([pb], [ob], o[:, g0:g0 + gn, :ts], p[:, :gn, :ts])
                S.dma("sp", dstv[:, :, t0:t0 + ts], o[:, :, :ts], [ob], [dbuf])

    def transpose_pass(self, name, src, sbuf_src, T, F, dst, dbuf, scale_col=None, scale_buf=None):
        nc, S = self.nc, self.S
        FT = F // 128
        with self.scope() as es:
            x_rot = self.rot_sb(es, f"{name}_x", [128, F], BF16, 2)
            o_rot = self.rot_sb(es, f"{name}_o", [128, FT, 128], BF16, 2)
            p_rot = self.rot_ps(es, f"{name}_pt", [128, 8, 128], BF16, 3)
            dstv = dst.rearrange("(kt p) t -> p kt t", p=128)
            for t0 in range(0, T, 128):
                x, xb = x_rot.next()
                S.dma("sp", x[:, :], src[t0:t0 + 128, :], [sbuf_src], [xb])
                o, ob = o_rot.next()
                for g0 in range(0, FT, 8):
                    p, pb = p_rot.next()

                    def tr(p=p, g0=g0):
                        for j in range(8):
                            ins = nc.tensor.transpose(out=p[:, j, :], in_=x[:, (g0 + j) * 128:(g0 + j + 1) * 128],
                                                      identity=self.idb[:, :])
                        return ins
                    S.run("pe", [xb, self.b_id], [pb], tr)
                    if scale_col is None:
                        self.evac([pb], [ob], o[:, g0:g0 + 8, :], p[:, :, :])
                    else:
                        S.run("dve", [pb, scale_buf], [ob], lambda p=p, g0=g0: nc.vector.tensor_tensor(
                            out=o[:, g0:g0 + 8, :], in0=p[:, :, :],
                            in1=scale_col[:, g0:g0 + 8].unsqueeze(2).to_broadcast([128, 8, 128]), op=ALU.mult))
                S.dma("sp", dstv[:, :, t0:t0 + 128], o[:, :, :], [ob], [dbuf])

    def bload(self, t, src_row, bufs_r, buf_w, np_=128):
        self.S.dma("sp", t, src_row.partition_broadcast(np_), bufs_r, [buf_w])

    def phase_mod(self):
        nc, S = self.nc, self.S
        cvec = self.inp("cvec", [2, D])
        w_ada = self.inp("w_ada", [D, 6 * D])
        b_ada = self.inp("b_ada", [1, 6 * D])
        self.modv, self.b_modv = self.scratch("modv", [2, 6 * D], F32)
        with self.scope() as es:
            c32 = self.sb(es, "A_c32", [32, 2, 128], F32)
            b_c = Buf("c32")
            S.dma("sp", c32[:], cvec.rearrange("r (kt p) -> kt r p", p=128), [], [b_c])
            S.run("act", [b_c], [b_c], lambda: nc.scalar.activation(out=c32[:], in_=c32[:], func=AF.Silu))
            cT = self.sb(es, "A_cT", [128, 32, 2], BF16)
            b_cT = Buf("cT")
            with self.scope() as es2:
                pt = self.ps(es2, "A_pt", [128, 2, 32], F32)
                b_pt = Buf("pt")

                def tr():
                    for r in range(2):
                        ins = nc.tensor.transpose(out=pt[:, r, :], in_=c32[:, r, :], identity=self.idf[:32, :32])
                    return ins
                S.run("pe", [b_c, self.b_id], [b_pt], tr)
                S.run("dve", [b_pt], [b_cT], lambda: nc.vector.tensor_copy(
                    out=cT[:].rearrange("p k r -> p r k"), in_=pt[:]))
            bias = self.sb(es, "A_bias", [2, 6 * D], F32)
            b_bias = Buf("bias")
            self.bload(bias[:], b_ada[0:1, :], [], b_bias, 2)
            st = self.rot_sb(es, "A_st", [2, 512], F32, 3)

            def epi(p, r0, c0, rs, cs, pb):
                t, tb = st.next()
                S.run("dve", [pb, b_bias], [tb], lambda: nc.vector.tensor_tensor(
                    out=t[:rs, :cs], in0=p, in1=bias[:rs, c0:c0 + cs], op=ALU.add))
                S.dma("sp", self.modv[0:rs, c0:c0 + cs], t[:rs, :cs], [tb], [self.b_modv])
            self.mm_phase("A", None, None, 2, D, [dict(w=w_ada, N=6 * D, mode="tm", epi=epi)], act_pre=(cT, b_cT))

    def mod_tile(self, es, name, row, chunk, normvec=None, plus1=False):
        nc, S = self.nc, self.S
        t = self.sb(es, name, [128, D], F32)
        b = Buf(name)
        self.bload(t[:], self.modv[row:row + 1, chunk * D:(chunk + 1) * D], [self.b_modv], b)
        if normvec is not None:
            nv, bn = self.nv, self.b_nv
            self.bload(nv[:], normvec[0:1, :], [], bn)
            if plus1:
                S.run("dve", [b, bn], [b], lambda: nc.vector.scalar_tensor_tensor(
                    out=t[:], in0=t[:], scalar=1.0, in1=nv[:], op0=ALU.add, op1=ALU.mult))
            else:
                S.run("dve", [b, bn], [b], lambda: nc.vector.tensor_tensor(out=t[:], in0=t[:], in1=nv[:], op=ALU.mult))
        return t, b

    def phase_h(self):
        xs = self.inp("xs", [NTOK, D])
        n1 = self.inp("norm_pre_mix", [1, D])
        self.hT, self.b_hT = self.scratch("hT", [D, NTOK], BF16)
        with self.scope() as es:
            self.nv, self.b_nv = self.sb(es, "B_nv", [128, D], F32), Buf("nv")
            W1, bW1 = self.mod_tile(es, "B_W1", 0, 1, n1, True)
            SH1, bS1 = self.mod_tile(es, "B_SH1", 0, 0)
            W1c, bW1c = self.mod_tile(es, "B_W1c", 1, 1, n1, True)
            SH1c, bS1c = self.mod_tile(es, "B_SH1c", 1, 0)
            self.norm_T("B", xs, None, NTOK, D, lambda ti: W1c if ti < 2 else W1,
                        lambda ti: SH1c if ti < 2 else SH1, self.hT, self.b_hT, [bW1, bS1, bW1c, bS1c])

    def phase_proj(self):
        nc, S = self.nc, self.S
        w_in = self.inp("w_in", [D, DIN])
        w_krs = self.inp("w_krs", [D, 64])
        b_g = self.inp("b_gates", [1, 32])
        sc = self.scratch
        self.qT, self.b_qT = sc("qT", [2048, NOWN], BF16)
        self.kT, self.b_kT = sc("kT", [2048, NTOK], BF16)
        self.k_tm, self.b_k_tm = sc("k_tm", [NTOK, 2048], BF16)
        self.v_tm, self.b_v_tm = sc("v_tm", [NTOK, 4096], BF16)
        self.so, self.b_so = sc("so", [NOWN, 4096], BF16)
        self.zcq, self.b_zcq = sc("zcq", [NOWN, 1024], F32)
        self.sgaT, self.b_sgaT = sc("sgaT", [D, NOWN], BF16)
        self.sgbT, self.b_sgbT = sc("sgbT", [D, NOWN], BF16)
        self.zgT, self.b_zgT = sc("zgT", [32, NTOK], F32)
        self.zckv, self.b_zckv = sc("zckv", [NTOK, 512], F32)
        self.zkrT, self.b_zkrT = sc("zkrT", [64, NTOK], F32)
        self.zkrsT, self.b_zkrsT = sc("zkrsT", [64, NTOK], F32)
        with self.scope() as es:
            stb = self.rot_sb(es, "C_stb", [128, 512], BF16, 3)
            stf = self.rot_sb(es, "C_stf", [128, 512], F32, 3)
            bg = self.sb(es, "C_bg", [32, 1], F32)
            self.b_bias = Buf("bg")
            S.dma("sp", bg[:], b_g.rearrange("o g -> g o"), [], [self.b_bias])
            E = self.store_epi

            def all_jobs(tok_off):
                return [
                    dict(w=w_in[:, CK:CK + 2048], N=2048, mode="fm", epi=E(stb, self.kT, self.b_kT, BF16, col_off=tok_off, scale=0.0625)),
                    dict(w=w_in[:, CK:CK + 2048], N=2048, mode="tm", epi=E(stb, self.k_tm, self.b_k_tm, BF16, row_off=tok_off, scale=0.0625)),
                    dict(w=w_in[:, CV:CV + 4096], N=4096, mode="tm", epi=E(stb, self.v_tm, self.b_v_tm, BF16, row_off=tok_off)),
                    dict(w=w_in[:, CG:CG + 32], N=32, mode="fm", epi=E(stf, self.zgT, self.b_zgT, F32, col_off=tok_off,
                                                                     bias=lambda r0, rs: bg[r0:r0 + rs, 0:1])),
                    dict(w=w_in[:, CCKV:CCKV + 512], N=512, mode="tm", epi=E(stf, self.zckv, self.b_zckv, F32, row_off=tok_off)),
                    dict(w=w_in[:, CKR:CKR + 64], N=64, mode="fm", epi=E(stf, self.zkrT, self.b_zkrT, F32, col_off=tok_off)),
                    dict(w=w_krs, N=64, mode="fm", epi=E(stf, self.zkrsT, self.b_zkrsT, F32, col_off=tok_off)),
                ]
            own_jobs = [
                dict(w=w_in[:, CQ:CQ + 2048], N=2048, mode="fm", epi=E(stb, self.qT, self.b_qT, BF16)),
                dict(w=w_in[:, CO:CO + 4096], N=4096, mode="tm", epi=E(stb, self.so, self.b_so, BF16, func=AF.Sigmoid)),
                dict(w=w_in[:, CCQ:CCQ + 1024], N=1024, mode="tm", epi=E(stf, self.zcq, self.b_zcq, F32)),
                dict(w=w_in[:, CGA:CGA + 4096], N=4096, mode="fm", epi=E(stb, self.sgaT, self.b_sgaT, BF16, func=AF.Sigmoid)),
                dict(w=w_in[:, CGB:CGB + 4096], N=4096, mode="fm", epi=E(stb, self.sgbT, self.b_sgbT, BF16, func=AF.Sigmoid)),
            ]
            self.mm_phase("C0", self.hT[:, 0:OWN0], self.b_hT, OWN0, D, all_jobs(0))
            self.mm_phase("C1", self.hT[:, OWN0:OWN1], self.b_hT, NOWN, D, all_jobs(OWN0) + own_jobs)
            self.mm_phase("C2", self.hT[:, OWN1:NTOK], self.b_hT, NTOK - OWN1, D, all_jobs(OWN1))


DEINT = list(range(0, 64, 2)) + list(range(1, 64, 2))
SWAPI = list(range(1, 64, 2)) + list(range(0, 64, 2))
_cache = {}


def core_inputs(inputs, c, names):
    b, half = c // 2, c % 2
    out = {}
    f32 = np.float32

    def shared(key, fn):
        if key not in _cache:
            _cache[key] = np.ascontiguousarray(fn(), dtype=f32)
        return _cache[key]

    for n in names:
        if n == "xs":
            xc, xx = inputs["ctx"][b], inputs["x"][b]
            if half:
                xc, xx = xc[::-1], xx[::-1]
            out[n] = np.ascontiguousarray(np.concatenate([xc, xx], 0), dtype=f32)
        elif n == "cvec":
            out[n] = np.ascontiguousarray(np.stack([inputs["c"][b], inputs["c_ctx"]], 0), dtype=f32)
        elif n == "posinfo":
            out[n] = np.array([[2047.0, -1.0]] if half else [[0.0, 1.0]], f32)
        elif n == "w_in":
            def mk(half=half):
                w = np.array(inputs["w_in"][0], dtype=f32)
                if half:
                    w[:, CG:CG + 32] = np.concatenate([w[:, CG + 16:CG + 32], w[:, CG:CG + 16]], 1)
                w[:, CKR:CKR + 64] = w[:, CKR:CKR + 64][:, DEINT]
                return w
            out[n] = shared(("w_in", half), mk)
        elif n == "w_krs":
            out[n] = shared("w_krs", lambda: inputs["w_in"][0][:, CKR:CKR + 64][:, SWAPI])
        elif n == "b_gates":
            g = inputs["b_gates"][0]
            if half:
                g = np.concatenate([g[16:32], g[0:16]])
            out[n] = np.ascontiguousarray(g[None, :], dtype=f32)
        elif n in ("w_uqn", "w_uqr", "w_uqs"):
            w3 = inputs["w_uq"][0].reshape(1024, 32, 192)
            if n == "w_uqn":
                out[n] = shared(n, lambda: w3[:, :, :128].reshape(1024, 4096))
            else:
                idx = DEINT if n == "w_uqr" else SWAPI
                out[n] = shared(n, lambda: w3[:, :, 128:][:, :, idx].reshape(1024, 2048))
        elif n in ("w_ukn", "w_ukvv"):
            w3 = inputs["w_ukv"][0].reshape(512, 32, 256)
            out[n] = shared(n, lambda: (w3[:, :, :128] if n == "w_ukn" else w3[:, :, 128:]).reshape(512, 4096))
        elif n == "w_uq":
            def mk():
                w = np.array(inputs["w_uq"][0], dtype=f32).reshape(1024, 32, 192)
                w[:, :, 128:] = w[:, :, 128:][:, :, DEINT]
                return w.reshape(1024, 6144)
            out[n] = shared("w_uq", mk)
        elif n == "w_uqs":
            out[n] = shared("w_uqs", lambda: inputs["w_uq"][0].reshape(1024, 32, 192)[:, :, 128:][:, :, SWAPI].reshape(1024, 2048))
        elif n == "w_gu":
            def mk():
                w = inputs["w_gu"][0].reshape(NE, D, 2, 12, 128)
                return np.ascontiguousarray(w.transpose(0, 1, 3, 2, 4)).reshape(NE, D, 3072)
            out[n] = shared("w_gu", mk)
        elif n == "b_gu":
            out[n] = shared("b_gu", lambda: inputs["b_gu"][0].reshape(NE, 2, 12, 128).transpose(0, 2, 1, 3).reshape(NE, 3072))
        elif n in ("w_down", "b_down"):
            out[n] = shared(n, lambda n=n: inputs[n][0])
        elif n in ("b_ada", "norm_pre_mix", "norm_post_mix", "norm_pre_ffn", "norm_post_ffn", "m_out_norm",
                   "q_norm", "kv_norm", "router_b"):
            out[n] = shared(n, lambda n=n: np.asarray(inputs[n]).reshape(1, -1))
        else:
            out[n] = shared(n, lambda n=n: inputs[n][0])
    return out


def _scan(P, X, Y, bx, by, pr, lo, hi, desc, op):
    nc, S = P.nc, P.S
    n = hi - lo
    s = 1
    src, dst, bs, bd = X, Y, bx, by
    while s < n:
        if not desc:
            a_out, a_in0, a_in1 = (lo + s, hi), (lo + s, hi), (lo, hi - s)
            c_rng = (lo, lo + s)
        else:
            a_out, a_in0, a_in1 = (lo, hi - s), (lo, hi - s), (lo + s, hi)
            c_rng = (hi - s, hi)
        S.run("dve", [bs], [bd], lambda src=src, dst=dst: nc.vector.tensor_tensor(
            out=dst[pr, a_out[0]:a_out[1]], in0=src[pr, a_in0[0]:a_in0[1]], in1=src[pr, a_in1[0]:a_in1[1]], op=op))
        S.run("act", [bs], [bd], lambda src=src, dst=dst: nc.scalar.copy(
            out=dst[pr, c_rng[0]:c_rng[1]], in_=src[pr, c_rng[0]:c_rng[1]]))
        src, dst, bs, bd = dst, src, bd, bs
        s *= 2
    if src is not X:
        S.run("dve", [by], [bx], lambda: nc.vector.tensor_copy(out=X[pr, lo:hi], in_=Y[pr, lo:hi]))


def phase_mlstm(P, d1_only=False):
    nc, S = P.nc, P.S
    R = 40
    P1, P2 = slice(0, 8), slice(32, 40)
    NCH = NTOK // 64
    P.h1, P.b_h1 = P.scratch("h1", [NOWN, 4096], F32)
    P.ya_tm, P.b_ya_tm = P.scratch("ya_tm", [NOWN, 4096], BF16)
    with P.scope() as es:
        sb = lambda n, s, d=F32: P.sb(es, "M_" + n, s, d)
        A = sb("A", [R, NTOK]); NEGMU = sb("NEGMU", [R, NTOK]); SCL = sb("SCL", [R, NTOK])
        WKtm = sb("WKtm", [64, NCH, R]); EMTtm = sb("EMTtm", [64, NCH, R]); DEC = sb("DEC", [R, NCH])
        MASKD = sb("MASKD", [R, 8, 64]); NM = [sb("NM1", [64, 8, 64]), sb("NM2", [64, 8, 64])]
        bA, bNEGMU, bSCL, bWK, bEMT, bDEC, bMASKD, bNM = (Buf(n) for n in "A NEGMU SCL WK EMT DEC MASKD NM".split())
        with P.scope() as es2:
            t2 = lambda n, s, d=F32: P.sb(es2, "G_" + n, s, d)
            I = t2("I", [R, NTOK]); X = t2("X", [R, NTOK]); Y = t2("Y", [R, NTOK]); Bt = t2("B", [R, NTOK])
            MU = t2("MU", [R, NTOK]); T3 = t2("T3", [R, NTOK]); ME = t2("ME", [R, NCH]); MP = t2("MP", [R, NCH])
            bI, bX, bY, bB, bMU, bT3, bME, bMP = (Buf(n) for n in "I X Y B MU T3 ME MP".split())

            def z():
                for t in (I, X, Y, A, NEGMU, SCL, MU, T3, MASKD, NM[0]):
                    nc.gpsimd.memset(t[:], 0.0)
                return nc.gpsimd.memset(NM[1][:], 0.0)
            S.run("pool", [], [bI, bX, bY, bA, bNEGMU, bSCL, bMU, bT3, bMASKD, bNM], z)
            S.run("pool", [bNM], [bNM], lambda: nc.gpsimd.affine_select(
                out=NM[0][:], in_=NM[0][:], pattern=[[0, 8], [1, 64]], compare_op=ALU.is_ge, fill=-30000.0, base=0, channel_multiplier=-1))
            S.run("pool", [bNM], [bNM], lambda: nc.gpsimd.affine_select(
                out=NM[1][:], in_=NM[1][:], pattern=[[0, 8], [-1, 64]], compare_op=ALU.is_ge, fill=-30000.0, base=0, channel_multiplier=1))
            for pr, off in ((P1, 0), (P2, 32)):
                S.run("dve", [P.b_id, bMASKD], [bMASKD], lambda pr=pr, off=off: nc.vector.tensor_copy(
                    out=MASKD[pr], in_=P.idf[pr, off:off + 8].unsqueeze(2).to_broadcast([8, 8, 64])))
            S.dma("sp", I[P1, :], P.zgT[0:8, :], [P.b_zgT, bI], [bI])
            S.dma("sp", X[P1, :], P.zgT[8:16, :], [P.b_zgT, bX], [bX])
            S.dma("sp", I[P2, :], P.zgT[16:24, :], [P.b_zgT, bI], [bI])
            S.dma("sp", X[P2, :], P.zgT[24:32, :], [P.b_zgT, bX], [bX])
            S.run("act", [bX], [bX], lambda: nc.scalar.activation(out=X[:], in_=X[:], func=AF.Exp, scale=-1.0))
            S.run("act", [bX], [bX], lambda: nc.scalar.activation(out=X[:], in_=X[:], func=AF.Ln, bias=1.0, scale=1.0))
            S.run("dve", [bX], [bX], lambda: nc.vector.tensor_scalar(out=X[:], in0=X[:], scalar1=-1.0, scalar2=None, op0=ALU.mult))
            _scan(P, X, Y, bX, bY, P1, 0, OWN1, False, ALU.add)
            _scan(P, X, Y, bX, bY, P2, 0, OWN0, True, ALU.add)
            _scan(P, X, Y, bX, bY, P2, OWN0, NTOK, True, ALU.add)
            S.run("dve", [bX], [bX], lambda: nc.vector.tensor_scalar(
                out=X[P2, OWN0:NTOK], in0=X[P2, OWN0:NTOK], scalar1=X[P2, 0:1], scalar2=None, op0=ALU.add))
            S.run("dve", [bX], [bB], lambda: nc.vector.tensor_copy(out=Bt[:], in_=X[:]))
            S.run("dve", [bI, bB], [bA], lambda: nc.vector.tensor_tensor(out=A[:], in0=I[:], in1=Bt[:], op=ALU.subtract))
            S.run("dve", [bA], [bX], lambda: nc.vector.tensor_copy(out=X[:], in_=A[:]))
            _scan(P, X, Y, bX, bY, P1, 0, OWN1, False, ALU.max)
            _scan(P, X, Y, bX, bY, P2, 0, OWN0, True, ALU.max)
            _scan(P, X, Y, bX, bY, P2, OWN0, NTOK, True, ALU.max)
            S.run("dve", [bX], [bX], lambda: nc.vector.tensor_scalar(out=X[:], in0=X[:], scalar1=0.0, scalar2=None, op0=ALU.max))
            S.run("dve", [bX], [bX], lambda: nc.vector.tensor_scalar(
                out=X[P2, OWN0:NTOK], in0=X[P2, OWN0:NTOK], scalar1=X[P2, 0:1], scalar2=None, op0=ALU.max))
            S.run("dve", [bX], [bMU], lambda: nc.vector.tensor_copy(out=MU[:], in_=X[:]))
            S.run("dve", [bMU], [bNEGMU], lambda: nc.vector.tensor_scalar(out=NEGMU[:], in0=MU[:], scalar1=-1.0, scalar2=None, op0=ALU.mult))
            MUv = MU[:].rearrange("p (c j) -> p c j", j=64)
            S.run("dve", [bMU], [bME], lambda: nc.vector.tensor_copy(out=ME[:], in_=MUv[:, :, 0]))
            S.run("dve", [bMU, bME], [bME], lambda: nc.vector.tensor_copy(out=ME[P1, :], in_=MUv[P1, :, 63]))

            def mp():
                nc.vector.memset(MP[:], 0.0)
                nc.vector.tensor_copy(out=MP[P1, 1:NCH], in_=ME[P1, 0:NCH - 1])
                nc.vector.tensor_copy(out=MP[P2, 0:NCH - 1], in_=ME[P2, 1:NCH])
                nc.vector.memset(MP[P2, 3:4], 0.0)
                return nc.vector.tensor_copy(out=MP[P2, NCH - 1:NCH], in_=ME[P2, 0:1])
            S.run("dve", [bME], [bMP], mp)
            S.run("dve", [bMP, bMU], [bT3], lambda: nc.vector.tensor_tensor(
                out=T3[:].rearrange("p (c j) -> p c j", j=64), in0=MP[:].unsqueeze(2).to_broadcast([R, NCH, 64]), in1=MUv, op=ALU.subtract))
            S.run("act", [bT3], [bSCL], lambda: nc.scalar.activation(out=SCL[:], in_=T3[:], func=AF.Exp))
            S.run("dve", [bMP, bME], [bDEC], lambda: nc.vector.tensor_tensor(out=DEC[:], in0=MP[:], in1=ME[:], op=ALU.subtract))
            S.run("act", [bDEC], [bDEC], lambda: nc.scalar.activation(out=DEC[:], in_=DEC[:], func=AF.Exp))
            S.run("dve", [bA, bME], [bX], lambda: nc.vector.tensor_tensor(
                out=X[:].rearrange("p (c j) -> p c j", j=64), in0=A[:].rearrange("p (c j) -> p c j", j=64),
                in1=ME[:].unsqueeze(2).to_broadcast([R, NCH, 64]), op=ALU.subtract))
            S.run("act", [bX], [bX], lambda: nc.scalar.activation(out=X[:], in_=X[:], func=AF.Exp))
            S.run("dve", [bB, bMU], [bY], lambda: nc.vector.tensor_tensor(out=Y[:], in0=Bt[:], in1=MU[:], op=ALU.add))
            S.run("act", [bY], [bY], lambda: nc.scalar.activation(out=Y[:], in_=Y[:], func=AF.Exp, scale=-1.0))
            pt_rot = P.rot_ps(es2, "G_pt", [64, 12, R], F32, 2)
            for src, bsrc, dst, bdst in ((X, bX, WKtm, bWK), (Y, bY, EMTtm, bEMT)):
                for c0 in range(0, NCH, 12):
                    pt, bpt = pt_rot.next()

                    def tr(pt=pt, c0=c0, src=src):
                        for j in range(12):
                            ins = nc.tensor.transpose(out=pt[:, j, :], in_=src[:, (c0 + j) * 64:(c0 + j + 1) * 64],
                                                      identity=P.idf[:R, :R])
                        return ins
                    S.run("pe", [bsrc, P.b_id], [bpt], tr)
                    P.evac([bpt], [bdst], dst[:, c0:c0 + 12, :], pt[:])
        if d1_only:
            dbg, bdbg = P.scratch("d1dbg", [R, 3 * NTOK + NCH], F32)
            S.dma("sp", dbg[:, 0:NTOK], A[:], [bA], [bdbg])
            S.dma("sp", dbg[:, NTOK:2 * NTOK], NEGMU[:], [bNEGMU], [bdbg])
            S.dma("sp", dbg[:, 2 * NTOK:3 * NTOK], SCL[:], [bSCL], [bdbg])
            S.dma("sp", dbg[:, 3 * NTOK:3 * NTOK + NCH], DEC[:], [bDEC], [bdbg])
            dbg2, bdbg2 = P.scratch("d1dbg2", [64, 2 * NCH * R], F32)
            S.dma("sp", dbg2[:, 0:NCH * R], WKtm[:].rearrange("p c r -> p (c r)"), [bWK], [bdbg2])
            S.dma("sp", dbg2[:, NCH * R:], EMTtm[:].rearrange("p c r -> p (c r)"), [bEMT], [bdbg2])
            P.b_d1dbg, P.b_d1dbg2 = bdbg, bdbg2
            return
        CT = sb("CT", [128, 8, 2, 512]); CTb = sb("CTb", [128, 8, 2, 512], BF16)
        NT = sb("NT", [128, 8, 2]); NTb = sb("NTb", [128, 8, 2, 2], BF16)
        bCT, bCTb, bNT, bNTb = Buf("CT"), Buf("CTb"), Buf("NT"), Buf("NTb")
        kt_rot = P.rot_sb(es, "M_kt", [64, 2048], BF16, 2)
        v_rot = P.rot_sb(es, "M_v", [64, 8, 512], BF16, 2)
        q_rot = P.rot_sb(es, "M_q", [128, 16, 64], BF16, 2)
        k_rot = P.rot_sb(es, "M_k", [128, 16, 64], BF16, 2)
        h1_rot = P.rot_sb(es, "M_h1", [64, 8, 512], F32, 1)
        so_rot = P.rot_sb(es, "M_so", [64, 8, 512], BF16, 1)
        vs_rot = P.rot_sb(es, "M_vs", [64, 8, 512], BF16, 1)
        hst_rot = P.rot_sb(es, "M_hst", [64, 512], F32, 3)
        R1 = sb("R1", [R, 8, 64]); R2 = sb("R2", [R, 8, 64]); R3 = sb("R3", [R, 8])
        bR1, bR2, bR3 = Buf("R1"), Buf("R2"), Buf("R3")
        WT = sb("WT", [64, 512]); PT = sb("PT", [64, 512], BF16); QS = sb("QS", [128, 16, 64], BF16)
        bWT, bPT, bQS = Buf("WT"), Buf("PT"), Buf("QS")
        RD = sb("RD", [64, 8]); RD2 = sb("RD2", [64, 8]); DECs = sb("DECs", [128, 8]); WKb = sb("WKb", [64, 8, 2], BF16)
        bRD, bDECs, bWKb = Buf("RD"), Buf("DECs"), Buf("WKb")
        HS = sb("HS", [64, 8, 512]); SS = sb("SS", [64, 16]); YA = sb("YA", [64, 4096], BF16)
        junk = sb("junk", [64, 512], BF16)
        bHS, bSS, bYA, bjunk = Buf("HS"), Buf("SS"), Buf("YA"), Buf("junk")
        psD = P.ps(es, "M_psD", [64, 512]); psE = P.ps(es, "M_psE", [128, 512]); psS = P.ps(es, "M_psS", [64, 512])
        psM = P.ps(es, "M_psM", [128, 512])
        bpsD, bpsE, bpsS = Buf("psD"), Buf("psE"), Buf("psS")
        bDEN = bNUPS = bDECp = Buf("psM")
        num_rot = P.rot_ps(es, "M_num", [64, 512], F32, 2)
        upd_rot = P.rot_ps(es, "M_upd", [128, 512], F32, 2)
        qTv = P.qT.rearrange("(g p) t -> p g t", p=128)
        kTv = P.kT.rearrange("(g p) t -> p g t", p=128)
        for d in (0, 1):
            pb = 0 if d == 0 else 32
            pr = slice(pb, pb + 8)
            if d == 0:
                seq = [(c, c >= 4) for c in range(0, 20)]
            else:
                seq = [(c, False) for c in (3, 2, 1, 0)] + [(c, False) for c in range(35, 19, -1)] + [(c, True) for c in range(19, 3, -1)]

            lim = getattr(P, "lim", None)
            if lim is not None:
                if d not in lim["dirs"]:
                    continue
                seq = [x for x in seq if lim["sel"](x)]
            def zs():
                nc.gpsimd.memset(CT[:], 0.0)
                nc.gpsimd.memset(CTb[:], 0.0)
                nc.gpsimd.memset(NT[:], 0.0)
                return nc.gpsimd.memset(NTb[:], 0.0)
            S.run("pool", [], [bCT, bCTb, bNT, bNTb], zs)
            for si, (c, is_out) in enumerate(seq):
                last = si == len(seq) - 1
                n0 = c * 64
                o0 = n0 - OWN0
                cs = slice(n0, n0 + 64)
                KTc, bKT = kt_rot.next()
                Vc, bV = v_rot.next()
                S.dma("sp", KTc[:], P.k_tm[n0:n0 + 64, :], [P.b_k_tm], [bKT])
                S.dma("sp", Vc[:].rearrange("p h v -> p (h v)"), P.v_tm[n0:n0 + 64, :], [P.b_v_tm], [bV])
                if is_out:
                    Qc, bQ = q_rot.next()
                    Kc, bK = k_rot.next()
                    S.dma("sp", Qc[:], qTv[:, :, o0:o0 + 64], [P.b_qT], [bQ])
                    S.dma("sp", Kc[:], kTv[:, :, n0:n0 + 64], [P.b_kT], [bK])
                    if d == 1:
                        H1c, bH1 = h1_rot.next()
                        SOc, bSO = so_rot.next()
                        S.dma("sp", H1c[:].rearrange("p h v -> p (h v)"), P.h1[o0:o0 + 64, :], [P.b_h1], [bH1])
                        S.dma("sp", SOc[:].rearrange("p h v -> p (h v)"), P.so[o0:o0 + 64, :], [P.b_so], [bSO])
                    S.run("dve", [bNEGMU, bMASKD], [bR1], lambda: nc.vector.tensor_tensor(
                        out=R1[pr], in0=NEGMU[pr, cs].unsqueeze(1).to_broadcast([8, 8, 64]), in1=MASKD[pr], op=ALU.mult))
                    S.run("dve", [bSCL, bMASKD], [bR2], lambda: nc.vector.tensor_tensor(
                        out=R2[pr], in0=SCL[pr, cs].unsqueeze(1).to_broadcast([8, 8, 64]), in1=MASKD[pr], op=ALU.mult))

                    def dmm():
                        nc.tensor.matmul(psD[:, :], lhsT=P.onesf[pr, 0:64], rhs=R1[pr].rearrange("p h t -> p (h t)"), start=True, stop=False)
                        nc.tensor.matmul(psD[:, :], lhsT=A[pr, cs], rhs=MASKD[pr].rearrange("p h t -> p (h t)"), start=False, stop=False)
                        return nc.tensor.matmul(psD[:, :], lhsT=P.idf[0:64, 0:64], rhs=NM[d][:].rearrange("p h t -> p (h t)"), start=False, stop=True)
                    S.run("pe", [bR1, bA, bMASKD, bNM, P.b_id], [bpsD], dmm)
                    S.run("act", [bpsD], [bWT], lambda: nc.scalar.activation(out=WT[:], in_=psD[:], func=AF.Exp))
                    S.run("pe", [bR2, P.b_id], [bpsE], lambda: nc.tensor.matmul(
                        psE[:, :], lhsT=P.onesf[pr, 0:128], rhs=R2[pr].rearrange("p h t -> p (h t)"), start=True, stop=True))
                    S.run("dve", [bpsE, bQ], [bQS], lambda: nc.vector.tensor_tensor(
                        out=QS[:].rearrange("p (h k) t -> p h k t", k=2), in0=Qc[:].rearrange("p (h k) t -> p h k t", k=2),
                        in1=psE[:].rearrange("p (h t) -> p h t", t=64).unsqueeze(2).to_broadcast([128, 8, 2, 64]), op=ALU.mult))

                    def smm():
                        for h in range(8):
                            for kc in range(2):
                                ins = nc.tensor.matmul(psS[:, h * 64:(h + 1) * 64], lhsT=Kc[:, h * 2 + kc, :], rhs=Qc[:, h * 2 + kc, :],
                                                       start=(kc == 0), stop=(kc == 1))
                        return ins
                    S.run("pe", [bK, bQ], [bpsS], smm)
                    S.run("dve", [bpsS, bWT], [bPT], lambda: nc.vector.tensor_tensor(out=PT[:], in0=psS[:], in1=WT[:], op=ALU.mult))

                    def denmm():
                        for h in range(8):
                            nc.tensor.matmul(psM[0:64, 2 * h:2 * h + 2], lhsT=PT[:, h * 64:(h + 1) * 64], rhs=P.onesb[0:64, 0:2], start=True, stop=False)
                            for kc in range(2):
                                ins = nc.tensor.matmul(psM[0:64, 2 * h:2 * h + 2], lhsT=QS[:, h * 2 + kc, :], rhs=NTb[:, h, kc, :],
                                                       start=False, stop=(kc == 1))
                        return ins
                    S.run("pe", [bPT, bQS, bNTb], [bDEN], denmm)

                    S.run("dve", [bDEN], [bRD], lambda: nc.vector.tensor_copy(out=RD2[:], in_=psM[0:64, 0:16].rearrange("p (h t) -> p h t", t=2)[:, :, 0]))
                    S.run("dve", [bRD], [bRD], lambda: nc.vector.scalar_tensor_tensor(
                        out=RD[:], in0=RD2[:], scalar=-1.0, in1=RD2[:], op0=ALU.mult, op1=ALU.max))
                    S.run("dve", [bRD, bEMT], [bRD], lambda: nc.vector.tensor_tensor(out=RD[:], in0=RD[:], in1=EMTtm[:, c, pr], op=ALU.max))
                    S.run("dve", [bRD], [bRD], lambda: nc.vector.reciprocal(out=RD[:], in_=RD[:]))
                    for h in range(8):
                        pn, bpn = num_rot.next()

                        def nmm(pn=pn, h=h):
                            nc.tensor.matmul(pn[:, :], lhsT=PT[:, h * 64:(h + 1) * 64], rhs=Vc[:, h, :], start=True, stop=False)
                            for kc in range(2):
                                ins = nc.tensor.matmul(pn[:, :], lhsT=QS[:, h * 2 + kc, :], rhs=CTb[:, h, kc, :], start=False, stop=(kc == 1))
                            return ins
                        S.run("pe", [bPT, bV, bQS, bCTb], [bpn], nmm)
                        if d == 0:
                            t, tb = hst_rot.next()
                            S.run("act", [bpn, bRD], [tb], lambda t=t, pn=pn, h=h: nc.scalar.activation(
                                out=t[:], in_=pn[:], func=AF.Copy, scale=RD[:, h:h + 1]))
                            S.dma("sp", P.h1[o0:o0 + 64, h * 512:(h + 1) * 512], t[:], [tb], [P.b_h1])
                        else:
                            S.run("dve", [bpn, bRD, bH1], [bHS], lambda pn=pn, h=h: nc.vector.scalar_tensor_tensor(
                                out=HS[:, h, :], in0=pn[:], scalar=RD[:, h:h + 1], in1=H1c[:, h, :], op0=ALU.mult, op1=ALU.add))
                    if d == 1:
                        S.run("dve", [], [bSS], lambda: nc.vector.memset(SS[:], 0.0))

                        def sq():
                            for h in range(8):
                                ins = nc.scalar.activation(out=junk[:], in_=HS[:, h, :], func=AF.Square, accum_out=SS[:, h:h + 1])
                            return ins
                        S.run("act", [bHS, bSS], [bjunk, bSS], sq)
                        S.run("act", [bSS], [bSS], lambda: nc.scalar.activation(
                            out=SS[:, 8:16], in_=SS[:, 0:8], func=AF.Sqrt, bias=EPS, scale=1.0 / 512))
                        S.run("dve", [bSS], [bSS], lambda: nc.vector.reciprocal(out=SS[:, 8:16], in_=SS[:, 8:16]))
                        S.run("dve", [bHS, bSS], [bHS], lambda: nc.vector.tensor_tensor(
                            out=HS[:], in0=HS[:], in1=SS[:, 8:16].unsqueeze(2).to_broadcast([64, 8, 512]), op=ALU.mult))
                        S.run("dve", [bHS, bSO], [bYA], lambda: nc.vector.tensor_tensor(
                            out=YA[:], in0=HS[:].rearrange("p h v -> p (h v)"), in1=SOc[:].rearrange("p h v -> p (h v)"), op=ALU.mult))
                        S.dma("sp", P.ya_tm[o0:o0 + 64, :], YA[:], [bYA], [P.b_ya_tm])
                if last:
                    continue
                VS, bVS = vs_rot.next()
                S.run("dve", [bV, bWK], [bVS], lambda: nc.vector.tensor_tensor(
                    out=VS[:], in0=Vc[:], in1=WKtm[:, c, pr].unsqueeze(2).to_broadcast([64, 8, 512]), op=ALU.mult))
                S.run("dve", [bWK], [bWKb], lambda: nc.vector.tensor_copy(out=WKb[:], in_=WKtm[:, c, pr].unsqueeze(2).to_broadcast([64, 8, 2])))
                stage = (lim or {}).get("stage", 99)
                if stage < 2:
                    continue
                S.run("dve", [P.b_id, bDEC], [bR3], lambda: nc.vector.tensor_scalar(
                    out=R3[pr], in0=P.idf[pr, pb:pb + 8], scalar1=DEC[pr, c:c + 1], scalar2=None, op0=ALU.mult))
                S.run("pe", [bR3, P.b_id], [bDECp], lambda: nc.tensor.matmul(
                    psM[:, 32:40], lhsT=P.onesf[pr, 0:128], rhs=R3[pr], start=True, stop=True))
                S.run("act", [bDECp], [bDECs], lambda: nc.scalar.copy(out=DECs[:], in_=psM[:, 32:40]))

                if stage < 3:
                    continue

                def numm():
                    for h in range(8):
                        for kc in range(2):
                            ins = nc.tensor.matmul(psM[:, 64 + (h * 2 + kc) * 2:66 + (h * 2 + kc) * 2], lhsT=KTc[:, h * 256 + kc * 128:h * 256 + (kc + 1) * 128],
                                                   rhs=WKb[:, h, :], start=True, stop=True)
                    return ins
                S.run("pe", [bKT, bWKb], [bNUPS], numm)

                if stage < 4:
                    continue
                S.run("dve", [bDECs, bNTb], [bNT], lambda: nc.vector.tensor_tensor(
                    out=NT[:], in0=NT[:], in1=DECs[:].unsqueeze(2).to_broadcast([128, 8, 2]), op=ALU.mult))
                S.run("dve", [bNUPS, bNT], [bNT], lambda: nc.vector.tensor_tensor(
                    out=NT[:], in0=NT[:], in1=psM[:, 64:96].rearrange("p (h k t) -> p h k t", k=2, t=2)[:, :, :, 0], op=ALU.add))
                S.run("dve", [bNT], [bNTb], lambda: nc.vector.tensor_copy(out=NTb[:], in_=NT[:].unsqueeze(3).to_broadcast([128, 8, 2, 2])))
                if stage < 5:
                    continue
                for h in range(8):
                    for kc in range(2):
                        pu, bpu = upd_rot.next()
                        S.run("pe", [bKT, bVS], [bpu], lambda pu=pu, h=h, kc=kc: nc.tensor.matmul(
                            pu[:, :], lhsT=KTc[:, h * 256 + kc * 128:h * 256 + (kc + 1) * 128], rhs=VS[:, h, :], start=True, stop=True))
                        S.run("dve", [bpu, bDECs, bCTb], [bCT], lambda pu=pu, h=h, kc=kc: nc.vector.scalar_tensor_tensor(
                            out=CT[:, h, kc, :], in0=CT[:, h, kc, :], scalar=DECs[:, h:h + 1], in1=pu[:], op0=ALU.mult, op1=ALU.add))
                        S.run("act", [bCT], [bCTb], lambda h=h, kc=kc: nc.scalar.copy(out=CTb[:, h, kc, :], in_=CT[:, h, kc, :]))


Prog.phase_mlstm = phase_mlstm


def small_T(P, es, name, vec, n):
    nc, S = P.nc, P.S
    t = P.sb(es, name, [128, n], F32)
    b = Buf(name)
    with P.scope() as es2:
        r = P.sb(es2, name + "_r", [n, 128], F32)
        br = Buf(name + "r")
        S.dma("sp", r[:], vec.rearrange("o (k p) -> (o k) p", p=128), [], [br])
        pt = P.ps(es2, name + "_pt", [128, n], F32)
        bpt = Buf(name + "pt")
        S.run("pe", [br, P.b_id], [bpt], lambda: nc.tensor.transpose(out=pt[:, :], in_=r[:, :], identity=P.idf[:n, :n]))
        S.run("dve", [bpt], [b], lambda: nc.vector.tensor_copy(out=t[:], in_=pt[:]))
    return t, b


def phase_mla(P):
    nc, S = P.nc, P.S
    sc = P.scratch
    P.cqT, P.b_cqT = sc("cqT", [1024, NOWN], BF16)
    P.ckvT, P.b_ckvT = sc("ckvT", [512, NTOK], BF16)
    P.qnT, P.b_qnT = sc("qnT", [4096, NOWN], BF16)
    P.qrA, P.b_qrA = sc("qrA", [2048, NOWN], F32)
    P.qrB, P.b_qrB = sc("qrB", [2048, NOWN], F32)
    P.knT, P.b_knT = sc("knT", [4096, NTOK], BF16)
    P.v_a, P.b_v_a = sc("v_a", [NTOK, 4096], BF16)
    P.ybT, P.b_ybT = sc("ybT", [4096, NOWN], BF16)
    q_norm = P.inp("q_norm", [1, 1024]); kv_norm = P.inp("kv_norm", [1, 512])
    w_uqn = P.inp("w_uqn", [1024, 4096]); w_uqr = P.inp("w_uqr", [1024, 2048]); w_uqs = P.inp("w_uqs", [1024, 2048])
    w_ukn = P.inp("w_ukn", [512, 4096]); w_ukv = P.inp("w_ukvv", [512, 4096])
    posinfo = P.inp("posinfo", [1, 2])
    with P.scope() as es:
        QN = P.sb(es, "E_QN", [128, 1024]); bQN = Buf("QN")
        P.bload(QN[:], q_norm[0:1, :], [], bQN)
        P.norm_T("E1", P.zcq, P.b_zcq, NOWN, 1024, lambda ti: QN, None, P.cqT, P.b_cqT, [bQN])
    with P.scope() as es:
        KVN = P.sb(es, "E_KVN", [128, 512]); bKVN = Buf("KVN")
        P.bload(KVN[:], kv_norm[0:1, :], [], bKVN)
        P.norm_T("E2", P.zckv, P.b_zckv, NTOK, 512, lambda ti: KVN, None, P.ckvT, P.b_ckvT, [bKVN])
    with P.scope() as es:
        stb = P.rot_sb(es, "E_stb", [128, 512], BF16, 3)
        stf = P.rot_sb(es, "E_stf", [128, 512], F32, 3)
        E = P.store_epi
        P.mm_phase("E4", P.cqT, P.b_cqT, NOWN, 1024, [
            dict(w=w_uqn, N=4096, mode="fm", epi=E(stb, P.qnT, P.b_qnT, BF16)),
            dict(w=w_uqr, N=2048, mode="fm", epi=E(stf, P.qrA, P.b_qrA, F32)),
            dict(w=w_uqs, N=2048, mode="fm", epi=E(stf, P.qrB, P.b_qrB, F32))])
        P.mm_phase("E5", P.ckvT, P.b_ckvT, NTOK, 512, [
            dict(w=w_ukn, N=4096, mode="fm", epi=E(stb, P.knT, P.b_knT, BF16)),
            dict(w=w_ukv, N=4096, mode="tm", epi=E(stb, P.v_a, P.b_v_a, BF16))], TG=1152)
    with P.scope() as es:
        sb = lambda n, s, d=F32: P.sb(es, "R_" + n, s, d)
        CC = sb("CC", [64, NTOK]); SSn = sb("SS", [64, NTOK]); KR = sb("KR", [64, NTOK], BF16)
        bCC, bSS, bKR = Buf("CC"), Buf("SS"), Buf("KR")
        with P.scope() as es2:
            t2 = lambda n, s, d=F32: P.sb(es2, "RT_" + n, s, d)
            ii = t2("ii", [64, 2048], I32); tf = t2("tf", [64, 2048]); rw = t2("rw", [64, 2048]); cl = t2("cl", [64, 2048])
            ki = t2("ki", [64, 2048], I32); ang = t2("ang", [64, 2048]); tmp = t2("tmp", [64, 2048])
            pi_ = t2("pi", [64, 2]); pp = t2("pp", [64, 8]); ppi = t2("ppi", [64, 2], I32)
            b = {n: Buf(n) for n in "ii tf rw cl ki ang tmp pi pp ppi".split()}
            P.bload(pi_[:], posinfo[0:1, :], [], b["pi"], 64)
            S.run("pool", [], [b["ii"]], lambda: nc.gpsimd.iota(ii[:], pattern=[[1, 2048]], base=0, channel_multiplier=0))
            S.run("pool", [], [b["ppi"]], lambda: nc.gpsimd.iota(ppi[:, 0:1], pattern=[[0, 1]], base=0, channel_multiplier=1))
            R_ = lambda e, r, w, f: S.run(e, [b[x] for x in r], [b[x] for x in w], f)
            R_("dve", ["ii"], ["tf"], lambda: nc.vector.tensor_copy(out=tf[:], in_=ii[:]))
            R_("dve", ["tf", "pi"], ["tf"], lambda: nc.vector.tensor_scalar(out=tf[:], in0=tf[:], scalar1=pi_[:, 1:2], scalar2=pi_[:, 0:1], op0=ALU.mult, op1=ALU.add))
            R_("dve", ["tf"], ["tmp"], lambda: nc.vector.tensor_scalar(out=tmp[:], in0=tf[:], scalar1=-31.5, scalar2=1.0 / 64, op0=ALU.add, op1=ALU.mult))
            R_("dve", ["tmp"], ["ki"], lambda: nc.vector.tensor_copy(out=ki[:], in_=tmp[:]))
            R_("dve", ["ki"], ["rw"], lambda: nc.vector.tensor_copy(out=rw[:], in_=ki[:]))
            R_("dve", ["rw", "tf"], ["cl"], lambda: nc.vector.scalar_tensor_tensor(out=cl[:], in0=rw[:], scalar=-64.0, in1=tf[:], op0=ALU.mult, op1=ALU.add))
            R_("dve", ["ppi"], ["pp"], lambda: nc.vector.tensor_copy(out=pp[:, 0:1], in_=ppi[:, 0:1]))
            R_("dve", ["pp"], ["pp"], lambda: nc.vector.tensor_scalar(out=pp[:, 1:2], in0=pp[:, 0:1], scalar1=-15.5, scalar2=1.0 / 32, op0=ALU.add, op1=ALU.mult))
            R_("dve", ["pp"], ["ppi"], lambda: nc.vector.tensor_copy(out=ppi[:, 1:2], in_=pp[:, 1:2]))
            R_("dve", ["ppi"], ["pp"], lambda: nc.vector.tensor_copy(out=pp[:, 1:2], in_=ppi[:, 1:2]))
            R_("dve", ["pp"], ["pp"], lambda: nc.vector.scalar_tensor_tensor(out=pp[:, 2:3], in0=pp[:, 1:2], scalar=-32.0, in1=pp[:, 0:1], op0=ALU.mult, op1=ALU.add))
            R_("dve", ["pp"], ["pp"], lambda: nc.vector.tensor_scalar(out=pp[:, 3:4], in0=pp[:, 2:3], scalar1=15.5, scalar2=None, op0=ALU.is_lt))
            R_("dve", ["pp"], ["pp"], lambda: nc.vector.scalar_tensor_tensor(out=pp[:, 4:5], in0=pp[:, 3:4], scalar=16.0, in1=pp[:, 2:3], op0=ALU.mult, op1=ALU.add))
            R_("dve", ["pp"], ["pp"], lambda: nc.vector.tensor_scalar(out=pp[:, 4:5], in0=pp[:, 4:5], scalar1=-16.0, scalar2=None, op0=ALU.add))
            R_("act", ["pp"], ["pp"], lambda: nc.scalar.activation(out=pp[:, 5:6], in_=pp[:, 4:5], func=AF.Exp, scale=-float(np.log(10000.0)) / 16))
            R_("dve", ["pp"], ["pp"], lambda: nc.vector.tensor_scalar(out=pp[:, 6:7], in0=pp[:, 0:1], scalar1=31.5, scalar2=2.0, op0=ALU.is_gt, op1=ALU.mult))
            R_("dve", ["pp"], ["pp"], lambda: nc.vector.tensor_scalar(out=pp[:, 6:7], in0=pp[:, 6:7], scalar1=-1.0, scalar2=None, op0=ALU.add))
            R_("dve", ["rw", "cl"], ["tmp"], lambda: nc.vector.tensor_tensor(out=tmp[:], in0=rw[:], in1=cl[:], op=ALU.subtract))
            R_("dve", ["tmp", "cl", "pp"], ["ang"], lambda: nc.vector.scalar_tensor_tensor(out=ang[:], in0=tmp[:], scalar=pp[:, 3:4], in1=cl[:], op0=ALU.mult, op1=ALU.add))
            R_("dve", ["ang", "pp"], ["ang"], lambda: nc.vector.tensor_scalar(out=ang[:], in0=ang[:], scalar1=pp[:, 5:6], scalar2=None, op0=ALU.mult))

            def sin_of(shift, out_ap, post_scale):
                R_("dve", ["ang"], ["tmp"], lambda: nc.vector.tensor_scalar(out=tmp[:], in0=ang[:], scalar1=shift, scalar2=1.0 / (2 * np.pi), op0=ALU.add, op1=ALU.mult))
                R_("dve", ["tmp"], ["ki"], lambda: nc.vector.tensor_copy(out=ki[:], in_=tmp[:]))
                R_("dve", ["ki"], ["rw"], lambda: nc.vector.tensor_copy(out=rw[:], in_=ki[:]))
                R_("dve", ["rw", "ang"], ["tmp"], lambda: nc.vector.scalar_tensor_tensor(out=tmp[:], in0=rw[:], scalar=-2 * np.pi, in1=ang[:], op0=ALU.mult, op1=ALU.add))
                R_("dve", ["tmp"], ["tmp"], lambda: nc.vector.tensor_scalar(out=tmp[:], in0=tmp[:], scalar1=shift, scalar2=None, op0=ALU.add))
                R_("dve", ["tmp"], ["tmp"], lambda: nc.vector.tensor_scalar(out=tmp[:], in0=tmp[:], scalar1=3.1415925, scalar2=-3.1415925, op0=ALU.min, op1=ALU.max))
                if post_scale is None:
                    S.run("act", [b["tmp"]], [bCC], lambda: nc.scalar.activation(out=out_ap, in_=tmp[:], func=AF.Sin))
                else:
                    S.run("act", [b["tmp"]], [bSS], lambda: nc.scalar.activation(out=out_ap, in_=tmp[:], func=AF.Sin))
                    S.run("dve", [bSS, b["pp"]], [bSS], lambda: nc.vector.tensor_scalar(out=out_ap, in0=out_ap, scalar1=pp[:, 6:7], scalar2=None, op0=ALU.mult))
            S.run("dve", [], [bCC], lambda: nc.vector.memset(CC[:, 0:OWN0], 1.0))
            S.run("dve", [], [bSS], lambda: nc.vector.memset(SSn[:, 0:OWN0], 0.0))
            sin_of(float(np.pi / 2), CC[:, OWN0:NTOK], None)
            sin_of(0.0, SSn[:, OWN0:NTOK], True)
            za = t2("za", [64, NTOK]); zb = t2("zb", [64, NTOK]); bza, bzb = Buf("za"), Buf("zb")
            S.dma("sp", za[:], P.zkrT[:, :], [P.b_zkrT], [bza])
            S.dma("sp", zb[:], P.zkrsT[:, :], [P.b_zkrsT], [bzb])
            S.run("dve", [bza, bCC], [bza], lambda: nc.vector.tensor_tensor(out=za[:], in0=za[:], in1=CC[:], op=ALU.mult))
            S.run("dve", [bzb, bSS], [bzb], lambda: nc.vector.tensor_tensor(out=zb[:], in0=zb[:], in1=SSn[:], op=ALU.mult))
            S.run("dve", [bza, bzb], [bKR], lambda: nc.vector.tensor_tensor(out=KR[:], in0=za[:], in1=zb[:], op=ALU.add))
        kn_rot = P.rot_sb(es, "R_kn", [128, NTOK], BF16, 2)
        v_rot = P.rot_sb(es, "R_v", [128, 18, 128], BF16, 2)
        qn_rot = P.rot_sb(es, "R_qn", [128, NOWN], BF16, 2)
        qa_rot = P.rot_sb(es, "R_qa", [64, NOWN], F32, 2)
        qb_rot = P.rot_sb(es, "R_qb", [64, NOWN], F32, 2)
        qr_rot = P.rot_sb(es, "R_qr", [64, NOWN], BF16, 2)
        pt_rot = P.rot_sb(es, "R_pt", [128, 512], BF16, 3)
        y_rot = P.rot_sb(es, "R_y", [128, 512], BF16, 2)
        rl_rot = P.rot_sb(es, "R_rl", [128, 512], F32, 2)
        s_rot = P.rot_ps(es, "R_s", [128, 512], F32, 3)
        o_rot = P.rot_ps(es, "R_o", [128, 512], F32, 2)
        l_rot = P.rot_ps(es, "R_l", [128, 512], F32, 2)
        knv = P.knT.rearrange("(h p) t -> h p t", p=128)
        qnv = P.qnT.rearrange("(h p) t -> h p t", p=128)
        qav = P.qrA.rearrange("(h p) t -> h p t", p=64)
        qbv = P.qrB.rearrange("(h p) t -> h p t", p=64)
        for h in range(32):
            kn, bkn = kn_rot.next(); v, bv = v_rot.next(); qn, bqn = qn_rot.next()
            qa, bqa = qa_rot.next(); qb, bqb = qb_rot.next(); qr, bqr = qr_rot.next()
            S.dma("sp", kn[:], knv[h], [P.b_knT], [bkn])
            S.dma("sp", v[:], P.v_a[:, h * 128:(h + 1) * 128].rearrange("(kb p) d -> p kb d", p=128), [P.b_v_a], [bv])
            S.dma("sp", qn[:], qnv[h], [P.b_qnT], [bqn])
            S.dma("sp", qa[:], qav[h], [P.b_qrA], [bqa])
            S.dma("sp", qb[:], qbv[h], [P.b_qrB], [bqb])
            S.run("dve", [bqa, bCC], [bqa], lambda: nc.vector.tensor_tensor(out=qa[:], in0=qa[:], in1=CC[:, OWN0:OWN1], op=ALU.mult))
            S.run("dve", [bqb, bSS], [bqb], lambda: nc.vector.tensor_tensor(out=qb[:], in0=qb[:], in1=SSn[:, OWN0:OWN1], op=ALU.mult))
            S.run("dve", [bqa, bqb], [bqr], lambda: nc.vector.tensor_tensor(out=qr[:], in0=qa[:], in1=qb[:], op=ALU.add))
            for qt in range(2):
                qs_ = slice(qt * 512, (qt + 1) * 512)
                po, bpo = o_rot.next(); pl, bpl = l_rot.next()
                def issue_s(kb):
                    ks_ = slice(kb * 128, (kb + 1) * 128)
                    ps_, bps = s_rot.next()

                    def smm():
                        nc.tensor.matmul(ps_[:, :], lhsT=kn[:, ks_], rhs=qn[:, qs_], start=True, stop=False)
                        return nc.tensor.matmul(ps_[:, :], lhsT=KR[:, ks_], rhs=qr[:, qs_], start=False, stop=True)
                    S.run("pe", [bkn, bqn, bKR, bqr], [bps], smm)
                    return ps_, bps
                nxt = issue_s(0)
                for kb in range(18):
                    ps_, bps = nxt
                    if kb + 1 < 18:
                        nxt = issue_s(kb + 1)
                    pt, bpt = pt_rot.next()
                    S.run("act", [bps], [bpt], lambda pt=pt, ps_=ps_: nc.scalar.activation(out=pt[:], in_=ps_[:], func=AF.Exp, scale=A_SCALE))

                    def pv(pt=pt, kb=kb):
                        nc.tensor.matmul(po[:, :], lhsT=v[:, kb, :], rhs=pt[:], start=(kb == 0), stop=(kb == 17))
                        return nc.tensor.matmul(pl[:, :], lhsT=P.onesb[:, :], rhs=pt[:], start=(kb == 0), stop=(kb == 17))
                    S.run("pe", [bv, bpt, P.b_id], [bpo, bpl], pv)
                rl, brl = rl_rot.next(); y, by = y_rot.next()
                S.run("dve", [bpl], [brl], lambda rl=rl: nc.vector.reciprocal(out=rl[:], in_=pl[:]))
                S.run("dve", [bpo, brl], [by], lambda y=y, rl=rl: nc.vector.tensor_tensor(out=y[:], in0=po[:], in1=rl[:], op=ALU.mult))
                S.dma("sp", P.ybT[h * 128:(h + 1) * 128, qs_], y[:], [by], [P.b_ybT])


Prog.phase_mla = phase_mla


def phase_merge(P):
    nc, S = P.nc, P.S
    sc = P.scratch
    P.yaT, P.b_yaT = sc("yaT", [D, NOWN], BF16)
    P.m1T, P.b_m1T = sc("m1T", [D, NOWN], F32)
    P.mT, P.b_mT = sc("mT", [D, NOWN], BF16)
    P.y, P.b_y = sc("y", [NOWN, D], F32)
    wa = P.inp("w_branch_a", [D, D]); wb = P.inp("w_branch_b", [D, D]); wo = P.inp("w_out", [D, D])
    mon = P.inp("m_out_norm", [1, D])
    with P.scope() as es:
        MON, bMON = small_T(P, es, "F_MON", mon, 32)
        P.transpose_pass("F0", P.ya_tm, P.b_ya_tm, NOWN, D, P.yaT, P.b_yaT, scale_col=MON, scale_buf=bMON)
    with P.scope() as es:
        g_rot = P.rot_sb(es, "F_g", [128, 512], BF16, 3)
        m_rot = P.rot_sb(es, "F_m", [128, 512], F32, 3)
        t_rot = P.rot_sb(es, "F_t", [128, 512], F32, 3)
        o_rot = P.rot_sb(es, "F_o", [128, 512], BF16, 3)

        def epi1(p, c0, t0, cs, ts, pb):
            g, bg = g_rot.next(); t, bt = t_rot.next()
            S.dma("sp", g[:cs, :ts], P.sgaT[c0:c0 + cs, t0:t0 + ts], [P.b_sgaT], [bg])
            S.run("dve", [pb, bg], [bt], lambda: nc.vector.tensor_tensor(out=t[:cs, :ts], in0=p, in1=g[:cs, :ts], op=ALU.mult))
            S.dma("sp", P.m1T[c0:c0 + cs, t0:t0 + ts], t[:cs, :ts], [bt], [P.b_m1T])

        def epi2(p, c0, t0, cs, ts, pb):
            g, bg = g_rot.next(); t, bt = t_rot.next(); m, bm = m_rot.next(); o, bo = o_rot.next()
            S.dma("sp", g[:cs, :ts], P.sgbT[c0:c0 + cs, t0:t0 + ts], [P.b_sgbT], [bg])
            S.dma("sp", m[:cs, :ts], P.m1T[c0:c0 + cs, t0:t0 + ts], [P.b_m1T], [bm])
            S.run("dve", [pb, bg], [bt], lambda: nc.vector.tensor_tensor(out=t[:cs, :ts], in0=p, in1=g[:cs, :ts], op=ALU.mult))
            S.run("dve", [bt, bm], [bo], lambda: nc.vector.tensor_tensor(out=o[:cs, :ts], in0=t[:cs, :ts], in1=m[:cs, :ts], op=ALU.add))
            S.dma("sp", P.mT[c0:c0 + cs, t0:t0 + ts], o[:cs, :ts], [bo], [P.b_mT])
        P.mm_phase("F1", P.yaT, P.b_yaT, NOWN, D, [dict(w=wa, N=D, mode="fm", epi=epi1)])
        P.mm_phase("F2", P.ybT, P.b_ybT, NOWN, D, [dict(w=wb, N=D, mode="fm", epi=epi2)])
        P.mm_phase("F3", P.mT, P.b_mT, NOWN, D, [dict(w=wo, N=D, mode="tm", epi=P.store_epi(t_rot, P.y, P.b_y, F32))])


def _rstd(P, x, xb, junk, bjunk, st, stb, F):
    nc, S = P.nc, P.S
    S.run("dve", [], [stb], lambda: nc.vector.memset(st[:, :], 0.0))
    S.run("act", [xb, stb], [bjunk, stb], lambda: nc.scalar.activation(out=junk[:, :], in_=x, func=AF.Square, accum_out=st[:, 0:1]))
    S.run("act", [stb], [stb], lambda: nc.scalar.activation(out=st[:, 1:2], in_=st[:, 0:1], func=AF.Sqrt, bias=EPS, scale=1.0 / F))
    S.run("dve", [stb], [stb], lambda: nc.vector.reciprocal(out=st[:, 1:2], in_=st[:, 1:2]))


def phase_ffn(P, experts=range(NE)):
    nc, S = P.nc, P.S
    sc = P.scratch
    xs = P.inp("xs", [NTOK, D])
    P.x1, P.b_x1 = sc("x1", [NOWN, D], F32)
    P.h2T, P.b_h2T = sc("h2T", [D, NOWN], BF16)
    P.gT, P.b_gT = sc("gT", [NE, NOWN], F32)
    P.fo, P.b_fo = sc("fo", [NOWN, D], F32)
    n2 = P.inp("norm_post_mix", [1, D]); n3 = P.inp("norm_pre_ffn", [1, D]); n4 = P.inp("norm_post_ffn", [1, D])
    rw = P.inp("router_w", [D, NE]); rb = P.inp("router_b", [1, NE])
    w_gu = P.inp("w_gu", [NE, D, 3072]); b_gu = P.inp("b_gu", [NE, 3072])
    w_dn = P.inp("w_down", [NE, FE, D]); b_dn = P.inp("b_down", [NE, D])
    out = P.nc.dram_tensor("out", [NOWN, D], F32, kind="ExternalOutput").ap()
    P.b_out = Buf("out")
    with P.scope() as es:
        P.nv, P.b_nv = P.sb(es, "PG_nv", [128, D]), Buf("nv")
        WG1, bWG1 = P.mod_tile(es, "PG_WG1", 0, 2, n2)
        W2, bW2 = P.mod_tile(es, "PG_W2", 0, 4, n3, True)
        SH2, bSH2 = P.mod_tile(es, "PG_SH2", 0, 3)
        RW = P.sb(es, "PG_RW", [128, 32, NE]); bRW = Buf("RW")
        S.dma("sp", RW[:], rw.rearrange("(kt p) e -> p kt e", p=128), [], [bRW])
        RB = P.sb(es, "PG_RB", [128, NE]); bRB = Buf("RB")
        P.bload(RB[:], rb[0:1, :], [], bRB)
        yt = P.sb(es, "PG_y", [128, D]); xt = P.sb(es, "PG_x", [128, D]); byt, bxt = Buf("yt"), Buf("xt")
        junk = P.sb(es, "PG_junk", [128, D], BF16); bjunk = Buf("junk")
        st = P.sb(es, "PG_st", [128, 4]); bst = Buf("st")
        hf = P.sb(es, "PG_hf", [128, 32, 128]); hb = P.sb(es, "PG_hb", [128, 32, 128], BF16); bhf, bhb = Buf("hf"), Buf("hb")
        L = P.sb(es, "PG_L", [128, NE]); E_ = P.sb(es, "PG_E", [128, NE]); MX = P.sb(es, "PG_MX", [128, 16])
        bL, bE, bMX = Buf("L"), Buf("E"), Buf("MX")
        GTs = P.sb(es, "PG_GT", [NE, 128]); bGTs = Buf("GTs")
        pt_rot = P.rot_ps(es, "PG_pt", [128, 4, 128], F32, 3)
        plg = P.ps(es, "PG_plg", [128, 512]); bplg = Buf("plg")
        h2v = P.h2T.rearrange("(kt p) t -> p kt t", p=128)
        for i in range(8):
            r0 = i * 128
            S.dma("sp", yt[:], P.y[r0:r0 + 128, :], [P.b_y], [byt])
            S.dma("sp", xt[:], xs[OWN0 + r0:OWN0 + r0 + 128, :], [], [bxt])
            _rstd(P, yt[:], byt, junk, bjunk, st, bst, D)
            S.run("dve", [byt, bst, bWG1], [byt], lambda: nc.vector.scalar_tensor_tensor(
                out=yt[:], in0=yt[:], scalar=st[:, 1:2], in1=WG1[:], op0=ALU.mult, op1=ALU.mult))
            S.run("dve", [byt, bxt], [bxt], lambda: nc.vector.tensor_tensor(out=xt[:], in0=yt[:], in1=xt[:], op=ALU.add))
            S.dma("sp", P.x1[r0:r0 + 128, :], xt[:], [bxt], [P.b_x1])
            _rstd(P, xt[:], bxt, junk, bjunk, st, bst, D)
            S.run("dve", [bxt, bst, bW2], [byt], lambda: nc.vector.scalar_tensor_tensor(
                out=yt[:], in0=xt[:], scalar=st[:, 1:2], in1=W2[:], op0=ALU.mult, op1=ALU.mult))
            S.run("dve", [byt, bSH2], [byt], lambda: nc.vector.tensor_tensor(out=yt[:], in0=yt[:], in1=SH2[:], op=ALU.add))
            for g0 in range(0, 32, 4):
                p, pb = pt_rot.next()

                def tr(p=p, g0=g0):
                    for j in range(4):
                        ins = nc.tensor.transpose(out=p[:, j, :], in_=yt[:, (g0 + j) * 128:(g0 + j + 1) * 128], identity=P.idf[:, :])
                    return ins
                S.run("pe", [byt, P.b_id], [pb], tr)
                S.run("act", [pb], [bhf], lambda p=p, g0=g0: nc.scalar.copy(out=hf[:, g0:g0 + 4, :], in_=p[:]))
                S.run("dve", [bhf], [bhb], lambda g0=g0: nc.vector.tensor_copy(out=hb[:, g0:g0 + 4, :], in_=hf[:, g0:g0 + 4, :]))
            S.dma("sp", h2v[:, :, r0:r0 + 128], hb[:], [bhb], [P.b_h2T])

            def lg():
                for kt in range(32):
                    ins = nc.tensor.matmul(plg[:, 0:NE], lhsT=hf[:, kt, :], rhs=RW[:, kt, :], start=(kt == 0), stop=(kt == 31))
                return ins
            S.run("pe", [bhf, bRW], [bplg], lg)
            S.run("dve", [bplg, bRB], [bL], lambda: nc.vector.tensor_tensor(out=L[:], in0=plg[:, 0:NE], in1=RB[:], op=ALU.add))
            S.run("dve", [bL], [bMX], lambda: nc.vector.max(out=MX[:, 0:8], in_=L[:]))
            S.run("dve", [bMX], [bMX], lambda: nc.vector.tensor_scalar(out=MX[:, 8:9], in0=MX[:, 0:1], scalar1=-1.0, scalar2=None, op0=ALU.mult))
            S.run("act", [bL, bMX], [bE], lambda: nc.scalar.activation(out=E_[:], in_=L[:], func=AF.Exp, bias=MX[:, 8:9], scale=1.0))
            S.run("dve", [bL, bMX], [bL], lambda: nc.vector.tensor_scalar(out=L[:], in0=L[:], scalar1=MX[:, 3:4], scalar2=None, op0=ALU.is_ge))
            S.run("dve", [bE, bL], [bE], lambda: nc.vector.tensor_tensor(out=E_[:], in0=E_[:], in1=L[:], op=ALU.mult))
            S.run("dve", [bE], [bMX], lambda: nc.vector.reduce_sum(out=MX[:, 9:10], in_=E_[:], axis=AX.X))
            S.run("dve", [bMX], [bMX], lambda: nc.vector.reciprocal(out=MX[:, 9:10], in_=MX[:, 9:10]))
            S.run("dve", [bE, bMX], [bE], lambda: nc.vector.tensor_scalar(out=E_[:], in0=E_[:], scalar1=MX[:, 9:10], scalar2=None, op0=ALU.mult))
            S.run("pe", [bE, P.b_id], [bplg], lambda: nc.tensor.transpose(out=plg[0:NE, 128:256], in_=E_[:, :], identity=P.idf[:, :]))
            S.run("dve", [bplg], [bGTs], lambda: nc.vector.tensor_copy(out=GTs[:], in_=plg[0:NE, 128:256]))
            S.dma("sp", P.gT[:, r0:r0 + 128], GTs[:], [bGTs], [P.b_gT])
    if getattr(P, "stop_after_g", False):
        return
    TH = NOWN
    NJ = TH // 128
    bfo = [[Buf(f"fo{j}_{cb}") for cb in range(8)] for j in range(NJ)]
    P.bfo_all = [b for row in bfo for b in row]
    with P.scope() as es:
        GT = P.sb(es, "H_GT", [NE, NOWN]); bGT = Buf("GT")
        S.dma("sp", GT[:], P.gT[:, :], [P.b_gT], [bGT])
        BD = P.sb(es, "H_BD", [NE, D]); bBD = Buf("BD")
        S.dma("sp", BD[:], b_dn[:, :], [], [bBD])
        BG = P.sb(es, "H_BG", [128, NE * 24]); bBG = Buf("BG")
        with P.scope() as es2:
            r_ = P.sb(es2, "H_bgr", [128, 128]); br_ = Buf("bgr")
            pp_ = P.ps(es2, "H_bgp", [128, 128]); bpp = Buf("bgp")
            bgv = b_gu.rearrange("e (r p) -> (e r) p", p=128)
            for blk in range(6):
                S.dma("sp", r_[:], bgv[blk * 128:(blk + 1) * 128, :], [], [br_])
                S.run("pe", [br_, P.b_id], [bpp], lambda: nc.tensor.transpose(out=pp_[:, :], in_=r_[:, :], identity=P.idf[:, :]))
                S.run("dve", [bpp], [bBG], lambda blk=blk: nc.vector.tensor_copy(out=BG[:, blk * 128:(blk + 1) * 128], in_=pp_[:, :]))
        act = P.sb(es, "H_act", [128, 32, TH], BF16); bact = Buf("act")
        actT = P.sb(es, "H_actT", [128, 12, TH], BF16); bactT = Buf("actT")
        GB = P.rot_sb(es, "H_GB", [128, TH], F32, 2)
        wg_rot = P.rot_sb(es, "H_wg", [128, 32, 256], BF16, 2)
        wd_rot = P.rot_sb(es, "H_wd", [128, 12, 512], BF16, 2)
        st_rot = P.rot_sb(es, "H_st", [128, 512], F32, 4)
        G1 = P.sb(es, "H_G1", [128, 512]); SG = P.sb(es, "H_SG", [128, 512]); L1 = P.sb(es, "H_L1", [128, 512])
        bG1, bSG, bL1 = Buf("G1"), Buf("SG"), Buf("L1")
        pg_rot = P.rot_ps(es, "H_pg", [128, 512], F32, 2)
        pl_rot = P.rot_ps(es, "H_pl", [128, 512], F32, 2)
        pd_rot = P.rot_ps(es, "H_pd", [128, 512], F32, 3)
        h2v = P.h2T.rearrange("(kt p) t -> p kt t", p=128)
        S.dma("sp", act[:], h2v[:, :, 0:TH], [P.b_h2T], [bact])
        for j in range(NJ):
            for cb in range(8):
                pd, bpd = pd_rot.next()
                S.run("pe", [bGT, bBD], [bpd], lambda pd=pd, j=j, cb=cb: nc.tensor.matmul(
                    pd[:, :], lhsT=GT[:, j * 128:(j + 1) * 128], rhs=BD[:, cb * 512:(cb + 1) * 512], start=True, stop=True))
                stt, bst = st_rot.next()
                P.evac([bpd], [bst], stt[:, :], pd[:, :])
                S.dma("sp", P.fo[j * 128:(j + 1) * 128, cb * 512:(cb + 1) * 512], stt[:, :], [bst], [bfo[j][cb]])
        blocks = []
        for e in experts:
            blocks += [("g", e, c) for c in range(12)] + [("d", e, cb) for cb in range(8)]
        loaded = {}

        def load(k):
            kind, e, x = blocks[k]
            if kind == "g":
                w, bw = wg_rot.next()
                S.dma("pq", w[:], w_gu[e].rearrange("(kt p) n -> p kt n", p=128)[:, :, x * 256:(x + 1) * 256], [], [bw])
            else:
                w, bw = wd_rot.next()
                S.dma("pq", w[:], w_dn[e].rearrange("(c p) n -> p c n", p=128)[:, :, x * 512:(x + 1) * 512], [], [bw])
            loaded[k] = (w, bw)
        load(0)
        gb = bgb = None
        for k, (kind, e, x) in enumerate(blocks):
            if k + 1 < len(blocks):
                load(k + 1)
            w, bw = loaded.pop(k)
            if kind == "g":
                c = x
                if c == 0:
                    gb, bgb = GB.next()
                    S.dma("sp", gb[:], P.gT[e:e + 1, 0:TH].partition_broadcast(128), [P.b_gT], [bgb])
                col = e * 24 + c * 2
                for hf_ in range(TH // 512):
                    ts_ = slice(hf_ * 512, (hf_ + 1) * 512)
                    pg, bpg = pg_rot.next(); pl, bpl = pl_rot.next()

                    def gmm(pg=pg, w=w, ts_=ts_):
                        for kt in range(32):
                            ins = nc.tensor.matmul(pg[:, :], lhsT=w[:, kt, 0:128], rhs=act[:, kt, ts_], start=(kt == 0), stop=(kt == 31))
                        return ins

                    def lmm(pl=pl, w=w, ts_=ts_):
                        for kt in range(32):
                            ins = nc.tensor.matmul(pl[:, :], lhsT=w[:, kt, 128:256], rhs=act[:, kt, ts_], start=(kt == 0), stop=(kt == 31))
                        return ins
                    S.run("pe", [bact, bw], [bpg], gmm)
                    S.run("pe", [bact, bw], [bpl], lmm)
                    S.run("dve", [bpg, bBG], [bG1], lambda pg=pg, col=col: nc.vector.tensor_scalar(
                        out=G1[:], in0=pg[:, :], scalar1=BG[:, col:col + 1], scalar2=7.0, op0=ALU.add, op1=ALU.min))
                    S.run("act", [bG1], [bSG], lambda: nc.scalar.activation(out=SG[:], in_=G1[:], func=AF.Sigmoid, scale=1.702))
                    S.run("dve", [bpl, bBG], [bL1], lambda pl=pl, col=col: nc.vector.tensor_scalar(
                        out=L1[:], in0=pl[:, :], scalar1=BG[:, col + 1:col + 2], scalar2=7.0, op0=ALU.add, op1=ALU.min))
                    S.run("dve", [bL1], [bL1], lambda: nc.vector.tensor_scalar(out=L1[:], in0=L1[:], scalar1=-7.0, scalar2=1.0, op0=ALU.max, op1=ALU.add))
                    S.run("dve", [bG1, bSG], [bG1], lambda: nc.vector.tensor_tensor(out=G1[:], in0=G1[:], in1=SG[:], op=ALU.mult))
                    S.run("dve", [bL1, bgb], [bL1], lambda gb=gb, ts_=ts_: nc.vector.tensor_tensor(out=L1[:], in0=L1[:], in1=gb[:, ts_], op=ALU.mult))
                    S.run("dve", [bG1, bL1], [bactT], lambda c=c, ts_=ts_: nc.vector.tensor_tensor(out=actT[:, c, ts_], in0=G1[:], in1=L1[:], op=ALU.mult))
            else:
                cb = x
                for j in range(NJ):
                    pd, bpd = pd_rot.next()

                    def dmm(pd=pd, w=w, j=j):
                        for c in range(12):
                            ins = nc.tensor.matmul(pd[:, :], lhsT=actT[:, c, j * 128:(j + 1) * 128], rhs=w[:, c, :], start=(c == 0), stop=(c == 11))
                        return ins
                    S.run("pe", [bactT, bw], [bpd], dmm)
                    stt, bst = st_rot.next()
                    P.evac([bpd], [bst], stt[:, :], pd[:, :])
                    S.dma("pq", P.fo[j * 128:(j + 1) * 128, cb * 512:(cb + 1) * 512], stt[:, :], [bst], [bfo[j][cb]], accum_op=ALU.add)
    with P.scope() as es:
        P.nv, P.b_nv = P.sb(es, "I_nv", [128, D]), Buf("nv")
        WG2, bWG2 = P.mod_tile(es, "I_WG2", 0, 5, n4)
        f_rot = P.rot_sb(es, "I_f", [128, D], F32, 2)
        x_rot = P.rot_sb(es, "I_x", [128, D], F32, 2)
        junk = P.sb(es, "I_junk", [128, D], BF16); bjunk = Buf("junk")
        st_rot = P.rot_sb(es, "I_st", [128, 4], F32, 2)
        for i in range(8):
            r0 = i * 128
            ft, bft = f_rot.next(); xt, bxt = x_rot.next(); st, bst = st_rot.next()
            S.dma("sp", ft[:], P.fo[r0:r0 + 128, :], P.bfo_all, [bft])
            S.dma("sp", xt[:], P.x1[r0:r0 + 128, :], [P.b_x1], [bxt])
            _rstd(P, ft[:], bft, junk, bjunk, st, bst, D)
            S.run("dve", [bft, bst, bWG2], [bft], lambda: nc.vector.scalar_tensor_tensor(
                out=ft[:], in0=ft[:], scalar=st[:, 1:2], in1=WG2[:], op0=ALU.mult, op1=ALU.mult))
            S.run("dve", [bft, bxt], [bxt], lambda: nc.vector.tensor_tensor(out=xt[:], in0=ft[:], in1=xt[:], op=ALU.add))
            S.dma("sp", out[r0:r0 + 128, :], xt[:], [bxt], [P.b_out])


Prog.phase_merge = phase_merge
Prog.phase_ffn = phase_ffn


def build_full(debug=()):
    P = Prog(debug=debug)
    with ExitStack() as es:
        P.consts(es)
        P.phase_mod(); P.phase_h(); P.phase_proj(); P.phase_mlstm(); P.phase_mla(); P.phase_merge(); P.phase_ffn()
        P.S.finish([P.b_out] + [getattr(P, "b_" + n) for n in debug])
    return P


_prog = None


def kernel(**inputs):
    global _prog
    if _prog is None:
        _prog = build_full()
    P = _prog
    names = list(P.inputs)
    in_maps = [core_inputs(inputs, c, names) for c in range(8)]
    res = run_bass_kernel_spmd(P.nc, in_maps, core_ids=list(range(8)))
    out = np.empty((4, 2048, D), np.float32)
    for c in range(8):
        b, half = c // 2, c % 2
        o = np.asarray(res.results[c]["out"], dtype=np.float32)
        if half == 0:
            out[b, 0:1024] = o
        else:
            out[b, 1024:2048] = o[::-1]
    return out
```
